# Optimizing a Trainium2 kernel written in Bass

```python
import math
import jax, jax.numpy as jnp
from jax import lax
import numpy as np

D_MODEL = 2048
BATCH = 8
SEQ = 4096
DEPTH = 4

N_MIXERS = 2
N_HEADS = 16
HEAD_DIM = D_MODEL // N_HEADS
D_INNER = N_HEADS * HEAD_DIM
Q_RANK = 512
KV_RANK = 256
IDX_HEADS = 16
IDX_DIM = 64
TOPK_MAX = 256
N_BUCKETS = 32
MAX_DISTANCE = 128
BLOCK_Q = 128
EPS = 1e-6
A_IN = Q_RANK + KV_RANK + IDX_DIM + IDX_HEADS + D_INNER
B_IN = 3 * D_INNER + N_HEADS + D_INNER

kernel_name = "hybrid_dsa_fox_gated_trunk"


def rmsnorm(x, g):
    xf = x.astype(jnp.float32)
    y = xf * lax.rsqrt(jnp.mean(xf * xf, axis=-1, keepdims=True) + EPS)
    return (y * g.astype(jnp.float32)).astype(x.dtype)


def t5_bucket(dist):
    max_exact = N_BUCKETS // 2
    d = jnp.maximum(dist, 0)
    df = jnp.maximum(d, 1).astype(jnp.float32)
    large = max_exact + (jnp.log(df / max_exact) / math.log(MAX_DISTANCE / max_exact)
                         * (N_BUCKETS - max_exact)).astype(jnp.int32)
    large = jnp.minimum(large, N_BUCKETS - 1)
    return jnp.where(d < max_exact, d, large)


def to_blocks(a):
    b, s = a.shape[:2]
    return jnp.swapaxes(a.reshape(b, s // BLOCK_Q, BLOCK_Q, *a.shape[2:]), 0, 1)


def from_blocks(a):
    nb, b, q = a.shape[:3]
    return jnp.swapaxes(a, 0, 1).reshape(b, nb * q, *a.shape[3:])


def dsa_mixer(h, w_in, q_norm, kv_norm, w_q_up, w_uk, w_uv, w_iq, w_out, rel_bias):
    b, s, _ = h.shape
    topk = min(TOPK_MAX, s // 4)
    proj = h @ w_in
    o1 = Q_RANK
    o2 = o1 + KV_RANK
    o3 = o2 + IDX_DIM
    o4 = o3 + IDX_HEADS
    cq = rmsnorm(proj[..., :o1], q_norm)
    c_kv = rmsnorm(proj[..., o1:o2], kv_norm)
    ik = proj[..., o2:o3]
    iw = proj[..., o3:o4] * (IDX_HEADS ** -0.5 * IDX_DIM ** -0.5)
    gate = proj[..., o4:]
    q = (cq @ w_q_up).reshape(b, s, N_HEADS, HEAD_DIM)
    q_abs = jnp.einsum('bshd,rhd->bshr', q, w_uk) * (HEAD_DIM ** -0.5)
    iq = (cq @ w_iq).reshape(b, s, IDX_HEADS, IDX_DIM)
    pos = jnp.arange(s, dtype=jnp.int32)

    def block(args):
        qa, iqb, iwb, tq = args
        dots = jnp.einsum('bthd,bsd->bths', iqb, ik)
        score = jnp.einsum('bths,bth->bts', jax.nn.relu(dots), iwb).astype(jnp.float32)
        causal = pos[None, :] <= tq[:, None]
        score = jnp.where(causal[None], score, -jnp.inf)
        _, idx = lax.top_k(score, topk)
        c_sel = jax.vmap(lambda c, i: c[i])(c_kv, idx)
        dist = tq[None, :, None] - idx
        valid = (dist >= 0)[:, None]
        bias = jnp.moveaxis(rel_bias[t5_bucket(dist)], -1, 1)
        logits = (jnp.einsum('bthr,btkr->bhtk', qa, c_sel).astype(jnp.float32)
                  + bias.astype(jnp.float32))
        logits = jnp.where(valid, logits, -jnp.inf)
        p = jax.nn.softmax(logits, axis=-1).astype(c_sel.dtype)
        return jnp.einsum('bhtk,btkr->bthr', p, c_sel)

    o_lat = from_blocks(lax.map(block, (to_blocks(q_abs), to_blocks(iq), to_blocks(iw),
                                        pos.reshape(-1, BLOCK_Q))))
    o = jnp.einsum('bshr,rhd->bshd', o_lat, w_uv).reshape(b, s, D_INNER)
    return (o * jax.nn.silu(gate)) @ w_out


def fox_mixer(h, w_in, f_bias, w_out):
    b, s, _ = h.shape
    proj = h @ w_in
    q = proj[..., :D_INNER].reshape(b, s, N_HEADS, HEAD_DIM) * (HEAD_DIM ** -0.5)
    k = proj[..., D_INNER:2 * D_INNER].reshape(b, s, N_HEADS, HEAD_DIM)
    v = proj[..., 2 * D_INNER:3 * D_INNER].reshape(b, s, N_HEADS, HEAD_DIM)
    f_pre = proj[..., 3 * D_INNER:3 * D_INNER + N_HEADS]
    gate = proj[..., 3 * D_INNER + N_HEADS:]
    log_f = jax.nn.log_sigmoid((f_pre + f_bias).astype(jnp.float32))
    cum = lax.cumsum(log_f, axis=1)
    cum_keys = jnp.moveaxis(cum, -1, 1)
    pos = jnp.arange(s, dtype=jnp.int32)

    def block(args):
        qb, cb, tq = args
        decay = jnp.moveaxis(cb, -1, 1)[..., None] - cum_keys[:, :, None, :]
        logits = jnp.einsum('bthd,bshd->bhts', qb, k).astype(jnp.float32) + decay
        causal = pos[None, :] <= tq[:, None]
        logits = jnp.where(causal[None, None], logits, -jnp.inf)
        p = jax.nn.softmax(logits, axis=-1).astype(v.dtype)
        return jnp.einsum('bhts,bshd->bthd', p, v)

    o = from_blocks(lax.map(block, (to_blocks(q), to_blocks(cum), pos.reshape(-1, BLOCK_Q))))
    o = o.reshape(b, s, D_INNER)
    return (o * jax.nn.silu(gate)) @ w_out


def setup_inputs(seed: int = 0) -> dict:
    key = jax.random.key(seed)
    ks = jax.random.split(key, 20)
    n_a = (DEPTH + 1) // 2
    n_b = DEPTH // 2
    nrm = lambda k, shape, scale: jax.random.normal(k, shape, jnp.float32) * scale
    return {
        "x": nrm(ks[0], (BATCH, SEQ, D_MODEL), 1.0),
        "norm_g": 1.0 + nrm(ks[1], (DEPTH, D_MODEL), 0.02),
        "final_g": 1.0 + nrm(ks[2], (D_MODEL,), 0.02),
        "rel_bias": nrm(ks[3], (N_BUCKETS, N_HEADS), 0.5),
        "a_w_in": nrm(ks[4], (n_a, D_MODEL, A_IN), D_MODEL ** -0.5),
        "a_q_norm": 1.0 + nrm(ks[5], (n_a, Q_RANK), 0.02),
        "a_kv_norm": 1.0 + nrm(ks[6], (n_a, KV_RANK), 0.02),
        "a_w_q_up": nrm(ks[7], (n_a, Q_RANK, D_INNER), Q_RANK ** -0.5),
        "a_w_uk": nrm(ks[8], (n_a, KV_RANK, N_HEADS, HEAD_DIM), KV_RANK ** -0.5),
        "a_w_uv": nrm(ks[9], (n_a, KV_RANK, N_HEADS, HEAD_DIM), KV_RANK ** -0.5),
        "a_w_iq": nrm(ks[10], (n_a, Q_RANK, IDX_HEADS * IDX_DIM), Q_RANK ** -0.5),
        "a_w_out": nrm(ks[11], (n_a, D_INNER, D_MODEL), D_INNER ** -0.5),
        "b_w_in": nrm(ks[12], (n_b, D_MODEL, B_IN), D_MODEL ** -0.5),
        "b_f_bias": 4.0 + nrm(ks[13], (n_b, N_HEADS), 0.5),
        "b_w_out": nrm(ks[14], (n_b, D_INNER, D_MODEL), D_INNER ** -0.5),
    }


def reference(x, norm_g, final_g, rel_bias, a_w_in, a_q_norm, a_kv_norm, a_w_q_up,
              a_w_uk, a_w_uv, a_w_iq, a_w_out, b_w_in, b_f_bias, b_w_out):
    h = x
    for i in range(DEPTH):
        hn = rmsnorm(h, norm_g[i])
        j = i // N_MIXERS
        if i % N_MIXERS == 0:
            y = dsa_mixer(hn, a_w_in[j], a_q_norm[j], a_kv_norm[j], a_w_q_up[j],
                          a_w_uk[j], a_w_uv[j], a_w_iq[j], a_w_out[j], rel_bias)
        else:
            y = fox_mixer(hn, b_w_in[j], b_f_bias[j], b_w_out[j])
        h = h + y
    return rmsnorm(h, final_g)
```

```python
import numpy as np
import concourse.bass as bass
import concourse.mybir as mybir
from concourse.bass_utils import run_bass_kernel_spmd

F32 = mybir.dt.float32
BF16 = mybir.dt.bfloat16
AF = mybir.ActivationFunctionType
ALU = mybir.AluOpType
AX = mybir.AxisListType

D = 2048
H = 16
DH = 128
NC_ = 16
QR = 512
KVR = 256
IDXH = 16
IDXD = 64
A_IN = QR + KVR + IDXD + IDXH + D
B_IN = 3 * D + H + D
EPS = 1e-6
NEG = -30000.0


class Dep:
    __slots__ = ("name", "w", "r", "dsem", "dcnt")

    def __init__(self, name):
        self.name = name
        self.w = {}
        self.r = {}
        self.dsem = None


class T:
    def __init__(self, t, name):
        self.t = t
        self.dep = Dep(name)

    def __getitem__(self, idx):
        return self.t[idx]


def _dep(d):
    return d.dep if isinstance(d, T) else d


class KB:
    NDMA = 48

    def __init__(self, nc):
        self.nc = nc
        self.engs = {"pe": nc.tensor, "act": nc.scalar, "dve": nc.vector,
                     "pool": nc.gpsimd, "sp": nc.sync}
        self._stack = [[]]
        self.semobj = {}
        self.semval = {}
        self.sems = {}
        for e in self.engs:
            key = "e_" + e
            self.semobj[key] = self._enter(nc.semaphore("s_" + e))
            self.semval[key] = 0
            self.sems[e] = key
        self.dfree = []
        for i in range(self.NDMA):
            key = "d_%d" % i
            self.semobj[key] = self._enter(nc.semaphore("sd_%d" % i))
            self.semval[key] = 0
            self.dfree.append(key)
        self.seen = {e: {} for e in self.engs}
        self.scope_deps = [[]]

    def _enter(self, cm):
        obj = cm.__enter__()
        self._stack[-1].append(cm)
        return obj

    def push(self):
        self._stack.append([])
        self.scope_deps.append([])

    def pop(self):
        self.barrier()
        for d in self.scope_deps.pop():
            if d.dsem is not None:
                self.dfree.append(d.dsem)
                d.dsem = None
        for cm in reversed(self._stack.pop()):
            cm.__exit__(None, None, None)

    def close(self):
        while len(self._stack) > 1:
            self.pop()
        for cm in reversed(self._stack[0]):
            cm.__exit__(None, None, None)

    def sb(self, name, shape, dt):
        self.uid = getattr(self, "uid", 0) + 1
        name = "%s_u%d" % (name, self.uid)
        t = T(self._enter(self.nc.sbuf_tensor(name, list(shape), dt)), name)
        self.scope_deps[-1].append(t.dep)
        return t

    def ps(self, name, shape, dt):
        t = T(self._enter(self.nc.psum_tensor(name, list(shape), dt)), name)
        self.scope_deps[-1].append(t.dep)
        return t

    def region(self, name):
        return Dep(name)

    def _wait(self, eng, key, val):
        if self.seen[eng].get(key, 0) >= val:
            return
        self.engs[eng].wait_ge(self.semobj[key], val)
        self.seen[eng][key] = val

    def _deps(self, eng, reads, writes, partial):
        own = self.sems[eng]
        skip_own = (eng == "pe")
        for d in reads:
            d = _dep(d)
            for k, (v, _p) in d.w.items():
                if skip_own and k == own:
                    continue
                self._wait(eng, k, v)
        for d in writes:
            d = _dep(d)
            for k, v in d.r.items():
                if skip_own and k == own:
                    continue
                self._wait(eng, k, v)
            for k, (v, p) in d.w.items():
                if partial and p:
                    continue
                if skip_own and k == own:
                    continue
                self._wait(eng, k, v)

    def _record(self, key, val, reads, writes, partial):
        for d in reads:
            d = _dep(d)
            if d.r.get(key, 0) < val:
                d.r[key] = val
        for d in writes:
            d = _dep(d)
            if partial and not d.r and all(p for (_v, p) in d.w.values()):
                d.w[key] = (val, True)
            else:
                d.w = {key: (val, partial)}
                d.r = {}

    def op(self, eng, fn, reads=(), writes=(), partial=False, inc=True):
        self._deps(eng, reads, writes, partial)
        ins = fn()
        key = self.sems[eng]
        if inc:
            self.semval[key] += 1
            ins.then_inc(self.semobj[key], 1)
            val = self.semval[key]
        else:
            val = self.semval[key] + 1
        self._record(key, val, reads, writes, partial)
        return ins

    def dma(self, q, out, in_, reads=(), writes=(), sem_of=None, partial=True, **kw):
        self._deps(q, reads, writes, partial)
        d = _dep(sem_of)
        if d.dsem is None:
            d.dsem = self.dfree.pop()
        key = d.dsem
        self.semval[key] += 16
        ins = self.engs[q].dma_start(out=out, in_=in_, **kw)
        ins.then_inc(self.semobj[key], 16)
        self._record(key, self.semval[key], reads, writes, partial)
        return ins

    def barrier(self):
        for e in self.engs:
            for key, v in self.semval.items():
                if v > 0:
                    self._wait(e, key, v)

    def wait_all_on(self, eng):
        for key, v in self.semval.items():
            if v > 0:
                self._wait(eng, key, v)


class Prog:
    def __init__(self, S, layers, first_src_is_x=True, do_final=True):
        self.S = S
        self.NTB = S // 512
        self.NTK = S // 128
        self.layers = layers
        self.do_final = do_final
        nc = self.nc = bass.Bass("TRN2", target_bir_lowering=False)
        self.kb = KB(nc)
        kb = self.kb
        dt = nc.dram_tensor
        self.xT = T(dt("xT", [D, S], F32, kind="ExternalInput").ap(), "xT")
        if do_final:
            self.outT = T(dt("outT", [D, S], F32, kind="ExternalOutput").ap(), "outT")
        self.hT = T(dt("hT", [D, S], F32, kind="Internal" if do_final else "ExternalOutput").ap(), "hT")
        self.consts_d = dt("consts", [128, 6, 128], F32, kind="ExternalInput").ap()
        self.gcols_d = dt("gcols", [128, 5, 16], F32, kind="ExternalInput").ap()
        self.w = {}
        for kind, li in layers:
            if kind == "b":
                self.w[li] = dict(
                    w_in=dt("w_in%d" % li, [D, B_IN], F32, kind="ExternalInput").ap(),
                    f_bias=dt("f_bias%d" % li, [16, 1], F32, kind="ExternalInput").ap(),
                    w_out=dt("w_out%d" % li, [D, D], F32, kind="ExternalInput").ap(),
                )
            else:
                self.w[li] = dict(
                    w_in=dt("w_in%d" % li, [D, A_IN], F32, kind="ExternalInput").ap(),
                    qn=dt("qn%d" % li, [128, 4], F32, kind="ExternalInput").ap(),
                    kvn=dt("kvn%d" % li, [128, 2], F32, kind="ExternalInput").ap(),
                    w_q_up=dt("w_q_up%d" % li, [QR, D], F32, kind="ExternalInput").ap(),
                    w_ukT=dt("w_ukT%d" % li, [H, DH, KVR], F32, kind="ExternalInput").ap(),
                    w_uv=dt("w_uv%d" % li, [KVR, D], F32, kind="ExternalInput").ap(),
                    w_iq=dt("w_iq%d" % li, [QR, IDXH * IDXD], F32, kind="ExternalInput").ap(),
                    w_out=dt("w_out%d" % li, [D, D], F32, kind="ExternalInput").ap(),
                    rb31=dt("rb31_%d" % li, [128, 16], F32, kind="ExternalInput").ap(),
                    rbD=dt("rbD_%d" % li, [128, 16, 2, 128], F32, kind="ExternalInput").ap(),
                )
        self.qT_s = T(dt("qT_s", [D, S], BF16, kind="Internal").ap(), "qT_s")
        self.kT_s = T(dt("kT_s", [D, S], BF16, kind="Internal").ap(), "kT_s")
        self.v_s = T(dt("v_s", [S, D], BF16, kind="Internal").ap(), "v_s")
        self.gT_s = T(dt("gT_s", [D, S], BF16, kind="Internal").ap(), "gT_s")

        self.consts = kb.sb("consts_sb", [128, 6, 128], F32)
        self.gcols = kb.sb("gcols_sb", [128, 5, 16], F32)
        kb.dma("sp", self.consts[:], self.consts_d, writes=[self.consts], sem_of=self.consts)
        kb.dma("sp", self.gcols[:], self.gcols_d, writes=[self.gcols], sem_of=self.gcols)
        self.ident_bf = kb.sb("ident_bf", [128, 128], BF16)
        self.tri_bf = kb.sb("tri_bf", [128, 128], BF16)
        self.ones_bf = kb.sb("ones_bf", [128, 128], BF16)
        for dst, ci in ((self.ident_bf, 0), (self.tri_bf, 1), (self.ones_bf, 3)):
            kb.op("dve", lambda: nc.vector.tensor_copy(out=dst[:], in_=self.consts[:, ci, :]),
                  reads=[self.consts], writes=[dst])
        self.epscol = kb.sb("epscol", [128, 1], F32)
        kb.op("dve", lambda: nc.vector.memset(self.epscol[:], EPS), writes=[self.epscol])
        self.pow2_d = dt("pow2", [128, 17], F32, kind="ExternalInput").ap()
        self.pow2 = kb.sb("pow2_sb", [128, 17], F32)
        kb.dma("sp", self.pow2[:], self.pow2_d, writes=[self.pow2], sem_of=self.pow2)
        self.qa_s = T(dt("qa_s", [2 * D, S], BF16, kind="Internal").ap(), "qa_s")
        self.mT_s = T(dt("mT_s", [S, S], BF16, kind="Internal").ap(), "mT_s")
        self.psb = [kb.ps("psb%d" % i, [128, 512], F32) for i in range(7)]
        self.pst = kb.ps("pst", [128, 1024], BF16)
        self.k_mm = 0

        src = self.xT
        for kind, li in layers:
            if kind == "b":
                self.fox_layer(li, src, self.hT)
            else:
                self.dsa_layer(li, src, self.hT)
            src = self.hT
        if do_final:
            self.norm_phase(src, 4, final=True)
        kb.wait_all_on("sp")
        kb.close()

    def mmps(self):
        self.k_mm += 1
        return self.psb[self.k_mm % 2]

    def load_w_cols(self, wt, w_ap, col0, ncols, nchunk):
        src = w_ap.rearrange("(c p) n -> p c n", p=128)[:, :, col0:col0 + ncols]
        self.kb.dma("pool", wt[:, 0:nchunk, 0:ncols], src, writes=[wt], sem_of=wt, partial=False)

    def norm_phase(self, hsrc, gi, final=False, hnT=None):
        kb, nc, S = self.kb, self.nc, self.S
        kb.push()
        hb = [kb.sb("n_hb%d" % i, [128, 512], F32) for i in range(3)]
        sq = [kb.sb("n_sq%d" % i, [128, 512], F32) for i in range(2)]
        lnv = kb.sb("n_lnv", [128, 512], F32)
        rstd = kb.sb("n_rstd", [128, 512], F32)
        ob = [kb.sb("n_ob%d" % i, [128, 512], F32) for i in range(2)] if final else None
        ones_f = self.consts
        pstat = self.psb[6]
        k = 0
        for tb in range(self.NTB):
            ts = slice(tb * 512, (tb + 1) * 512)
            for c in range(NC_):
                h = hb[k % 3]; s = sq[k % 2]; k += 1
                kb.dma("sp", h[:], hsrc[c * 128:(c + 1) * 128, ts], reads=[hsrc], writes=[h], sem_of=h, partial=False)
                kb.op("act", lambda: nc.scalar.activation(out=s[:], in_=h[:], func=AF.Square), reads=[h], writes=[s])
                kb.op("pe", lambda: nc.tensor.matmul(pstat[:], lhsT=ones_f[:, 3, :], rhs=s[:], start=(c == 0), stop=(c == NC_ - 1)),
                      reads=[s, ones_f], writes=[pstat], partial=(c > 0), inc=True)
            kb.op("act", lambda: nc.scalar.activation(out=lnv[:], in_=pstat[:], func=AF.Ln, scale=1.0 / D, bias=self.epscol[:]),
                  reads=[pstat, self.epscol], writes=[lnv])
            kb.op("act", lambda: nc.scalar.activation(out=rstd[:], in_=lnv[:], func=AF.Exp, scale=-0.5), reads=[lnv], writes=[rstd])
            for c in range(NC_):
                h = hb[k % 3]; k += 1
                kb.dma("sp", h[:], hsrc[c * 128:(c + 1) * 128, ts], reads=[hsrc], writes=[h], sem_of=h, partial=False)
                if final:
                    o = ob[c % 2]
                    kb.op("dve", lambda: nc.vector.scalar_tensor_tensor(out=o[:], in0=h[:], scalar=self.gcols[:, gi, c:c + 1], in1=rstd[:], op0=ALU.mult, op1=ALU.mult),
                          reads=[h, rstd, self.gcols], writes=[o])
                    kb.dma("sp", self.outT[c * 128:(c + 1) * 128, ts], o[:], reads=[o], writes=[self.outT], sem_of=o)
                else:
                    kb.op("dve", lambda: nc.vector.scalar_tensor_tensor(out=hnT[:, c, ts], in0=h[:], scalar=self.gcols[:, gi, c:c + 1], in1=rstd[:], op0=ALU.mult, op1=ALU.mult),
                          reads=[h, rstd, self.gcols], writes=[hnT], partial=True)
        kb.pop()

    def proj_cols(self, hnT, w_ap, col0, ncols, dst, scale=1.0, token_major=False, wbuf=None, obuf=None):
        kb, nc, S = self.kb, self.nc, self.S
        for cc in range(ncols // 128):
            wt = wbuf[cc % 2]
            ob = obuf[cc % 2]
            self.load_w_cols(wt, w_ap, col0 + cc * 128, 128, NC_)
            if not token_major:
                for tb in range(self.NTB):
                    ts = slice(tb * 512, (tb + 1) * 512)
                    ps = self.mmps()
                    for c in range(NC_):
                        kb.op("pe", lambda: nc.tensor.matmul(ps[:], lhsT=wt[:, c, :], rhs=hnT[:, c, ts], start=(c == 0), stop=(c == NC_ - 1)),
                              reads=[wt, hnT], writes=[ps], partial=(c > 0), inc=(c == NC_ - 1))
                    self.evac(ob[:, ts], ps[:], scale, [ps], ob)
                kb.dma("sp", dst[cc * 128:(cc + 1) * 128, :], ob[:], reads=[ob], writes=[dst], sem_of=ob)
            else:
                for tq in range(self.NTK // 4):
                    ps = self.mmps()
                    for q in range(4):
                        tk = tq * 4 + q
                        for c in range(NC_):
                            kb.op("pe", lambda: nc.tensor.matmul(ps[:, q * 128:(q + 1) * 128], lhsT=hnT[:, c, tk * 128:(tk + 1) * 128], rhs=wt[:, c, :],
                                                                 start=(c == 0), stop=(c == NC_ - 1)),
                                  reads=[wt, hnT], writes=[ps], partial=not (c == 0 and q == 0), inc=(c == NC_ - 1 and q == 3))
                    self.evac(ob[:, tq * 512:(tq + 1) * 512], ps[:], scale, [ps], ob)
                dv = dst.t.rearrange("(k p) n -> p k n", p=128)[:, :, cc * 128:(cc + 1) * 128]
                kb.dma("sp", dv, ob[:].rearrange("p (k n) -> p k n", n=128), reads=[ob], writes=[dst], sem_of=ob)

    def evac(self, out_ap, in_ap, scale, reads, wtile, partial=True):
        kb, nc = self.kb, self.nc
        self.k_ev = getattr(self, "k_ev", 0) + 1
        if self.k_ev % 2 == 0:
            kb.op("act", lambda: nc.scalar.activation(out=out_ap, in_=in_ap, func=AF.Copy, scale=float(scale)), reads=reads, writes=[wtile], partial=partial)
        else:
            kb.op("dve", lambda: nc.vector.tensor_scalar(out=out_ap, in0=in_ap, scalar1=float(scale), scalar2=None, op0=ALU.mult), reads=reads, writes=[wtile], partial=partial)

    def wout_block(self, tb, og, wo, hsrc, hdst, hres, psY):
        kb, nc = self.kb, self.nc
        ts = slice(tb * 512, (tb + 1) * 512)
        for dc in range(NC_):
            hr = hres[dc % 2]
            kb.dma("sp", hr[:], hsrc[dc * 128:(dc + 1) * 128, ts], reads=[hsrc], writes=[hr], sem_of=hr, partial=False)
            for h in range(H):
                kb.op("pe", lambda: nc.tensor.matmul(psY[:], lhsT=wo[:, h, dc * 128:(dc + 1) * 128], rhs=og[:, h, :], start=(h == 0), stop=(h == H - 1)),
                      reads=[wo, og], writes=[psY], partial=(h > 0), inc=(h == H - 1))
            kb.op("dve", lambda: nc.vector.tensor_tensor(out=hr[:], in0=psY[:], in1=hr[:], op=ALU.add), reads=[psY, hr], writes=[hr])
            kb.dma("sp", hdst[dc * 128:(dc + 1) * 128, ts], hr[:], reads=[hr], writes=[hdst], sem_of=hr)

    def fox_layer(self, li, hsrc, hdst):
        kb, nc, S = self.kb, self.nc, self.S
        W = self.w[li]
        NTB, NTK = self.NTB, self.NTK
        kb.push()
        csT = kb.sb("csT", [128, NTK, 16], F32)
        csl = kb.sb("csl", [128, NTK, 16], F32)
        kb.push()
        hnT = kb.sb("hnT", [128, NC_, S], BF16)
        self.norm_phase(hsrc, li, hnT=hnT)
        kb.push()
        wbuf = [kb.sb("wbuf%d" % i, [128, NC_, 128], BF16) for i in range(2)]
        obuf = [kb.sb("obuf%d" % i, [128, S], BF16) for i in range(2)]
        self.proj_cols(hnT, W["w_in"], 0, D, self.qT_s, scale=DH ** -0.5, wbuf=wbuf, obuf=obuf)
        self.proj_cols(hnT, W["w_in"], D, D, self.kT_s, wbuf=wbuf, obuf=obuf)
        self.proj_cols(hnT, W["w_in"], 3 * D + H, D, self.gT_s, wbuf=wbuf, obuf=obuf)
        self.proj_cols(hnT, W["w_in"], 2 * D, D, self.v_s, token_major=True, wbuf=wbuf, obuf=obuf)
        kb.pop()
        wf = kb.sb("wf", [128, NC_, 16], BF16)
        self.load_w_cols(wf, W["w_in"], 3 * D, 16, NC_)
        fb = kb.sb("fb", [16, 1], F32)
        kb.dma("sp", fb[:], W["f_bias"], writes=[fb], sem_of=fb, partial=False)
        nfb = kb.sb("nfb", [16, 1], F32)
        kb.op("dve", lambda: nc.vector.tensor_scalar(out=nfb[:], in0=fb[:], scalar1=-1.0, scalar2=None, op0=ALU.mult), reads=[fb], writes=[nfb])
        lf = kb.sb("lf", [16, S], F32)
        cs = kb.sb("cs", [16, S], F32)
        onesr = kb.sb("onesr", [16, S], F32)
        kb.op("dve", lambda: nc.vector.memset(onesr[:], 1.0), writes=[onesr])
        for tb in range(NTB):
            ts = slice(tb * 512, (tb + 1) * 512)
            ps = self.mmps()
            for c in range(NC_):
                kb.op("pe", lambda: nc.tensor.matmul(ps[0:16, :], lhsT=wf[:, c, :], rhs=hnT[:, c, ts], start=(c == 0), stop=(c == NC_ - 1)),
                      reads=[wf, hnT], writes=[ps], partial=(c > 0), inc=(c == NC_ - 1))
            kb.op("act", lambda: nc.scalar.activation(out=lf[:, ts], in_=ps[0:16, :], func=AF.Exp, scale=-1.0, bias=nfb[:]),
                  reads=[ps, nfb], writes=[lf], partial=True)
        one16 = kb.sb("one16", [16, 1], F32)
        kb.op("dve", lambda: nc.vector.memset(one16[:], 1.0), writes=[one16])
        kb.op("act", lambda: nc.scalar.activation(out=lf[:], in_=lf[:], func=AF.Ln, scale=1.0, bias=one16[:]), reads=[lf, one16], writes=[lf])
        kb.op("dve", lambda: nc.vector.tensor_tensor_scan(out=cs[:], data0=onesr[:], data1=lf[:], initial=0.0, op0=ALU.mult, op1=ALU.add),
              reads=[onesr, lf], writes=[cs])
        pT = self.psb[5]
        for tk in range(NTK):
            kb.op("pe", lambda: nc.tensor.transpose(out=pT[:, (tk % 32) * 16:(tk % 32) * 16 + 16], in_=cs[0:16, tk * 128:(tk + 1) * 128], identity=self.consts[0:16, 0, 0:16]),
                  reads=[cs, self.consts], writes=[pT], partial=(tk > 0), inc=(tk == NTK - 1))
        kb.op("dve", lambda: nc.vector.tensor_copy(out=csT[:].rearrange("p k h -> p (k h)"), in_=pT[:, 0:NTK * 16]), reads=[pT], writes=[csT])
        kb.op("pe", lambda: nc.tensor.matmul(pT[:, 0:NTK * 16], lhsT=self.consts[:, 2, :], rhs=csT[:].rearrange("p k h -> p (k h)"), start=True, stop=True),
              reads=[csT, self.consts], writes=[pT])
        kb.op("dve", lambda: nc.vector.tensor_copy(out=csl[:].rearrange("p k h -> p (k h)"), in_=pT[:, 0:NTK * 16]), reads=[pT], writes=[csl])
        kb.pop()

        kb.push()
        wo = kb.sb("wo", [128, H, D], BF16)
        for h4 in range(4):
            src = W["w_out"].rearrange("(c p) n -> p c n", p=128)[:, h4 * 4:(h4 + 1) * 4, :]
            kb.dma("pool", wo[:, h4 * 4:(h4 + 1) * 4, :], src, writes=[wo], sem_of=wo, partial=True)
        kbuf = [kb.sb("kbuf%d" % i, [128, S], BF16) for i in range(2)]
        vbuf = [kb.sb("vbuf%d" % i, [128, NTK, 128], BF16) for i in range(2)]
        qbuf = [kb.sb("qbuf%d" % i, [128, 512], BF16) for i in range(2)]
        gbuf = [kb.sb("gbuf%d" % i, [128, 512], BF16) for i in range(2)]
        pbuf = [kb.sb("pbuf%d" % i, [128, 512], BF16) for i in range(3)]
        bc = [kb.sb("bc%d" % i, [128, NTK], F32) for i in range(2)]
        rd = kb.sb("rd", [128, 512], F32)
        sg = kb.sb("sg", [128, 512], F32)
        og = kb.sb("og", [128, H, 512], BF16)
        hres = [kb.sb("hres%d" % i, [128, 512], F32) for i in range(2)]
        psO = [self.psb[2], self.psb[3]]
        psD = [self.psb[4], self.psb[5]]
        psY = self.psb[6]
        kp = 0
        v_view = self.v_s.t.rearrange("(k p) n -> p k n", p=128)
        for tb in range(NTB):
            ts = slice(tb * 512, (tb + 1) * 512)
            nch = 4 * (tb + 1)
            for h in range(H):
                hs = slice(h * 128, (h + 1) * 128)
                kt = kbuf[h % 2]; vt = vbuf[h % 2]; qt = qbuf[h % 2]; gt = gbuf[h % 2]; b = bc[h % 2]
                kb.dma("sp", kt[:, 0:nch * 128], self.kT_s[hs, 0:nch * 128], reads=[self.kT_s], writes=[kt], sem_of=kt, partial=False)
                kb.dma("sp", vt[:, 0:nch, :], v_view[:, 0:nch, hs], reads=[self.v_s], writes=[vt], sem_of=vt, partial=False)
                kb.dma("sp", qt[:], self.qT_s[hs, ts], reads=[self.qT_s], writes=[qt], sem_of=qt, partial=False)
                kb.dma("sp", gt[:], self.gT_s[hs, ts], reads=[self.gT_s], writes=[gt], sem_of=gt, partial=False)
                kb.op("dve", lambda: nc.vector.tensor_scalar(out=b[:, 0:nch], in0=csT[:, 0:nch, h], scalar1=csl[:, nch - 1, h:h + 1], scalar2=None, op0=ALU.subtract),
                      reads=[csT, csl], writes=[b])
                O = psO[h % 2]; Dn = psD[h % 2]
                for i in range(nch):
                    a = i - 4 * tb
                    c0 = max(a, 0) * 128
                    ps = self.mmps()
                    kb.op("pe", lambda: nc.tensor.matmul(ps[:, c0:512], lhsT=kt[:, i * 128:(i + 1) * 128], rhs=qt[:, c0:512], start=True, stop=(a < 0)),
                          reads=[kt, qt], writes=[ps], inc=(a < 0))
                    if a >= 0:
                        kb.op("pe", lambda: nc.tensor.matmul(ps[:, c0:c0 + 128], lhsT=self.ident_bf[:], rhs=self.tri_bf[:], start=False, stop=True),
                              reads=[self.ident_bf, self.tri_bf], writes=[ps], partial=True)
                    pt = pbuf[kp % 3]; kp += 1
                    kb.op("act", lambda: nc.scalar.activation(out=pt[:, c0:512], in_=ps[:, c0:512], func=AF.Exp, bias=b[:, i:i + 1], scale=1.0),
                          reads=[ps, b], writes=[pt])
                    kb.op("pe", lambda: nc.tensor.matmul(O[:, c0:512], lhsT=vt[:, i, :], rhs=pt[:, c0:512], start=(i == 0), stop=(i == nch - 1)),
                          reads=[vt, pt], writes=[O], partial=(i > 0), inc=False)
                    kb.op("pe", lambda: nc.tensor.matmul(Dn[:, c0:512], lhsT=self.ones_bf[:], rhs=pt[:, c0:512], start=(i == 0), stop=(i == nch - 1)),
                          reads=[self.ones_bf, pt], writes=[Dn], partial=(i > 0), inc=True)
                kb.op("dve", lambda: nc.vector.reciprocal(out=rd[:], in_=Dn[:]), reads=[Dn], writes=[rd])
                kb.op("act", lambda: nc.scalar.activation(out=sg[:], in_=gt[:], func=AF.Silu), reads=[gt], writes=[sg])
                kb.op("dve", lambda: nc.vector.tensor_tensor(out=sg[:], in0=sg[:], in1=rd[:], op=ALU.mult), reads=[sg, rd], writes=[sg])
                kb.op("dve", lambda: nc.vector.tensor_tensor(out=og[:, h, :], in0=O[:], in1=sg[:], op=ALU.mult), reads=[O, sg], writes=[og], partial=True)
            self.wout_block(tb, og, wo, hsrc, hdst, hres, psY)
        kb.pop()
        kb.pop()

    def rms_block(self, src, nchk, gcol, dst, nfeat):
        kb, nc = self.kb, self.nc
        pstat = self.psb[6]
        for c in range(nchk):
            s_ = self.r_sq[c % 2]
            kb.op("act", lambda: nc.scalar.activation(out=s_[:], in_=src[:, c, :], func=AF.Square), reads=[src], writes=[s_])
            kb.op("pe", lambda: nc.tensor.matmul(pstat[:], lhsT=self.consts[:, 3, :], rhs=s_[:], start=(c == 0), stop=(c == nchk - 1)),
                  reads=[s_, self.consts], writes=[pstat], partial=(c > 0), inc=True)
        kb.op("act", lambda: nc.scalar.activation(out=self.r_ln[:], in_=pstat[:], func=AF.Ln, scale=1.0 / nfeat, bias=self.epscol[:]),
              reads=[pstat, self.epscol], writes=[self.r_ln])
        kb.op("act", lambda: nc.scalar.activation(out=self.r_rstd[:], in_=self.r_ln[:], func=AF.Exp, scale=-0.5), reads=[self.r_ln], writes=[self.r_rstd])
        for c in range(nchk):
            d_ap, d_t = dst(c)
            kb.op("dve", lambda: nc.vector.scalar_tensor_tensor(out=d_ap, in0=src[:, c, :], scalar=gcol[:, c:c + 1], in1=self.r_rstd[:], op0=ALU.mult, op1=ALU.mult),
                  reads=[src, gcol, self.r_rstd], writes=[d_t], partial=True)

    def dsa_layer(self, li, hsrc, hdst):
        kb, nc, S = self.kb, self.nc, self.S
        W = self.w[li]
        NTB, NTK = self.NTB, self.NTK
        KIT = 16
        TOPK = min(256, S // 4)
        c_s = self.kT_s
        iq_s = self.qT_s
        kb.push()
        absw = kb.sb("absw", [128, NTK, 16], F32)
        sgn = kb.sb("sgn", [128, NTK, 16], F32)
        ckvnT = kb.sb("ckvnT", [128, 2, S], BF16)
        ckvtok = kb.sb("ckvtok", [128, NTK, 256], BF16)
        kb.push()
        hnT = kb.sb("hnT", [128, NC_, S], BF16)
        self.norm_phase(hsrc, li, hnT=hnT)
        wbuf = [kb.sb("wbuf%d" % i, [128, NC_, 128], BF16) for i in range(2)]
        obuf = [kb.sb("obuf%d" % i, [128, S], BF16) for i in range(2)]
        self.proj_cols(hnT, W["w_in"], QR + KVR + IDXD + IDXH, D, self.gT_s, wbuf=wbuf, obuf=obuf)
        self.proj_cols(hnT, W["w_in"], 0, 896, c_s, wbuf=wbuf, obuf=obuf)
        wiw = kb.sb("wiw", [128, NC_, 16], BF16)
        self.load_w_cols(wiw, W["w_in"], QR + KVR + IDXD, 16, NC_)
        pw = self.psb[5]
        for tk in range(NTK):
            for c in range(NC_):
                kb.op("pe", lambda: nc.tensor.matmul(pw[:, tk * 16:(tk + 1) * 16], lhsT=hnT[:, c, tk * 128:(tk + 1) * 128], rhs=wiw[:, c, :], start=(c == 0), stop=(c == NC_ - 1)),
                      reads=[hnT, wiw], writes=[pw], partial=not (tk == 0 and c == 0), inc=(c == NC_ - 1 and tk == NTK - 1))
        kb.op("act", lambda: nc.scalar.activation(out=absw[:].rearrange("p k h -> p (k h)"), in_=pw[:, 0:NTK * 16], func=AF.Abs, scale=1.0 / 32.0),
              reads=[pw], writes=[absw])
        kb.op("act", lambda: nc.scalar.activation(out=sgn[:].rearrange("p k h -> p (k h)"), in_=pw[:, 0:NTK * 16], func=AF.Sign), reads=[pw], writes=[sgn])
        kb.pop()
        kb.push()
        wq = kb.sb("wq", [128, 4, D], BF16)
        kb.dma("pool", wq[:], W["w_q_up"].rearrange("(c p) n -> p c n", p=128), writes=[wq], sem_of=wq, partial=False)
        wiq = kb.sb("wiq", [128, 4, 1024], BF16)
        kb.dma("pool", wiq[:], W["w_iq"].rearrange("(c p) n -> p c n", p=128), writes=[wiq], sem_of=wiq, partial=False)
        wuk = kb.sb("wuk", [128, H, KVR], BF16)
        kb.dma("pool", wuk[:], W["w_ukT"].rearrange("h d r -> d h r"), writes=[wuk], sem_of=wuk, partial=False)
        qn = kb.sb("qn", [128, 4], F32)
        kvn = kb.sb("kvn", [128, 2], F32)
        kb.dma("sp", qn[:], W["qn"], writes=[qn], sem_of=qn, partial=False)
        kb.dma("sp", kvn[:], W["kvn"], writes=[kvn], sem_of=kvn, partial=False)
        self.r_sq = [kb.sb("r_sq%d" % i, [128, 512], F32) for i in range(2)]
        self.r_ln = kb.sb("r_ln", [128, 512], F32)
        self.r_rstd = kb.sb("r_rstd", [128, 512], F32)
        cin = [kb.sb("cin%d" % i, [128, 6, 512], BF16) for i in range(2)]
        cqn = [kb.sb("cqn%d" % i, [128, 4, 512], BF16) for i in range(2)]
        qh = [kb.sb("qh%d" % i, [128, 512], BF16) for i in range(2)]
        qst = [kb.sb("qst%d" % i, [128, 2, 512], BF16) for i in range(2)]
        ist = [kb.sb("ist%d" % i, [128, 512], BF16) for i in range(2)]
        tst = kb.sb("tst", [128, 1024], BF16)
        qa_view = self.qa_s.t.rearrange("(h r p) t -> p h r t", r=2, p=128)
        c_view = c_s.t.rearrange("(c p) t -> p c t", p=128)
        for tb in range(NTB):
            ts = slice(tb * 512, (tb + 1) * 512)
            ci = cin[tb % 2]; cn = cqn[tb % 2]
            kb.dma("sp", ci[:], c_view[:, 0:6, ts], reads=[c_s], writes=[ci], sem_of=ci, partial=False)
            self.rms_block(ci, 4, qn, lambda c: (cn[:, c, :], cn), QR)
            ckv_src = T(ci.t[:, 4:6, :], "x"); ckv_src.dep = ci.dep
            self.rms_block(ckv_src, 2, kvn, lambda c: (ckvnT[:, c, ts], ckvnT), KVR)
            for q4 in range(4):
                tk = tb * 4 + q4
                for rc in range(2):
                    kb.op("pe", lambda: nc.tensor.transpose(out=self.pst[:, (q4 * 2 + rc) * 128:(q4 * 2 + rc + 1) * 128], in_=ckvnT[:, rc, tk * 128:(tk + 1) * 128], identity=self.ident_bf[:]),
                          reads=[ckvnT, self.ident_bf], writes=[self.pst], partial=not (q4 == 0 and rc == 0), inc=(q4 == 3 and rc == 1))
            kb.op("dve", lambda: nc.vector.tensor_copy(out=ckvtok[:, tb * 4:(tb + 1) * 4, :].rearrange("p k r -> p (k r)"), in_=self.pst[:, :]), reads=[self.pst], writes=[ckvtok], partial=True)
            for h in range(H):
                ps = self.mmps()
                for rc in range(4):
                    kb.op("pe", lambda: nc.tensor.matmul(ps[:], lhsT=wq[:, rc, h * 128:(h + 1) * 128], rhs=cn[:, rc, :], start=(rc == 0), stop=(rc == 3)),
                          reads=[wq, cn], writes=[ps], partial=(rc > 0), inc=(rc == 3))
                qt = qh[h % 2]
                self.evac(qt[:], ps[:], DH ** -0.5, [ps], qt, partial=False)
                st = qst[h % 2]
                for r2 in range(2):
                    ps2 = self.mmps()
                    kb.op("pe", lambda: nc.tensor.matmul(ps2[:], lhsT=wuk[:, h, r2 * 128:(r2 + 1) * 128], rhs=qt[:], start=True, stop=True), reads=[wuk, qt], writes=[ps2])
                    self.evac(st[:, r2, :], ps2[:], 1.0, [ps2], st, partial=(r2 > 0))
                kb.dma("sp", qa_view[:, h, :, ts], st[:], reads=[st], writes=[self.qa_s], sem_of=st)
            for m in range(8):
                ps = self.mmps()
                for rc in range(4):
                    kb.op("pe", lambda: nc.tensor.matmul(ps[:], lhsT=wiq[:, rc, m * 128:(m + 1) * 128], rhs=cn[:, rc, :], start=(rc == 0), stop=(rc == 3)),
                          reads=[wiq, cn], writes=[ps], partial=(rc > 0), inc=(rc == 3))
                it = ist[m % 2]
                self.evac(it[:], ps[:], 1.0, [ps], it, partial=False)
                kb.dma("sp", iq_s[m * 128:(m + 1) * 128, ts], it[:], reads=[it], writes=[iq_s], sem_of=it)
        kb.pop()
        kb.push()
        ikT = kb.sb("ikT", [64, S], BF16)
        kb.dma("sp", ikT[:], c_s[768:832, :], reads=[c_s], writes=[ikT], sem_of=ikT, partial=False)
        iqb = [kb.sb("iqb%d" % i, [64, 16, 128], BF16) for i in range(2)]
        acc = kb.sb("acc", [128, S], F32)
        junk = kb.sb("junk", [128, S], BF16)
        mk = kb.sb("mk", [128, S], BF16)
        rb = [kb.sb("rb%d" % i, [128, 512], F32) for i in range(3)]
        Mx = kb.sb("Mx", [128, 1], F32)
        stepv = kb.sb("stepv", [128, KIT + 1], F32)
        mid = kb.sb("mid", [128, KIT + 1], F32)
        cnt = kb.sb("cnt", [128, KIT + 1], F32)
        uu = kb.sb("uu", [128, 1], F32)
        thr = kb.sb("thr", [128, 1], F32)
        mst = [kb.sb("mst%d" % i, [128, NTK, 128], BF16) for i in range(2)]
        L01 = kb.sb("L01", [128, 128], BF16)
        kb.op("dve", lambda: nc.vector.tensor_copy(out=L01[:], in_=self.consts[:, 4, :]), reads=[self.consts], writes=[L01])
        iq_view = iq_s.t[0:1024, :].rearrange("(h d) t -> d h t", d=64)
        m_view = self.mT_s.t.rearrange("(c p) t -> p c t", p=128)
        kr = 0
        for qb in range(NTK):
            n = (qb + 1) * 128
            tq = slice(qb * 128, (qb + 1) * 128)
            if n > TOPK:
                iqt = iqb[qb % 2]
                kb.dma("sp", iqt[:], iq_view[:, :, tq], reads=[iq_s], writes=[iqt], sem_of=iqt, partial=False)
                for sb_ in range((n + 511) // 512):
                    wd = min(512, n - sb_ * 512)
                    ss = slice(sb_ * 512, sb_ * 512 + wd)
                    for h in range(IDXH):
                        ps = self.mmps()
                        kb.op("pe", lambda: nc.tensor.matmul(ps[:, 0:wd], lhsT=iqt[:, h, :], rhs=ikT[:, ss], start=True, stop=True), reads=[iqt, ikT], writes=[ps])
                        r_ = rb[kr % 3]; kr += 1
                        kb.op("act", lambda: nc.scalar.activation(out=r_[:, 0:wd], in_=ps[:, 0:wd], func=AF.Relu, scale=absw[:, qb, h:h + 1]), reads=[ps, absw], writes=[r_])
                        if h == 0:
                            kb.op("dve", lambda: nc.vector.tensor_scalar(out=acc[:, ss], in0=r_[:, 0:wd], scalar1=sgn[:, qb, 0:1], scalar2=None, op0=ALU.mult), reads=[r_, sgn], writes=[acc])
                        else:
                            kb.op("dve", lambda: nc.vector.scalar_tensor_tensor(out=acc[:, ss], in0=r_[:, 0:wd], scalar=sgn[:, qb, h:h + 1], in1=acc[:, ss], op0=ALU.mult, op1=ALU.add),
                                  reads=[r_, sgn, acc], writes=[acc])
                kb.op("dve", lambda: nc.vector.tensor_reduce(out=Mx[:], in_=acc[:, 0:n], axis=AX.X, op=ALU.max, apply_absolute_value=True), reads=[acc], writes=[Mx])
                kb.op("dve", lambda: nc.vector.tensor_tensor(out=acc[:, tq], in0=acc[:, tq], in1=self.consts[:, 5, :], op=ALU.add), reads=[acc, self.consts], writes=[acc])
                kb.op("dve", lambda: nc.vector.tensor_scalar(out=stepv[:], in0=self.pow2[:], scalar1=Mx[:, 0:1], scalar2=None, op0=ALU.mult), reads=[self.pow2, Mx], writes=[stepv])
                kb.op("dve", lambda: nc.vector.memset(mid[:, 0:1], 0.0), writes=[mid])
                for k in range(KIT):
                    kb.op("dve", lambda: nc.vector.tensor_scalar(out=junk[:, 0:n], in0=acc[:, 0:n], scalar1=mid[:, k:k + 1], scalar2=None, op0=ALU.is_ge, op1=ALU.add, accum_out=cnt[:, k:k + 1]),
                          reads=[acc, mid], writes=[junk, cnt])
                    kb.op("dve", lambda: nc.vector.tensor_scalar(out=uu[:], in0=cnt[:, k:k + 1], scalar1=TOPK - 0.5, scalar2=stepv[:, k:k + 1], op0=ALU.is_ge, op1=ALU.mult),
                          reads=[cnt, stepv], writes=[uu])
                    kb.op("dve", lambda: nc.vector.scalar_tensor_tensor(out=mid[:, k + 1:k + 2], in0=uu[:], scalar=stepv[:, k + 1:k + 2], in1=mid[:, k:k + 1], op0=ALU.subtract, op1=ALU.add),
                          reads=[uu, stepv, mid], writes=[mid])
                kb.op("dve", lambda: nc.vector.tensor_tensor(out=thr[:], in0=mid[:, KIT:KIT + 1], in1=stepv[:, KIT:KIT + 1], op=ALU.subtract), reads=[mid, stepv], writes=[thr])
                kb.op("dve", lambda: nc.vector.tensor_scalar(out=mk[:, 0:n], in0=acc[:, 0:n], scalar1=thr[:, 0:1], scalar2=None, op0=ALU.is_ge), reads=[acc, thr], writes=[mk])
            else:
                if qb > 0:
                    kb.op("dve", lambda: nc.vector.memset(mk[:, 0:qb * 128], 1.0), writes=[mk])
                kb.op("dve", lambda: nc.vector.tensor_copy(out=mk[:, tq], in_=L01[:]), reads=[L01], writes=[mk], partial=(qb > 0))
            ms = mst[qb % 2]
            for c0 in range(0, qb + 1, 8):
                ncb = min(8, qb + 1 - c0)
                for c in range(ncb):
                    kb.op("pe", lambda: nc.tensor.transpose(out=self.pst[:, c * 128:(c + 1) * 128], in_=mk[:, (c0 + c) * 128:(c0 + c + 1) * 128], identity=self.ident_bf[:]),
                          reads=[mk, self.ident_bf], writes=[self.pst], partial=(c > 0), inc=(c == ncb - 1))
                kb.op("act", lambda: nc.scalar.activation(out=ms[:, c0:c0 + ncb, :].rearrange("p c t -> p (c t)"), in_=self.pst[:, 0:ncb * 128], func=AF.Copy), reads=[self.pst], writes=[ms], partial=(c0 > 0))
            kb.dma("sp", m_view[:, 0:qb + 1, tq], ms[:, 0:qb + 1, :], reads=[ms], writes=[self.mT_s], sem_of=ms)
        kb.pop()
        kb.push()
        wo = kb.sb("wo", [128, H, D], BF16)
        for h4 in range(4):
            src = W["w_out"].rearrange("(c p) n -> p c n", p=128)[:, h4 * 4:(h4 + 1) * 4, :]
            kb.dma("pool", wo[:, h4 * 4:(h4 + 1) * 4, :], src, writes=[wo], sem_of=wo, partial=True)
        wuv = kb.sb("wuv", [128, 2, D], BF16)
        kb.dma("pool", wuv[:], W["w_uv"].rearrange("(c p) n -> p c n", p=128), writes=[wuv], sem_of=wuv, partial=False)
        b31 = kb.sb("b31", [128, 16], F32)
        kb.dma("sp", b31[:], W["rb31"], writes=[b31], sem_of=b31, partial=False)
        Dt = kb.sb("Dt", [128, H, 2, 128], BF16)
        dtmp = kb.sb("dtmp", [128, H, 2, 128], F32)
        kb.dma("sp", dtmp[:], W["rbD"], writes=[dtmp], sem_of=dtmp, partial=False)
        for h in range(H):
            kb.op("dve", lambda: nc.vector.tensor_scalar(out=Dt[:, h, :, :], in0=dtmp[:, h, :, :], scalar1=b31[:, h:h + 1], scalar2=None, op0=ALU.subtract),
                  reads=[dtmp, b31], writes=[Dt], partial=(h > 0))
        mT = kb.sb("mT", [128, NTK, 512], BF16)
        qab = [kb.sb("qab%d" % i, [128, 2, 512], BF16) for i in range(2)]
        gbuf = [kb.sb("gbuf%d" % i, [128, 512], BF16) for i in range(2)]
        pbuf = [kb.sb("pbuf%d" % i, [128, 512], BF16) for i in range(3)]
        ol = kb.sb("ol", [128, 2, 512], BF16)
        rd = kb.sb("rd", [128, 512], F32)
        sg = kb.sb("sg", [128, 512], F32)
        og = kb.sb("og", [128, H, 512], BF16)
        hres = [kb.sb("hres%d" % i, [128, 512], F32) for i in range(2)]
        O0, O1, Dn, psU, psY = self.psb[2], self.psb[3], self.psb[4], self.psb[5], self.psb[6]
        kp = 0
        for tb in range(NTB):
            ts = slice(tb * 512, (tb + 1) * 512)
            nch = 4 * (tb + 1)
            kb.dma("sp", mT[:, 0:nch, :], m_view[:, 0:nch, ts], reads=[self.mT_s], writes=[mT], sem_of=mT, partial=False)
            for h in range(H):
                hs = slice(h * 128, (h + 1) * 128)
                qa = qab[h % 2]; gt = gbuf[h % 2]
                kb.dma("sp", qa[:], qa_view[:, h, :, ts], reads=[self.qa_s], writes=[qa], sem_of=qa, partial=False)
                kb.dma("sp", gt[:], self.gT_s[hs, ts], reads=[self.gT_s], writes=[gt], sem_of=gt, partial=False)
                for i in range(nch):
                    a = i - 4 * tb
                    c0 = max(a, 0) * 128
                    extra = [(c, 4 * tb + c - i) for c in range(4) if (4 * tb + c - i) in (0, 1) and c * 128 >= c0]
                    ps = self.mmps()
                    kb.op("pe", lambda: nc.tensor.matmul(ps[:, c0:512], lhsT=ckvnT[:, 0, i * 128:(i + 1) * 128], rhs=qa[:, 0, c0:512], start=True, stop=False),
                          reads=[ckvnT, qa], writes=[ps], inc=False)
                    kb.op("pe", lambda: nc.tensor.matmul(ps[:, c0:512], lhsT=ckvnT[:, 1, i * 128:(i + 1) * 128], rhs=qa[:, 1, c0:512], start=False, stop=(len(extra) == 0)),
                          reads=[ckvnT, qa], writes=[ps], partial=True, inc=(len(extra) == 0))
                    for ei, (c, df) in enumerate(extra):
                        kb.op("pe", lambda: nc.tensor.matmul(ps[:, c * 128:(c + 1) * 128], lhsT=self.ident_bf[:], rhs=Dt[:, h, df, :], start=False, stop=(ei == len(extra) - 1)),
                              reads=[self.ident_bf, Dt], writes=[ps], partial=True, inc=(ei == len(extra) - 1))
                    pt = pbuf[kp % 3]; kp += 1
                    kb.op("act", lambda: nc.scalar.activation(out=pt[:, c0:512], in_=ps[:, c0:512], func=AF.Exp, bias=b31[:, h:h + 1], scale=1.0),
                          reads=[ps, b31], writes=[pt])
                    kb.op("pool", lambda: nc.gpsimd.tensor_tensor(out=pt[:, c0:512], in0=pt[:, c0:512], in1=mT[:, i, c0:512], op=ALU.mult), reads=[pt, mT], writes=[pt])
                    kb.op("pe", lambda: nc.tensor.matmul(O0[:, c0:512], lhsT=ckvtok[:, i, 0:128], rhs=pt[:, c0:512], start=(i == 0), stop=(i == nch - 1)),
                          reads=[ckvtok, pt], writes=[O0], partial=(i > 0), inc=False)
                    kb.op("pe", lambda: nc.tensor.matmul(O1[:, c0:512], lhsT=ckvtok[:, i, 128:256], rhs=pt[:, c0:512], start=(i == 0), stop=(i == nch - 1)),
                          reads=[ckvtok, pt], writes=[O1], partial=(i > 0), inc=False)
                    kb.op("pe", lambda: nc.tensor.matmul(Dn[:, c0:512], lhsT=self.ones_bf[:], rhs=pt[:, c0:512], start=(i == 0), stop=(i == nch - 1)),
                          reads=[self.ones_bf, pt], writes=[Dn], partial=(i > 0), inc=True)
                kb.op("act", lambda: nc.scalar.activation(out=ol[:, 0, :], in_=O0[:], func=AF.Copy), reads=[O0], writes=[ol])
                kb.op("dve", lambda: nc.vector.tensor_copy(out=ol[:, 1, :], in_=O1[:]), reads=[O1], writes=[ol], partial=True)
                kb.op("pe", lambda: nc.tensor.matmul(psU[:], lhsT=wuv[:, 0, hs], rhs=ol[:, 0, :], start=True, stop=False), reads=[wuv, ol], writes=[psU], inc=False)
                kb.op("pe", lambda: nc.tensor.matmul(psU[:], lhsT=wuv[:, 1, hs], rhs=ol[:, 1, :], start=False, stop=True), reads=[wuv, ol], writes=[psU], partial=True)
                kb.op("dve", lambda: nc.vector.reciprocal(out=rd[:], in_=Dn[:]), reads=[Dn], writes=[rd])
                kb.op("act", lambda: nc.scalar.activation(out=sg[:], in_=gt[:], func=AF.Silu), reads=[gt], writes=[sg])
                kb.op("dve", lambda: nc.vector.tensor_tensor(out=sg[:], in0=sg[:], in1=rd[:], op=ALU.mult), reads=[sg, rd], writes=[sg])
                kb.op("dve", lambda: nc.vector.tensor_tensor(out=og[:, h, :], in0=psU[:], in1=sg[:], op=ALU.mult), reads=[psU, sg], writes=[og], partial=True)
            self.wout_block(tb, og, wo, hsrc, hdst, hres, psY)
        kb.pop()
        kb.pop()


def make_consts():
    c = np.zeros((128, 6, 128), np.float32)
    pp = np.arange(128)[:, None]; jj = np.arange(128)[None, :]
    c[:, 4, :] = np.where(jj <= pp, 1.0, 0.0)
    c[:, 5, :] = np.where(jj <= pp, 0.0, -1e30)
    c[:, 0, :] = np.eye(128, dtype=np.float32)
    sp = np.arange(128)[:, None]; tp = np.arange(128)[None, :]
    c[:, 1, :] = np.where(sp <= tp, 0.0, NEG)
    c[127, 2, :] = 1.0
    c[:, 3, :] = 1.0
    return c


def cols16(v):
    return np.ascontiguousarray(v.reshape(16, 128).T)


def t5_bucket_np(dist):
    import math
    max_exact = 16
    d = np.maximum(dist, 0)
    df = np.maximum(d, 1).astype(np.float32)
    large = max_exact + (np.log(df / max_exact) / math.log(128 / max_exact) * (32 - max_exact)).astype(np.int32)
    large = np.minimum(large, 31)
    return np.where(d < max_exact, d, large)


def shared_inputs(p, layers, do_final=True):
    im = {"consts": make_consts()}
    g = np.zeros((128, 5, 16), np.float32)
    for i in range(4):
        g[:, i, :] = cols16(p["norm_g"][i])
    g[:, 4, :] = cols16(p["final_g"])
    im["gcols"] = g
    im["pow2"] = np.ascontiguousarray(np.broadcast_to((2.0 ** -np.arange(17, dtype=np.float64)).astype(np.float32), (128, 17)))
    sp = np.arange(128)[:, None]; tp = np.arange(128)[None, :]
    bk = np.stack([t5_bucket_np(tp - sp), t5_bucket_np(128 + tp - sp)], 0)
    for kind, li in layers:
        j = li // 2
        if kind == "b":
            im["w_in%d" % li] = np.ascontiguousarray(p["b_w_in"][j])
            im["f_bias%d" % li] = np.ascontiguousarray(p["b_f_bias"][j].reshape(16, 1))
            im["w_out%d" % li] = np.ascontiguousarray(p["b_w_out"][j])
        else:
            im["w_in%d" % li] = np.ascontiguousarray(p["a_w_in"][j])
            im["qn%d" % li] = np.ascontiguousarray(p["a_q_norm"][j].reshape(4, 128).T)
            im["kvn%d" % li] = np.ascontiguousarray(p["a_kv_norm"][j].reshape(2, 128).T)
            im["w_q_up%d" % li] = np.ascontiguousarray(p["a_w_q_up"][j])
            im["w_ukT%d" % li] = np.ascontiguousarray(np.transpose(p["a_w_uk"][j], (1, 2, 0)))
            im["w_uv%d" % li] = np.ascontiguousarray(p["a_w_uv"][j].reshape(KVR, D))
            im["w_iq%d" % li] = np.ascontiguousarray(p["a_w_iq"][j])
            im["w_out%d" % li] = np.ascontiguousarray(p["a_w_out"][j])
            rb = p["rel_bias"]
            im["rb31_%d" % li] = np.ascontiguousarray(np.broadcast_to(rb[31][None, :], (128, 16)))
            gath = rb[bk]
            im["rbD_%d" % li] = np.ascontiguousarray(np.transpose(gath, (1, 3, 0, 2)))
    return im


_PROG_CACHE = {}


def get_prog(S, layers, do_final):
    key = (S, tuple(layers), do_final)
    if key not in _PROG_CACHE:
        _PROG_CACHE[key] = Prog(S, list(layers), do_final=do_final)
    return _PROG_CACHE[key]


LAYERS = [("a", 0), ("b", 1), ("a", 2), ("b", 3)]
FUSED = True


def run_layers(xT_list, p, layers, do_final):
    S = xT_list[0].shape[1]
    prog = get_prog(S, layers, do_final)
    sh = shared_inputs(p, layers, do_final)
    in_maps = []
    for xT in xT_list:
        m = dict(sh)
        m["xT"] = xT
        in_maps.append(m)
    res = run_bass_kernel_spmd(prog.nc, in_maps, core_ids=list(range(len(xT_list))))
    key = "outT" if do_final else "hT"
    return [r[key] for r in res.results]


def kernel(**inputs):
    p = {k: np.asarray(v) for k, v in inputs.items()}
    x = p["x"]
    B = x.shape[0]
    xT = [np.ascontiguousarray(x[b].T) for b in range(B)]
    if FUSED:
        outT = run_layers(xT, p, LAYERS, True)
    else:
        cur = xT
        for n, lay in enumerate(LAYERS):
            cur = run_layers(cur, p, [lay], n == len(LAYERS) - 1)
        outT = cur
    return np.stack([np.ascontiguousarray(o.T) for o in outT], 0).astype(np.float32)
```

```python
import numpy as np
import concourse.bass as bass
import concourse.mybir as mybir
from concourse.bass_utils import run_bass_kernel_spmd

F32 = mybir.dt.float32
BF16 = mybir.dt.bfloat16
AF = mybir.ActivationFunctionType
ALU = mybir.AluOpType
AX = mybir.AxisListType

D = 2048
H = 16
DH = 128
NC_ = 16
QR = 512
KVR = 256
IDXH = 16
IDXD = 64
A_IN = QR + KVR + IDXD + IDXH + D
B_IN = 3 * D + H + D
EPS = 1e-6
NEG = -30000.0


class Dep:
    __slots__ = ("name", "w", "r", "dsem", "dcnt")

    def __init__(self, name):
        self.name = name
        self.w = {}
        self.r = {}
        self.dsem = None


class T:
    def __init__(self, t, name):
        self.t = t
        self.dep = Dep(name)

    def __getitem__(self, idx):
        return self.t[idx]


def _dep(d):
    return d.dep if isinstance(d, T) else d


class KB:
    NDMA = 48

    def __init__(self, nc):
        self.nc = nc
        self.engs = {"pe": nc.tensor, "act": nc.scalar, "dve": nc.vector,
                     "pool": nc.gpsimd, "sp": nc.sync}
        self._stack = [[]]
        self.semobj = {}
        self.semval = {}
        self.sems = {}
        for e in self.engs:
            key = "e_" + e
            self.semobj[key] = self._enter(nc.semaphore("s_" + e))
            self.semval[key] = 0
            self.sems[e] = key
        self.dfree = []
        for i in range(self.NDMA):
            key = "d_%d" % i
            self.semobj[key] = self._enter(nc.semaphore("sd_%d" % i))
            self.semval[key] = 0
            self.dfree.append(key)
        self.seen = {e: {} for e in self.engs}
        self.scope_deps = [[]]

    def _enter(self, cm):
        obj = cm.__enter__()
        self._stack[-1].append(cm)
        return obj

    def push(self):
        self._stack.append([])
        self.scope_deps.append([])

    def pop(self):
        self.barrier()
        for d in self.scope_deps.pop():
            if d.dsem is not None:
                self.dfree.append(d.dsem)
                d.dsem = None
        for cm in reversed(self._stack.pop()):
            cm.__exit__(None, None, None)

    def close(self):
        while len(self._stack) > 1:
            self.pop()
        for cm in reversed(self._stack[0]):
            cm.__exit__(None, None, None)

    def sb(self, name, shape, dt):
        self.uid = getattr(self, "uid", 0) + 1
        name = "%s_u%d" % (name, self.uid)
        t = T(self._enter(self.nc.sbuf_tensor(name, list(shape), dt)), name)
        self.scope_deps[-1].append(t.dep)
        return t

    def ps(self, name, shape, dt):
        t = T(self._enter(self.nc.psum_tensor(name, list(shape), dt)), name)
        self.scope_deps[-1].append(t.dep)
        return t

    def region(self, name):
        return Dep(name)

    def _wait(self, eng, key, val):
        if self.seen[eng].get(key, 0) >= val:
            return
        self.engs[eng].wait_ge(self.semobj[key], val)
        self.seen[eng][key] = val

    def _deps(self, eng, reads, writes, partial):
        own = self.sems[eng]
        skip_own = (eng == "pe")
        for d in reads:
            d = _dep(d)
            for k, (v, _p) in d.w.items():
                if skip_own and k == own:
                    continue
                self._wait(eng, k, v)
        for d in writes:
            d = _dep(d)
            for k, v in d.r.items():
                if skip_own and k == own:
                    continue
                self._wait(eng, k, v)
            for k, (v, p) in d.w.items():
                if partial and p:
                    continue
                if skip_own and k == own:
                    continue
                self._wait(eng, k, v)

    def _record(self, key, val, reads, writes, partial):
        for d in reads:
            d = _dep(d)
            if d.r.get(key, 0) < val:
                d.r[key] = val
        for d in writes:
            d = _dep(d)
            if partial and not d.r and all(p for (_v, p) in d.w.values()):
                d.w[key] = (val, True)
            else:
                d.w = {key: (val, partial)}
                d.r = {}

    def op(self, eng, fn, reads=(), writes=(), partial=False, inc=True):
        self._deps(eng, reads, writes, partial)
        ins = fn()
        key = self.sems[eng]
        if inc:
            self.semval[key] += 1
            ins.then_inc(self.semobj[key], 1)
            val = self.semval[key]
        else:
            val = self.semval[key] + 1
        self._record(key, val, reads, writes, partial)
        return ins

    def dma(self, q, out, in_, reads=(), writes=(), sem_of=None, partial=True, **kw):
        self._deps(q, reads, writes, partial)
        d = _dep(sem_of)
        if d.dsem is None:
            d.dsem = self.dfree.pop()
        key = d.dsem
        self.semval[key] += 16
        ins = self.engs[q].dma_start(out=out, in_=in_, **kw)
        ins.then_inc(self.semobj[key], 16)
        self._record(key, self.semval[key], reads, writes, partial)
        return ins

    def barrier(self):
        for e in self.engs:
            for key, v in self.semval.items():
                if v > 0:
                    self._wait(e, key, v)

    def wait_all_on(self, eng):
        for key, v in self.semval.items():
            if v > 0:
                self._wait(eng, key, v)


class Prog:
    def __init__(self, S, layers, first_src_is_x=True, do_final=True):
        self.S = S
        self.NTB = S // 512
        self.NTK = S // 128
        self.layers = layers
        self.do_final = do_final
        nc = self.nc = bass.Bass("TRN2", target_bir_lowering=False)
        self.kb = KB(nc)
        kb = self.kb
        dt = nc.dram_tensor
        self.xT = T(dt("xT", [D, S], F32, kind="ExternalInput").ap(), "xT")
        if do_final:
            self.outT = T(dt("outT", [D, S], F32, kind="ExternalOutput").ap(), "outT")
        self.hT = T(dt("hT", [D, S], F32, kind="Internal" if do_final else "ExternalOutput").ap(), "hT")
        self.consts_d = dt("consts", [128, 6, 128], F32, kind="ExternalInput").ap()
        self.gcols_d = dt("gcols", [128, 5, 16], F32, kind="ExternalInput").ap()
        self.w = {}
        for kind, li in layers:
            if kind == "b":
                self.w[li] = dict(
                    w_in=dt("w_in%d" % li, [D, B_IN], F32, kind="ExternalInput").ap(),
                    f_bias=dt("f_bias%d" % li, [16, 1], F32, kind="ExternalInput").ap(),
                    w_out=dt("w_out%d" % li, [D, D], F32, kind="ExternalInput").ap(),
                )
            else:
                self.w[li] = dict(
                    w_in=dt("w_in%d" % li, [D, A_IN], F32, kind="ExternalInput").ap(),
                    qn=dt("qn%d" % li, [128, 4], F32, kind="ExternalInput").ap(),
                    kvn=dt("kvn%d" % li, [128, 2], F32, kind="ExternalInput").ap(),
                    w_q_up=dt("w_q_up%d" % li, [QR, D], F32, kind="ExternalInput").ap(),
                    w_ukT=dt("w_ukT%d" % li, [H, DH, KVR], F32, kind="ExternalInput").ap(),
                    w_uv=dt("w_uv%d" % li, [KVR, D], F32, kind="ExternalInput").ap(),
                    w_iq=dt("w_iq%d" % li, [QR, IDXH * IDXD], F32, kind="ExternalInput").ap(),
                    w_out=dt("w_out%d" % li, [D, D], F32, kind="ExternalInput").ap(),
                    rb31=dt("rb31_%d" % li, [128, 16], F32, kind="ExternalInput").ap(),
                    rbD=dt("rbD_%d" % li, [128, 16, 2, 128], F32, kind="ExternalInput").ap(),
                )
        self.qT_s = T(dt("qT_s", [D, S], BF16, kind="Internal").ap(), "qT_s")
        self.kT_s = T(dt("kT_s", [D, S], BF16, kind="Internal").ap(), "kT_s")
        self.v_s = T(dt("v_s", [S, D], BF16, kind="Internal").ap(), "v_s")
        self.gT_s = T(dt("gT_s", [D, S], BF16, kind="Internal").ap(), "gT_s")

        self.consts = kb.sb("consts_sb", [128, 6, 128], F32)
        self.gcols = kb.sb("gcols_sb", [128, 5, 16], F32)
        kb.dma("sp", self.consts[:], self.consts_d, writes=[self.consts], sem_of=self.consts)
        kb.dma("sp", self.gcols[:], self.gcols_d, writes=[self.gcols], sem_of=self.gcols)
        self.ident_bf = kb.sb("ident_bf", [128, 128], BF16)
        self.tri_bf = kb.sb("tri_bf", [128, 128], BF16)
        self.ones_bf = kb.sb("ones_bf", [128, 128], BF16)
        for dst, ci in ((self.ident_bf, 0), (self.tri_bf, 1), (self.ones_bf, 3)):
            kb.op("dve", lambda: nc.vector.tensor_copy(out=dst[:], in_=self.consts[:, ci, :]),
                  reads=[self.consts], writes=[dst])
        self.epscol = kb.sb("epscol", [128, 1], F32)
        kb.op("dve", lambda: nc.vector.memset(self.epscol[:], EPS), writes=[self.epscol])
        self.pow2_d = dt("pow2", [128, 17], F32, kind="ExternalInput").ap()
        self.pow2 = kb.sb("pow2_sb", [128, 17], F32)
        kb.dma("sp", self.pow2[:], self.pow2_d, writes=[self.pow2], sem_of=self.pow2)
        self.qa_s = T(dt("qa_s", [2 * D, S], BF16, kind="Internal").ap(), "qa_s")
        self.mT_s = T(dt("mT_s", [S, S], BF16, kind="Internal").ap(), "mT_s")
        self.psb = [kb.ps("psb%d" % i, [128, 512], F32) for i in range(7)]
        self.pst = kb.ps("pst", [128, 1024], BF16)
        self.k_mm = 0

        src = self.xT
        for kind, li in layers:
            if kind == "b":
                self.fox_layer(li, src, self.hT)
            else:
                self.dsa_layer(li, src, self.hT)
            src = self.hT
        if do_final:
            self.norm_phase(src, 4, final=True)
        kb.wait_all_on("sp")
        kb.close()

    def mmps(self):
        self.k_mm += 1
        return self.psb[self.k_mm % 2]

    def load_w_cols(self, wt, w_ap, col0, ncols, nchunk):
        src = w_ap.rearrange("(c p) n -> p c n", p=128)[:, :, col0:col0 + ncols]
        self.kb.dma("pool", wt[:, 0:nchunk, 0:ncols], src, writes=[wt], sem_of=wt, partial=False)

    def norm_phase(self, hsrc, gi, final=False, hnT=None):
        kb, nc, S = self.kb, self.nc, self.S
        kb.push()
        hb = [kb.sb("n_hb%d" % i, [128, 512], F32) for i in range(3)]
        sq = [kb.sb("n_sq%d" % i, [128, 512], F32) for i in range(2)]
        lnv = kb.sb("n_lnv", [128, 512], F32)
        rstd = kb.sb("n_rstd", [128, 512], F32)
        ob = [kb.sb("n_ob%d" % i, [128, 512], F32) for i in range(2)] if final else None
        ones_f = self.consts
        pstat = self.psb[6]
        k = 0
        for tb in range(self.NTB):
            ts = slice(tb * 512, (tb + 1) * 512)
            for c in range(NC_):
                h = hb[k % 3]; s = sq[k % 2]; k += 1
                kb.dma("sp", h[:], hsrc[c * 128:(c + 1) * 128, ts], reads=[hsrc], writes=[h], sem_of=h, partial=False)
                kb.op("act", lambda: nc.scalar.activation(out=s[:], in_=h[:], func=AF.Square), reads=[h], writes=[s])
                kb.op("pe", lambda: nc.tensor.matmul(pstat[:], lhsT=ones_f[:, 3, :], rhs=s[:], start=(c == 0), stop=(c == NC_ - 1)),
                      reads=[s, ones_f], writes=[pstat], partial=(c > 0), inc=True)
            kb.op("act", lambda: nc.scalar.activation(out=lnv[:], in_=pstat[:], func=AF.Ln, scale=1.0 / D, bias=self.epscol[:]),
                  reads=[pstat, self.epscol], writes=[lnv])
            kb.op("act", lambda: nc.scalar.activation(out=rstd[:], in_=lnv[:], func=AF.Exp, scale=-0.5), reads=[lnv], writes=[rstd])
            for c in range(NC_):
                h = hb[k % 3]; k += 1
                kb.dma("sp", h[:], hsrc[c * 128:(c + 1) * 128, ts], reads=[hsrc], writes=[h], sem_of=h, partial=False)
                if final:
                    o = ob[c % 2]
                    kb.op("dve", lambda: nc.vector.scalar_tensor_tensor(out=o[:], in0=h[:], scalar=self.gcols[:, gi, c:c + 1], in1=rstd[:], op0=ALU.mult, op1=ALU.mult),
                          reads=[h, rstd, self.gcols], writes=[o])
                    kb.dma("sp", self.outT[c * 128:(c + 1) * 128, ts], o[:], reads=[o], writes=[self.outT], sem_of=o)
                else:
                    kb.op("dve", lambda: nc.vector.scalar_tensor_tensor(out=hnT[:, c, ts], in0=h[:], scalar=self.gcols[:, gi, c:c + 1], in1=rstd[:], op0=ALU.mult, op1=ALU.mult),
                          reads=[h, rstd, self.gcols], writes=[hnT], partial=True)
        kb.pop()

    def proj_cols(self, hnT, w_ap, col0, ncols, dst, scale=1.0, token_major=False, wbuf=None, obuf=None):
        kb, nc, S = self.kb, self.nc, self.S
        for cc in range(ncols // 128):
            wt = wbuf[cc % 2]
            ob = obuf[cc % 2]
            self.load_w_cols(wt, w_ap, col0 + cc * 128, 128, NC_)
            if not token_major:
                for tb in range(self.NTB):
                    ts = slice(tb * 512, (tb + 1) * 512)
                    ps = self.mmps()
                    for c in range(NC_):
                        kb.op("pe", lambda: nc.tensor.matmul(ps[:], lhsT=wt[:, c, :], rhs=hnT[:, c, ts], start=(c == 0), stop=(c == NC_ - 1)),
                              reads=[wt, hnT], writes=[ps], partial=(c > 0), inc=(c == NC_ - 1))
                    self.evac(ob[:, ts], ps[:], scale, [ps], ob)
                kb.dma("sp", dst[cc * 128:(cc + 1) * 128, :], ob[:], reads=[ob], writes=[dst], sem_of=ob)
            else:
                for tq in range(self.NTK // 4):
                    ps = self.mmps()
                    for q in range(4):
                        tk = tq * 4 + q
                        for c in range(NC_):
                            kb.op("pe", lambda: nc.tensor.matmul(ps[:, q * 128:(q + 1) * 128], lhsT=hnT[:, c, tk * 128:(tk + 1) * 128], rhs=wt[:, c, :],
                                                                 start=(c == 0), stop=(c == NC_ - 1)),
                                  reads=[wt, hnT], writes=[ps], partial=not (c == 0 and q == 0), inc=(c == NC_ - 1 and q == 3))
                    self.evac(ob[:, tq * 512:(tq + 1) * 512], ps[:], scale, [ps], ob)
                dv = dst.t.rearrange("(k p) n -> p k n", p=128)[:, :, cc * 128:(cc + 1) * 128]
                kb.dma("sp", dv, ob[:].rearrange("p (k n) -> p k n", n=128), reads=[ob], writes=[dst], sem_of=ob)

    def evac(self, out_ap, in_ap, scale, reads, wtile, partial=True):
        kb, nc = self.kb, self.nc
        self.k_ev = getattr(self, "k_ev", 0) + 1
        if self.k_ev % 2 == 0:
            kb.op("act", lambda: nc.scalar.activation(out=out_ap, in_=in_ap, func=AF.Copy, scale=float(scale)), reads=reads, writes=[wtile], partial=partial)
        else:
            kb.op("dve", lambda: nc.vector.tensor_scalar(out=out_ap, in0=in_ap, scalar1=float(scale), scalar2=None, op0=ALU.mult), reads=reads, writes=[wtile], partial=partial)

    def wout_block(self, tb, og, wo, hsrc, hdst, hres, psY):
        kb, nc = self.kb, self.nc
        ts = slice(tb * 512, (tb + 1) * 512)
        for dc in range(NC_):
            hr = hres[dc % 2]
            kb.dma("sp", hr[:], hsrc[dc * 128:(dc + 1) * 128, ts], reads=[hsrc], writes=[hr], sem_of=hr, partial=False)
            for h in range(H):
                kb.op("pe", lambda: nc.tensor.matmul(psY[:], lhsT=wo[:, h, dc * 128:(dc + 1) * 128], rhs=og[:, h, :], start=(h == 0), stop=(h == H - 1)),
                      reads=[wo, og], writes=[psY], partial=(h > 0), inc=(h == H - 1))
            kb.op("dve", lambda: nc.vector.tensor_tensor(out=hr[:], in0=psY[:], in1=hr[:], op=ALU.add), reads=[psY, hr], writes=[hr])
            kb.dma("sp", hdst[dc * 128:(dc + 1) * 128, ts], hr[:], reads=[hr], writes=[hdst], sem_of=hr)

    def fox_layer(self, li, hsrc, hdst):
        kb, nc, S = self.kb, self.nc, self.S
        W = self.w[li]
        NTB, NTK = self.NTB, self.NTK
        kb.push()
        csT = kb.sb("csT", [128, NTK, 16], F32)
        csl = kb.sb("csl", [128, NTK, 16], F32)
        kb.push()
        hnT = kb.sb("hnT", [128, NC_, S], BF16)
        self.norm_phase(hsrc, li, hnT=hnT)
        kb.push()
        wbuf = [kb.sb("wbuf%d" % i, [128, NC_, 128], BF16) for i in range(2)]
        obuf = [kb.sb("obuf%d" % i, [128, S], BF16) for i in range(2)]
        self.proj_cols(hnT, W["w_in"], 0, D, self.qT_s, scale=DH ** -0.5, wbuf=wbuf, obuf=obuf)
        self.proj_cols(hnT, W["w_in"], D, D, self.kT_s, wbuf=wbuf, obuf=obuf)
        self.proj_cols(hnT, W["w_in"], 3 * D + H, D, self.gT_s, wbuf=wbuf, obuf=obuf)
        self.proj_cols(hnT, W["w_in"], 2 * D, D, self.v_s, token_major=True, wbuf=wbuf, obuf=obuf)
        kb.pop()
        wf = kb.sb("wf", [128, NC_, 16], BF16)
        self.load_w_cols(wf, W["w_in"], 3 * D, 16, NC_)
        fb = kb.sb("fb", [16, 1], F32)
        kb.dma("sp", fb[:], W["f_bias"], writes=[fb], sem_of=fb, partial=False)
        nfb = kb.sb("nfb", [16, 1], F32)
        kb.op("dve", lambda: nc.vector.tensor_scalar(out=nfb[:], in0=fb[:], scalar1=-1.0, scalar2=None, op0=ALU.mult), reads=[fb], writes=[nfb])
        lf = kb.sb("lf", [16, S], F32)
        cs = kb.sb("cs", [16, S], F32)
        onesr = kb.sb("onesr", [16, S], F32)
        kb.op("dve", lambda: nc.vector.memset(onesr[:], 1.0), writes=[onesr])
        for tb in range(NTB):
            ts = slice(tb * 512, (tb + 1) * 512)
            ps = self.mmps()
            for c in range(NC_):
                kb.op("pe", lambda: nc.tensor.matmul(ps[0:16, :], lhsT=wf[:, c, :], rhs=hnT[:, c, ts], start=(c == 0), stop=(c == NC_ - 1)),
                      reads=[wf, hnT], writes=[ps], partial=(c > 0), inc=(c == NC_ - 1))
            kb.op("act", lambda: nc.scalar.activation(out=lf[:, ts], in_=ps[0:16, :], func=AF.Exp, scale=-1.0, bias=nfb[:]),
                  reads=[ps, nfb], writes=[lf], partial=True)
        one16 = kb.sb("one16", [16, 1], F32)
        kb.op("dve", lambda: nc.vector.memset(one16[:], 1.0), writes=[one16])
        kb.op("act", lambda: nc.scalar.activation(out=lf[:], in_=lf[:], func=AF.Ln, scale=1.0, bias=one16[:]), reads=[lf, one16], writes=[lf])
        kb.op("dve", lambda: nc.vector.tensor_tensor_scan(out=cs[:], data0=onesr[:], data1=lf[:], initial=0.0, op0=ALU.mult, op1=ALU.add),
              reads=[onesr, lf], writes=[cs])
        pT = self.psb[5]
        for tk in range(NTK):
            kb.op("pe", lambda: nc.tensor.transpose(out=pT[:, (tk % 32) * 16:(tk % 32) * 16 + 16], in_=cs[0:16, tk * 128:(tk + 1) * 128], identity=self.consts[0:16, 0, 0:16]),
                  reads=[cs, self.consts], writes=[pT], partial=(tk > 0), inc=(tk == NTK - 1))
        kb.op("dve", lambda: nc.vector.tensor_copy(out=csT[:].rearrange("p k h -> p (k h)"), in_=pT[:, 0:NTK * 16]), reads=[pT], writes=[csT])
        kb.op("pe", lambda: nc.tensor.matmul(pT[:, 0:NTK * 16], lhsT=self.consts[:, 2, :], rhs=csT[:].rearrange("p k h -> p (k h)"), start=True, stop=True),
              reads=[csT, self.consts], writes=[pT])
        kb.op("dve", lambda: nc.vector.tensor_copy(out=csl[:].rearrange("p k h -> p (k h)"), in_=pT[:, 0:NTK * 16]), reads=[pT], writes=[csl])
        kb.pop()

        kb.push()
        wo = kb.sb("wo", [128, H, D], BF16)
        for h4 in range(4):
            src = W["w_out"].rearrange("(c p) n -> p c n", p=128)[:, h4 * 4:(h4 + 1) * 4, :]
            kb.dma("pool", wo[:, h4 * 4:(h4 + 1) * 4, :], src, writes=[wo], sem_of=wo, partial=True)
        kbuf = [kb.sb("kbuf%d" % i, [128, S], BF16) for i in range(2)]
        vbuf = [kb.sb("vbuf%d" % i, [128, NTK, 128], BF16) for i in range(2)]
        qbuf = [kb.sb("qbuf%d" % i, [128, 512], BF16) for i in range(2)]
        gbuf = [kb.sb("gbuf%d" % i, [128, 512], BF16) for i in range(2)]
        pbuf = [kb.sb("pbuf%d" % i, [128, 512], BF16) for i in range(4)]
        bc = [kb.sb("bc%d" % i, [128, NTK], F32) for i in range(2)]
        rd = kb.sb("rd", [128, 512], F32)
        sg = kb.sb("sg", [128, 512], F32)
        og = kb.sb("og", [128, H, 512], BF16)
        hres = [kb.sb("hres%d" % i, [128, 512], F32) for i in range(2)]
        psO = [self.psb[2], self.psb[3]]
        psD = [self.psb[4], self.psb[5]]
        psY = self.psb[6]
        kp = 0
        v_view = self.v_s.t.rearrange("(k p) n -> p k n", p=128)
        prev = None

        def fox_pv(h, i, nch, c0, pt, vt, O, Dn, gt):
            kb.op("pe", lambda: nc.tensor.matmul(O[:, c0:512], lhsT=vt[:, i, :], rhs=pt[:, c0:512], start=(i == 0), stop=(i == nch - 1)),
                  reads=[vt, pt], writes=[O], partial=(i > 0), inc=False)
            kb.op("pe", lambda: nc.tensor.matmul(Dn[:, c0:512], lhsT=self.ones_bf[:], rhs=pt[:, c0:512], start=(i == 0), stop=(i == nch - 1)),
                  reads=[self.ones_bf, pt], writes=[Dn], partial=(i > 0), inc=True)
            if i == nch - 1:
                kb.op("dve", lambda: nc.vector.reciprocal(out=rd[:], in_=Dn[:]), reads=[Dn], writes=[rd])
                kb.op("act", lambda: nc.scalar.activation(out=sg[:], in_=gt[:], func=AF.Silu), reads=[gt], writes=[sg])
                kb.op("dve", lambda: nc.vector.tensor_tensor(out=sg[:], in0=sg[:], in1=rd[:], op=ALU.mult), reads=[sg, rd], writes=[sg])
                kb.op("dve", lambda: nc.vector.tensor_tensor(out=og[:, h, :], in0=O[:], in1=sg[:], op=ALU.mult), reads=[O, sg], writes=[og], partial=True)

        for tb in range(NTB):
            ts = slice(tb * 512, (tb + 1) * 512)
            nch = 4 * (tb + 1)
            for h in range(H):
                hs = slice(h * 128, (h + 1) * 128)
                kt = kbuf[h % 2]; vt = vbuf[h % 2]; qt = qbuf[h % 2]; gt = gbuf[h % 2]; b = bc[h % 2]
                kb.dma("sp", kt[:, 0:nch * 128], self.kT_s[hs, 0:nch * 128], reads=[self.kT_s], writes=[kt], sem_of=kt, partial=False)
                kb.dma("sp", vt[:, 0:nch, :], v_view[:, 0:nch, hs], reads=[self.v_s], writes=[vt], sem_of=vt, partial=False)
                kb.dma("sp", qt[:], self.qT_s[hs, ts], reads=[self.qT_s], writes=[qt], sem_of=qt, partial=False)
                kb.dma("sp", gt[:], self.gT_s[hs, ts], reads=[self.gT_s], writes=[gt], sem_of=gt, partial=False)
                kb.op("dve", lambda: nc.vector.tensor_scalar(out=b[:, 0:nch], in0=csT[:, 0:nch, h], scalar1=csl[:, nch - 1, h:h + 1], scalar2=None, op0=ALU.subtract),
                      reads=[csT, csl], writes=[b])
                O = psO[h % 2]; Dn = psD[h % 2]
                for i in range(nch):
                    a = i - 4 * tb
                    c0 = max(a, 0) * 128
                    ps = self.mmps()
                    kb.op("pe", lambda: nc.tensor.matmul(ps[:, c0:512], lhsT=kt[:, i * 128:(i + 1) * 128], rhs=qt[:, c0:512], start=True, stop=(a < 0)),
                          reads=[kt, qt], writes=[ps], inc=(a < 0))
                    if a >= 0:
                        kb.op("pe", lambda: nc.tensor.matmul(ps[:, c0:c0 + 128], lhsT=self.ident_bf[:], rhs=self.tri_bf[:], start=False, stop=True),
                              reads=[self.ident_bf, self.tri_bf], writes=[ps], partial=True)
                    pt = pbuf[kp % 4]; kp += 1
                    kb.op("act", lambda: nc.scalar.activation(out=pt[:, c0:512], in_=ps[:, c0:512], func=AF.Exp, bias=b[:, i:i + 1], scale=1.0),
                          reads=[ps, b], writes=[pt])
                    if prev is not None:
                        fox_pv(*prev)
                    prev = (h, i, nch, c0, pt, vt, O, Dn, gt)
            fox_pv(*prev)
            prev = None
            self.wout_block(tb, og, wo, hsrc, hdst, hres, psY)
        kb.pop()
        kb.pop()

    def rms_block(self, src, nchk, gcol, dst, nfeat):
        kb, nc = self.kb, self.nc
        pstat = self.psb[6]
        for c in range(nchk):
            s_ = self.r_sq[c % 2]
            kb.op("act", lambda: nc.scalar.activation(out=s_[:], in_=src[:, c, :], func=AF.Square), reads=[src], writes=[s_])
            kb.op("pe", lambda: nc.tensor.matmul(pstat[:], lhsT=self.consts[:, 3, :], rhs=s_[:], start=(c == 0), stop=(c == nchk - 1)),
                  reads=[s_, self.consts], writes=[pstat], partial=(c > 0), inc=True)
        kb.op("act", lambda: nc.scalar.activation(out=self.r_ln[:], in_=pstat[:], func=AF.Ln, scale=1.0 / nfeat, bias=self.epscol[:]),
              reads=[pstat, self.epscol], writes=[self.r_ln])
        kb.op("act", lambda: nc.scalar.activation(out=self.r_rstd[:], in_=self.r_ln[:], func=AF.Exp, scale=-0.5), reads=[self.r_ln], writes=[self.r_rstd])
        for c in range(nchk):
            d_ap, d_t = dst(c)
            kb.op("dve", lambda: nc.vector.scalar_tensor_tensor(out=d_ap, in0=src[:, c, :], scalar=gcol[:, c:c + 1], in1=self.r_rstd[:], op0=ALU.mult, op1=ALU.mult),
                  reads=[src, gcol, self.r_rstd], writes=[d_t], partial=True)

    def dsa_layer(self, li, hsrc, hdst):
        kb, nc, S = self.kb, self.nc, self.S
        W = self.w[li]
        NTB, NTK = self.NTB, self.NTK
        KIT = 16
        TOPK = min(256, S // 4)
        c_s = self.kT_s
        iq_s = self.qT_s
        kb.push()
        absw = kb.sb("absw", [128, NTK, 16], F32)
        sgn = kb.sb("sgn", [128, NTK, 16], F32)
        ckvnT = kb.sb("ckvnT", [128, 2, S], BF16)
        ckvtok = kb.sb("ckvtok", [128, NTK, 256], BF16)
        kb.push()
        hnT = kb.sb("hnT", [128, NC_, S], BF16)
        self.norm_phase(hsrc, li, hnT=hnT)
        wbuf = [kb.sb("wbuf%d" % i, [128, NC_, 128], BF16) for i in range(2)]
        obuf = [kb.sb("obuf%d" % i, [128, S], BF16) for i in range(2)]
        self.proj_cols(hnT, W["w_in"], QR + KVR + IDXD + IDXH, D, self.gT_s, wbuf=wbuf, obuf=obuf)
        self.proj_cols(hnT, W["w_in"], 0, 896, c_s, wbuf=wbuf, obuf=obuf)
        wiw = kb.sb("wiw", [128, NC_, 16], BF16)
        self.load_w_cols(wiw, W["w_in"], QR + KVR + IDXD, 16, NC_)
        pw = self.psb[5]
        for tk in range(NTK):
            for c in range(NC_):
                kb.op("pe", lambda: nc.tensor.matmul(pw[:, tk * 16:(tk + 1) * 16], lhsT=hnT[:, c, tk * 128:(tk + 1) * 128], rhs=wiw[:, c, :], start=(c == 0), stop=(c == NC_ - 1)),
                      reads=[hnT, wiw], writes=[pw], partial=not (tk == 0 and c == 0), inc=(c == NC_ - 1 and tk == NTK - 1))
        kb.op("act", lambda: nc.scalar.activation(out=absw[:].rearrange("p k h -> p (k h)"), in_=pw[:, 0:NTK * 16], func=AF.Abs, scale=1.0 / 32.0),
              reads=[pw], writes=[absw])
        kb.op("act", lambda: nc.scalar.activation(out=sgn[:].rearrange("p k h -> p (k h)"), in_=pw[:, 0:NTK * 16], func=AF.Sign), reads=[pw], writes=[sgn])
        kb.pop()
        kb.push()
        wq = kb.sb("wq", [128, 4, D], BF16)
        kb.dma("pool", wq[:], W["w_q_up"].rearrange("(c p) n -> p c n", p=128), writes=[wq], sem_of=wq, partial=False)
        wiq = kb.sb("wiq", [128, 4, 1024], BF16)
        kb.dma("pool", wiq[:], W["w_iq"].rearrange("(c p) n -> p c n", p=128), writes=[wiq], sem_of=wiq, partial=False)
        wuk = kb.sb("wuk", [128, H, KVR], BF16)
        kb.dma("pool", wuk[:], W["w_ukT"].rearrange("h d r -> d h r"), writes=[wuk], sem_of=wuk, partial=False)
        qn = kb.sb("qn", [128, 4], F32)
        kvn = kb.sb("kvn", [128, 2], F32)
        kb.dma("sp", qn[:], W["qn"], writes=[qn], sem_of=qn, partial=False)
        kb.dma("sp", kvn[:], W["kvn"], writes=[kvn], sem_of=kvn, partial=False)
        self.r_sq = [kb.sb("r_sq%d" % i, [128, 512], F32) for i in range(2)]
        self.r_ln = kb.sb("r_ln", [128, 512], F32)
        self.r_rstd = kb.sb("r_rstd", [128, 512], F32)
        cin = [kb.sb("cin%d" % i, [128, 6, 512], BF16) for i in range(2)]
        cqn = [kb.sb("cqn%d" % i, [128, 4, 512], BF16) for i in range(2)]
        qh = [kb.sb("qh%d" % i, [128, 512], BF16) for i in range(2)]
        qst = [kb.sb("qst%d" % i, [128, 2, 512], BF16) for i in range(2)]
        ist = [kb.sb("ist%d" % i, [128, 512], BF16) for i in range(2)]
        tst = kb.sb("tst", [128, 1024], BF16)
        qa_view = self.qa_s.t.rearrange("(h r p) t -> p h r t", r=2, p=128)
        c_view = c_s.t.rearrange("(c p) t -> p c t", p=128)
        for tb in range(NTB):
            ts = slice(tb * 512, (tb + 1) * 512)
            ci = cin[tb % 2]; cn = cqn[tb % 2]
            kb.dma("sp", ci[:], c_view[:, 0:6, ts], reads=[c_s], writes=[ci], sem_of=ci, partial=False)
            self.rms_block(ci, 4, qn, lambda c: (cn[:, c, :], cn), QR)
            ckv_src = T(ci.t[:, 4:6, :], "x"); ckv_src.dep = ci.dep
            self.rms_block(ckv_src, 2, kvn, lambda c: (ckvnT[:, c, ts], ckvnT), KVR)
            for q4 in range(4):
                tk = tb * 4 + q4
                for rc in range(2):
                    kb.op("pe", lambda: nc.tensor.transpose(out=self.pst[:, (q4 * 2 + rc) * 128:(q4 * 2 + rc + 1) * 128], in_=ckvnT[:, rc, tk * 128:(tk + 1) * 128], identity=self.ident_bf[:]),
                          reads=[ckvnT, self.ident_bf], writes=[self.pst], partial=not (q4 == 0 and rc == 0), inc=(q4 == 3 and rc == 1))
            kb.op("dve", lambda: nc.vector.tensor_copy(out=ckvtok[:, tb * 4:(tb + 1) * 4, :].rearrange("p k r -> p (k r)"), in_=self.pst[:, :]), reads=[self.pst], writes=[ckvtok], partial=True)
            for h in range(H):
                ps = self.mmps()
                for rc in range(4):
                    kb.op("pe", lambda: nc.tensor.matmul(ps[:], lhsT=wq[:, rc, h * 128:(h + 1) * 128], rhs=cn[:, rc, :], start=(rc == 0), stop=(rc == 3)),
                          reads=[wq, cn], writes=[ps], partial=(rc > 0), inc=(rc == 3))
                qt = qh[h % 2]
                self.evac(qt[:], ps[:], DH ** -0.5, [ps], qt, partial=False)
                st = qst[h % 2]
                for r2 in range(2):
                    ps2 = self.mmps()
                    kb.op("pe", lambda: nc.tensor.matmul(ps2[:], lhsT=wuk[:, h, r2 * 128:(r2 + 1) * 128], rhs=qt[:], start=True, stop=True), reads=[wuk, qt], writes=[ps2])
                    self.evac(st[:, r2, :], ps2[:], 1.0, [ps2], st, partial=(r2 > 0))
                kb.dma("sp", qa_view[:, h, :, ts], st[:], reads=[st], writes=[self.qa_s], sem_of=st)
            for m in range(8):
                ps = self.mmps()
                for rc in range(4):
                    kb.op("pe", lambda: nc.tensor.matmul(ps[:], lhsT=wiq[:, rc, m * 128:(m + 1) * 128], rhs=cn[:, rc, :], start=(rc == 0), stop=(rc == 3)),
                          reads=[wiq, cn], writes=[ps], partial=(rc > 0), inc=(rc == 3))
                it = ist[m % 2]
                self.evac(it[:], ps[:], 1.0, [ps], it, partial=False)
                kb.dma("sp", iq_s[m * 128:(m + 1) * 128, ts], it[:], reads=[it], writes=[iq_s], sem_of=it)
        kb.pop()
        kb.push()
        ikT = kb.sb("ikT", [64, S], BF16)
        kb.dma("sp", ikT[:], c_s[768:832, :], reads=[c_s], writes=[ikT], sem_of=ikT, partial=False)
        iqb = [kb.sb("iqb%d" % i, [64, 16, 128], BF16) for i in range(2)]
        acc = kb.sb("acc", [128, S], F32)
        junk = kb.sb("junk", [128, S], BF16)
        mk = kb.sb("mk", [128, S], BF16)
        rb = [kb.sb("rb%d" % i, [128, 512], F32) for i in range(3)]
        Mx = kb.sb("Mx", [128, 1], F32)
        stepv = kb.sb("stepv", [128, KIT + 1], F32)
        mid = kb.sb("mid", [128, KIT + 1], F32)
        cnt = kb.sb("cnt", [128, KIT + 1], F32)
        uu = kb.sb("uu", [128, 1], F32)
        thr = kb.sb("thr", [128, 1], F32)
        mst = [kb.sb("mst%d" % i, [128, NTK, 128], BF16) for i in range(2)]
        L01 = kb.sb("L01", [128, 128], BF16)
        kb.op("dve", lambda: nc.vector.tensor_copy(out=L01[:], in_=self.consts[:, 4, :]), reads=[self.consts], writes=[L01])
        iq_view = iq_s.t[0:1024, :].rearrange("(h d) t -> d h t", d=64)
        m_view = self.mT_s.t.rearrange("(c p) t -> p c t", p=128)
        kr = 0
        for qb in range(NTK):
            n = (qb + 1) * 128
            tq = slice(qb * 128, (qb + 1) * 128)
            if n > TOPK:
                iqt = iqb[qb % 2]
                kb.dma("sp", iqt[:], iq_view[:, :, tq], reads=[iq_s], writes=[iqt], sem_of=iqt, partial=False)
                for sb_ in range((n + 511) // 512):
                    wd = min(512, n - sb_ * 512)
                    ss = slice(sb_ * 512, sb_ * 512 + wd)
                    for h in range(IDXH):
                        ps = self.mmps()
                        kb.op("pe", lambda: nc.tensor.matmul(ps[:, 0:wd], lhsT=iqt[:, h, :], rhs=ikT[:, ss], start=True, stop=True), reads=[iqt, ikT], writes=[ps])
                        r_ = rb[kr % 3]; kr += 1
                        kb.op("act", lambda: nc.scalar.activation(out=r_[:, 0:wd], in_=ps[:, 0:wd], func=AF.Relu, scale=absw[:, qb, h:h + 1]), reads=[ps, absw], writes=[r_])
                        if h == 0:
                            kb.op("dve", lambda: nc.vector.tensor_scalar(out=acc[:, ss], in0=r_[:, 0:wd], scalar1=sgn[:, qb, 0:1], scalar2=None, op0=ALU.mult), reads=[r_, sgn], writes=[acc])
                        else:
                            kb.op("dve", lambda: nc.vector.scalar_tensor_tensor(out=acc[:, ss], in0=r_[:, 0:wd], scalar=sgn[:, qb, h:h + 1], in1=acc[:, ss], op0=ALU.mult, op1=ALU.add),
                                  reads=[r_, sgn, acc], writes=[acc])
                kb.op("dve", lambda: nc.vector.tensor_reduce(out=Mx[:], in_=acc[:, 0:n], axis=AX.X, op=ALU.max, apply_absolute_value=True), reads=[acc], writes=[Mx])
                kb.op("dve", lambda: nc.vector.tensor_tensor(out=acc[:, tq], in0=acc[:, tq], in1=self.consts[:, 5, :], op=ALU.add), reads=[acc, self.consts], writes=[acc])
                kb.op("dve", lambda: nc.vector.tensor_scalar(out=stepv[:], in0=self.pow2[:], scalar1=Mx[:, 0:1], scalar2=None, op0=ALU.mult), reads=[self.pow2, Mx], writes=[stepv])
                kb.op("dve", lambda: nc.vector.memset(mid[:, 0:1], 0.0), writes=[mid])
                for k in range(KIT):
                    kb.op("dve", lambda: nc.vector.tensor_scalar(out=junk[:, 0:n], in0=acc[:, 0:n], scalar1=mid[:, k:k + 1], scalar2=None, op0=ALU.is_ge, op1=ALU.add, accum_out=cnt[:, k:k + 1]),
                          reads=[acc, mid], writes=[junk, cnt])
                    kb.op("dve", lambda: nc.vector.tensor_scalar(out=uu[:], in0=cnt[:, k:k + 1], scalar1=TOPK - 0.5, scalar2=stepv[:, k:k + 1], op0=ALU.is_ge, op1=ALU.mult),
                          reads=[cnt, stepv], writes=[uu])
                    kb.op("dve", lambda: nc.vector.scalar_tensor_tensor(out=mid[:, k + 1:k + 2], in0=uu[:], scalar=stepv[:, k + 1:k + 2], in1=mid[:, k:k + 1], op0=ALU.subtract, op1=ALU.add),
                          reads=[uu, stepv, mid], writes=[mid])
                kb.op("dve", lambda: nc.vector.tensor_tensor(out=thr[:], in0=mid[:, KIT:KIT + 1], in1=stepv[:, KIT:KIT + 1], op=ALU.subtract), reads=[mid, stepv], writes=[thr])
                kb.op("dve", lambda: nc.vector.tensor_scalar(out=mk[:, 0:n], in0=acc[:, 0:n], scalar1=thr[:, 0:1], scalar2=None, op0=ALU.is_ge), reads=[acc, thr], writes=[mk])
            else:
                if qb > 0:
                    kb.op("dve", lambda: nc.vector.memset(mk[:, 0:qb * 128], 1.0), writes=[mk])
                kb.op("dve", lambda: nc.vector.tensor_copy(out=mk[:, tq], in_=L01[:]), reads=[L01], writes=[mk], partial=(qb > 0))
            ms = mst[qb % 2]
            for c0 in range(0, qb + 1, 8):
                ncb = min(8, qb + 1 - c0)
                for c in range(ncb):
                    kb.op("pe", lambda: nc.tensor.transpose(out=self.pst[:, c * 128:(c + 1) * 128], in_=mk[:, (c0 + c) * 128:(c0 + c + 1) * 128], identity=self.ident_bf[:]),
                          reads=[mk, self.ident_bf], writes=[self.pst], partial=(c > 0), inc=(c == ncb - 1))
                kb.op("act", lambda: nc.scalar.activation(out=ms[:, c0:c0 + ncb, :].rearrange("p c t -> p (c t)"), in_=self.pst[:, 0:ncb * 128], func=AF.Copy), reads=[self.pst], writes=[ms], partial=(c0 > 0))
            kb.dma("sp", m_view[:, 0:qb + 1, tq], ms[:, 0:qb + 1, :], reads=[ms], writes=[self.mT_s], sem_of=ms)
        kb.pop()
        kb.push()
        wo = kb.sb("wo", [128, H, D], BF16)
        for h4 in range(4):
            src = W["w_out"].rearrange("(c p) n -> p c n", p=128)[:, h4 * 4:(h4 + 1) * 4, :]
            kb.dma("pool", wo[:, h4 * 4:(h4 + 1) * 4, :], src, writes=[wo], sem_of=wo, partial=True)
        wuv = kb.sb("wuv", [128, 2, D], BF16)
        kb.dma("pool", wuv[:], W["w_uv"].rearrange("(c p) n -> p c n", p=128), writes=[wuv], sem_of=wuv, partial=False)
        b31 = kb.sb("b31", [128, 16], F32)
        kb.dma("sp", b31[:], W["rb31"], writes=[b31], sem_of=b31, partial=False)
        Dt = kb.sb("Dt", [128, H, 2, 128], BF16)
        dtmp = kb.sb("dtmp", [128, H, 2, 128], F32)
        kb.dma("sp", dtmp[:], W["rbD"], writes=[dtmp], sem_of=dtmp, partial=False)
        for h in range(H):
            kb.op("dve", lambda: nc.vector.tensor_scalar(out=Dt[:, h, :, :], in0=dtmp[:, h, :, :], scalar1=b31[:, h:h + 1], scalar2=None, op0=ALU.subtract),
                  reads=[dtmp, b31], writes=[Dt], partial=(h > 0))
        mT = kb.sb("mT", [128, NTK, 512], BF16)
        qab = [kb.sb("qab%d" % i, [128, 2, 512], BF16) for i in range(2)]
        gbuf = [kb.sb("gbuf%d" % i, [128, 512], BF16) for i in range(2)]
        pbuf = [kb.sb("pbuf%d" % i, [128, 512], BF16) for i in range(4)]
        ol = kb.sb("ol", [128, 2, 512], BF16)
        rd = kb.sb("rd", [128, 512], F32)
        sg = kb.sb("sg", [128, 512], F32)
        og = kb.sb("og", [128, H, 512], BF16)
        hres = [kb.sb("hres%d" % i, [128, 512], F32) for i in range(2)]
        O0, O1, Dn, psU, psY = self.psb[2], self.psb[3], self.psb[4], self.psb[5], self.psb[6]
        kp = 0
        prev = None

        def dsa_pv(h, i, nch, c0, pt, gt):
            hs = slice(h * 128, (h + 1) * 128)
            kb.op("pe", lambda: nc.tensor.matmul(O0[:, c0:512], lhsT=ckvtok[:, i, 0:128], rhs=pt[:, c0:512], start=(i == 0), stop=(i == nch - 1)),
                  reads=[ckvtok, pt], writes=[O0], partial=(i > 0), inc=False)
            kb.op("pe", lambda: nc.tensor.matmul(O1[:, c0:512], lhsT=ckvtok[:, i, 128:256], rhs=pt[:, c0:512], start=(i == 0), stop=(i == nch - 1)),
                  reads=[ckvtok, pt], writes=[O1], partial=(i > 0), inc=False)
            kb.op("pe", lambda: nc.tensor.matmul(Dn[:, c0:512], lhsT=self.ones_bf[:], rhs=pt[:, c0:512], start=(i == 0), stop=(i == nch - 1)),
                  reads=[self.ones_bf, pt], writes=[Dn], partial=(i > 0), inc=True)
            if i == nch - 1:
                kb.op("act", lambda: nc.scalar.activation(out=ol[:, 0, :], in_=O0[:], func=AF.Copy), reads=[O0], writes=[ol])
                kb.op("dve", lambda: nc.vector.tensor_copy(out=ol[:, 1, :], in_=O1[:]), reads=[O1], writes=[ol], partial=True)
                kb.op("dve", lambda: nc.vector.reciprocal(out=rd[:], in_=Dn[:]), reads=[Dn], writes=[rd])
                kb.op("pe", lambda: nc.tensor.matmul(psU[:], lhsT=wuv[:, 0, hs], rhs=ol[:, 0, :], start=True, stop=False), reads=[wuv, ol], writes=[psU], inc=False)
                kb.op("pe", lambda: nc.tensor.matmul(psU[:], lhsT=wuv[:, 1, hs], rhs=ol[:, 1, :], start=False, stop=True), reads=[wuv, ol], writes=[psU], partial=True)
                kb.op("act", lambda: nc.scalar.activation(out=sg[:], in_=gt[:], func=AF.Silu), reads=[gt], writes=[sg])
                kb.op("dve", lambda: nc.vector.tensor_tensor(out=sg[:], in0=sg[:], in1=rd[:], op=ALU.mult), reads=[sg, rd], writes=[sg])
                kb.op("dve", lambda: nc.vector.tensor_tensor(out=og[:, h, :], in0=psU[:], in1=sg[:], op=ALU.mult), reads=[psU, sg], writes=[og], partial=True)

        for tb in range(NTB):
            ts = slice(tb * 512, (tb + 1) * 512)
            nch = 4 * (tb + 1)
            kb.dma("sp", mT[:, 0:nch, :], m_view[:, 0:nch, ts], reads=[self.mT_s], writes=[mT], sem_of=mT, partial=False)
            for h in range(H):
                hs = slice(h * 128, (h + 1) * 128)
                qa = qab[h % 2]; gt = gbuf[h % 2]
                kb.dma("sp", qa[:], qa_view[:, h, :, ts], reads=[self.qa_s], writes=[qa], sem_of=qa, partial=False)
                kb.dma("sp", gt[:], self.gT_s[hs, ts], reads=[self.gT_s], writes=[gt], sem_of=gt, partial=False)
                for i in range(nch):
                    a = i - 4 * tb
                    c0 = max(a, 0) * 128
                    extra = [(c, 4 * tb + c - i) for c in range(4) if (4 * tb + c - i) in (0, 1) and c * 128 >= c0]
                    ps = self.mmps()
                    kb.op("pe", lambda: nc.tensor.matmul(ps[:, c0:512], lhsT=ckvnT[:, 0, i * 128:(i + 1) * 128], rhs=qa[:, 0, c0:512], start=True, stop=False),
                          reads=[ckvnT, qa], writes=[ps], inc=False)
                    kb.op("pe", lambda: nc.tensor.matmul(ps[:, c0:512], lhsT=ckvnT[:, 1, i * 128:(i + 1) * 128], rhs=qa[:, 1, c0:512], start=False, stop=(len(extra) == 0)),
                          reads=[ckvnT, qa], writes=[ps], partial=True, inc=(len(extra) == 0))
                    for ei, (c, df) in enumerate(extra):
                        kb.op("pe", lambda: nc.tensor.matmul(ps[:, c * 128:(c + 1) * 128], lhsT=self.ident_bf[:], rhs=Dt[:, h, df, :], start=False, stop=(ei == len(extra) - 1)),
                              reads=[self.ident_bf, Dt], writes=[ps], partial=True, inc=(ei == len(extra) - 1))
                    pt = pbuf[kp % 4]; kp += 1
                    kb.op("act", lambda: nc.scalar.activation(out=pt[:, c0:512], in_=ps[:, c0:512], func=AF.Exp, bias=b31[:, h:h + 1], scale=1.0),
                          reads=[ps, b31], writes=[pt])
                    kb.op("dve", lambda: nc.vector.tensor_tensor(out=pt[:, c0:512], in0=pt[:, c0:512], in1=mT[:, i, c0:512], op=ALU.mult), reads=[pt, mT], writes=[pt])
                    if prev is not None:
                        dsa_pv(*prev)
                    prev = (h, i, nch, c0, pt, gt)
            dsa_pv(*prev)
            prev = None
            self.wout_block(tb, og, wo, hsrc, hdst, hres, psY)
        kb.pop()
        kb.pop()


def make_consts():
    c = np.zeros((128, 6, 128), np.float32)
    pp = np.arange(128)[:, None]; jj = np.arange(128)[None, :]
    c[:, 4, :] = np.where(jj <= pp, 1.0, 0.0)
    c[:, 5, :] = np.where(jj <= pp, 0.0, -1e30)
    c[:, 0, :] = np.eye(128, dtype=np.float32)
    sp = np.arange(128)[:, None]; tp = np.arange(128)[None, :]
    c[:, 1, :] = np.where(sp <= tp, 0.0, NEG)
    c[127, 2, :] = 1.0
    c[:, 3, :] = 1.0
    return c


def cols16(v):
    return np.ascontiguousarray(v.reshape(16, 128).T)


def t5_bucket_np(dist):
    import math
    max_exact = 16
    d = np.maximum(dist, 0)
    df = np.maximum(d, 1).astype(np.float32)
    large = max_exact + (np.log(df / max_exact) / math.log(128 / max_exact) * (32 - max_exact)).astype(np.int32)
    large = np.minimum(large, 31)
    return np.where(d < max_exact, d, large)


def shared_inputs(p, layers, do_final=True):
    im = {"consts": make_consts()}
    g = np.zeros((128, 5, 16), np.float32)
    for i in range(4):
        g[:, i, :] = cols16(p["norm_g"][i])
    g[:, 4, :] = cols16(p["final_g"])
    im["gcols"] = g
    im["pow2"] = np.ascontiguousarray(np.broadcast_to((2.0 ** -np.arange(17, dtype=np.float64)).astype(np.float32), (128, 17)))
    sp = np.arange(128)[:, None]; tp = np.arange(128)[None, :]
    bk = np.stack([t5_bucket_np(tp - sp), t5_bucket_np(128 + tp - sp)], 0)
    for kind, li in layers:
        j = li // 2
        if kind == "b":
            im["w_in%d" % li] = np.ascontiguousarray(p["b_w_in"][j])
            im["f_bias%d" % li] = np.ascontiguousarray(p["b_f_bias"][j].reshape(16, 1))
            im["w_out%d" % li] = np.ascontiguousarray(p["b_w_out"][j])
        else:
            im["w_in%d" % li] = np.ascontiguousarray(p["a_w_in"][j])
            im["qn%d" % li] = np.ascontiguousarray(p["a_q_norm"][j].reshape(4, 128).T)
            im["kvn%d" % li] = np.ascontiguousarray(p["a_kv_norm"][j].reshape(2, 128).T)
            im["w_q_up%d" % li] = np.ascontiguousarray(p["a_w_q_up"][j])
            im["w_ukT%d" % li] = np.ascontiguousarray(np.transpose(p["a_w_uk"][j], (1, 2, 0)))
            im["w_uv%d" % li] = np.ascontiguousarray(p["a_w_uv"][j].reshape(KVR, D))
            im["w_iq%d" % li] = np.ascontiguousarray(p["a_w_iq"][j])
            im["w_out%d" % li] = np.ascontiguousarray(p["a_w_out"][j])
            rb = p["rel_bias"]
            im["rb31_%d" % li] = np.ascontiguousarray(np.broadcast_to(rb[31][None, :], (128, 16)))
            gath = rb[bk]
            im["rbD_%d" % li] = np.ascontiguousarray(np.transpose(gath, (1, 3, 0, 2)))
    return im


_PROG_CACHE = {}


def get_prog(S, layers, do_final):
    key = (S, tuple(layers), do_final)
    if key not in _PROG_CACHE:
        _PROG_CACHE[key] = Prog(S, list(layers), do_final=do_final)
    return _PROG_CACHE[key]


LAYERS = [("a", 0), ("b", 1), ("a", 2), ("b", 3)]
FUSED = True


def run_layers(xT_list, p, layers, do_final):
    S = xT_list[0].shape[1]
    prog = get_prog(S, layers, do_final)
    sh = shared_inputs(p, layers, do_final)
    in_maps = []
    for xT in xT_list:
        m = dict(sh)
        m["xT"] = xT
        in_maps.append(m)
    res = run_bass_kernel_spmd(prog.nc, in_maps, core_ids=list(range(len(xT_list))))
    key = "outT" if do_final else "hT"
    return [r[key] for r in res.results]


def kernel(**inputs):
    p = {k: np.asarray(v) for k, v in inputs.items()}
    x = p["x"]
    B = x.shape[0]
    xT = [np.ascontiguousarray(x[b].T) for b in range(B)]
    if FUSED:
        outT = run_layers(xT, p, LAYERS, True)
    else:
        cur = xT
        for n, lay in enumerate(LAYERS):
            cur = run_layers(cur, p, [lay], n == len(LAYERS) - 1)
        outT = cur
    return np.stack([np.ascontiguousarray(o.T) for o in outT], 0).astype(np.float32)
```

```python
import numpy as np
import concourse.bass as bass
import concourse.mybir as mybir
from concourse.bass_utils import run_bass_kernel_spmd

F32 = mybir.dt.float32
BF16 = mybir.dt.bfloat16
AF = mybir.ActivationFunctionType
ALU = mybir.AluOpType
AX = mybir.AxisListType

D = 2048
H = 16
DH = 128
NC_ = 16
QR = 512
KVR = 256
IDXH = 16
IDXD = 64
A_IN = QR + KVR + IDXD + IDXH + D
B_IN = 3 * D + H + D
EPS = 1e-6
NEG = -30000.0


class Dep:
    __slots__ = ("name", "w", "r", "dsem", "dcnt")

    def __init__(self, name):
        self.name = name
        self.w = {}
        self.r = {}
        self.dsem = None


class T:
    def __init__(self, t, name):
        self.t = t
        self.dep = Dep(name)

    def __getitem__(self, idx):
        return self.t[idx]


def _dep(d):
    return d.dep if isinstance(d, T) else d


class KB:
    NDMA = 48

    def __init__(self, nc):
        self.nc = nc
        self.engs = {"pe": nc.tensor, "act": nc.scalar, "dve": nc.vector,
                     "pool": nc.gpsimd, "sp": nc.sync}
        self._stack = [[]]
        self.semobj = {}
        self.semval = {}
        self.sems = {}
        for e in self.engs:
            key = "e_" + e
            self.semobj[key] = self._enter(nc.semaphore("s_" + e))
            self.semval[key] = 0
            self.sems[e] = key
        self.dfree = []
        for i in range(self.NDMA):
            key = "d_%d" % i
            self.semobj[key] = self._enter(nc.semaphore("sd_%d" % i))
            self.semval[key] = 0
            self.dfree.append(key)
        self.seen = {e: {} for e in self.engs}
        self.scope_deps = [[]]

    def _enter(self, cm):
        obj = cm.__enter__()
        self._stack[-1].append(cm)
        return obj

    def push(self):
        self._stack.append([])
        self.scope_deps.append([])

    def pop(self):
        self.barrier()
        for d in self.scope_deps.pop():
            if d.dsem is not None:
                self.dfree.append(d.dsem)
                d.dsem = None
        for cm in reversed(self._stack.pop()):
            cm.__exit__(None, None, None)

    def close(self):
        while len(self._stack) > 1:
            self.pop()
        for cm in reversed(self._stack[0]):
            cm.__exit__(None, None, None)

    def sb(self, name, shape, dt):
        self.uid = getattr(self, "uid", 0) + 1
        name = "%s_u%d" % (name, self.uid)
        t = T(self._enter(self.nc.sbuf_tensor(name, list(shape), dt)), name)
        self.scope_deps[-1].append(t.dep)
        return t

    def ps(self, name, shape, dt):
        t = T(self._enter(self.nc.psum_tensor(name, list(shape), dt)), name)
        self.scope_deps[-1].append(t.dep)
        return t

    def region(self, name):
        return Dep(name)

    def _wait(self, eng, key, val):
        if self.seen[eng].get(key, 0) >= val:
            return
        if key == self.sems[eng] and val <= self.semval[key] - 2:
            return
        self.engs[eng].wait_ge(self.semobj[key], val)
        self.seen[eng][key] = val

    def _deps(self, eng, reads, writes, partial):
        own = self.sems[eng]
        skip_own = (eng == "pe")
        for d in reads:
            d = _dep(d)
            for k, (v, _p) in d.w.items():
                if skip_own and k == own:
                    continue
                self._wait(eng, k, v)
        for d in writes:
            d = _dep(d)
            for k, v in d.r.items():
                if skip_own and k == own:
                    continue
                self._wait(eng, k, v)
            for k, (v, p) in d.w.items():
                if partial and p:
                    continue
                if skip_own and k == own:
                    continue
                self._wait(eng, k, v)

    def _record(self, key, val, reads, writes, partial):
        for d in reads:
            d = _dep(d)
            if d.r.get(key, 0) < val:
                d.r[key] = val
        for d in writes:
            d = _dep(d)
            if partial and not d.r and all(p for (_v, p) in d.w.values()):
                d.w[key] = (val, True)
            else:
                d.w = {key: (val, partial)}
                d.r = {}

    def op(self, eng, fn, reads=(), writes=(), partial=False, inc=True):
        self._deps(eng, reads, writes, partial)
        ins = fn()
        key = self.sems[eng]
        if inc:
            self.semval[key] += 1
            ins.then_inc(self.semobj[key], 1)
            val = self.semval[key]
        else:
            val = self.semval[key] + 1
        self._record(key, val, reads, writes, partial)
        return ins

    def dma(self, q, out, in_, reads=(), writes=(), sem_of=None, partial=True, **kw):
        self._deps(q, reads, writes, partial)
        d = _dep(sem_of)
        if d.dsem is None:
            d.dsem = self.dfree.pop()
        key = d.dsem
        self.semval[key] += 16
        ins = self.engs[q].dma_start(out=out, in_=in_, **kw)
        ins.then_inc(self.semobj[key], 16)
        self._record(key, self.semval[key], reads, writes, partial)
        return ins

    def barrier(self):
        for e in self.engs:
            for key, v in self.semval.items():
                if v > 0:
                    self._wait(e, key, v)

    def wait_all_on(self, eng):
        for key, v in self.semval.items():
            if v > 0:
                self._wait(eng, key, v)


class Prog:
    def __init__(self, S, layers, first_src_is_x=True, do_final=True):
        self.S = S
        self.NTB = S // 512
        self.NTK = S // 128
        self.layers = layers
        self.do_final = do_final
        nc = self.nc = bass.Bass("TRN2", target_bir_lowering=False)
        self.kb = KB(nc)
        kb = self.kb
        dt = nc.dram_tensor
        self.xT = T(dt("xT", [D, S], F32, kind="ExternalInput").ap(), "xT")
        if do_final:
            self.outT = T(dt("outT", [D, S], F32, kind="ExternalOutput").ap(), "outT")
        self.hT = T(dt("hT", [D, S], F32, kind="Internal" if do_final else "ExternalOutput").ap(), "hT")
        self.consts_d = dt("consts", [128, 6, 128], F32, kind="ExternalInput").ap()
        self.gcols_d = dt("gcols", [128, 5, 16], F32, kind="ExternalInput").ap()
        self.w = {}
        for kind, li in layers:
            if kind == "b":
                self.w[li] = dict(
                    w_in=dt("w_in%d" % li, [D, B_IN], F32, kind="ExternalInput").ap(),
                    f_bias=dt("f_bias%d" % li, [16, 1], F32, kind="ExternalInput").ap(),
                    w_out=dt("w_out%d" % li, [D, D], F32, kind="ExternalInput").ap(),
                )
            else:
                self.w[li] = dict(
                    w_in=dt("w_in%d" % li, [D, A_IN], F32, kind="ExternalInput").ap(),
                    qn=dt("qn%d" % li, [128, 4], F32, kind="ExternalInput").ap(),
                    kvn=dt("kvn%d" % li, [128, 2], F32, kind="ExternalInput").ap(),
                    w_q_up=dt("w_q_up%d" % li, [QR, D], F32, kind="ExternalInput").ap(),
                    w_ukT=dt("w_ukT%d" % li, [H, DH, KVR], F32, kind="ExternalInput").ap(),
                    w_uv=dt("w_uv%d" % li, [KVR, D], F32, kind="ExternalInput").ap(),
                    w_iq=dt("w_iq%d" % li, [QR, IDXH * IDXD], F32, kind="ExternalInput").ap(),
                    w_out=dt("w_out%d" % li, [D, D], F32, kind="ExternalInput").ap(),
                    rb31=dt("rb31_%d" % li, [128, 16], F32, kind="ExternalInput").ap(),
                    rbD=dt("rbD_%d" % li, [128, 16, 2, 128], F32, kind="ExternalInput").ap(),
                )
        self.qT_s = T(dt("qT_s", [D, S], BF16, kind="Internal").ap(), "qT_s")
        self.kT_s = T(dt("kT_s", [D, S], BF16, kind="Internal").ap(), "kT_s")
        self.v_s = T(dt("v_s", [S, D], BF16, kind="Internal").ap(), "v_s")
        self.gT_s = T(dt("gT_s", [D, S], BF16, kind="Internal").ap(), "gT_s")

        self.consts = kb.sb("consts_sb", [128, 6, 128], F32)
        self.gcols = kb.sb("gcols_sb", [128, 5, 16], F32)
        kb.dma("sp", self.consts[:], self.consts_d, writes=[self.consts], sem_of=self.consts)
        kb.dma("sp", self.gcols[:], self.gcols_d, writes=[self.gcols], sem_of=self.gcols)
        self.ident_bf = kb.sb("ident_bf", [128, 128], BF16)
        self.tri_bf = kb.sb("tri_bf", [128, 128], BF16)
        self.ones_bf = kb.sb("ones_bf", [128, 128], BF16)
        for dst, ci in ((self.ident_bf, 0), (self.tri_bf, 1), (self.ones_bf, 3)):
            kb.op("dve", lambda: nc.vector.tensor_copy(out=dst[:], in_=self.consts[:, ci, :]),
                  reads=[self.consts], writes=[dst])
        self.epscol = kb.sb("epscol", [128, 1], F32)
        kb.op("dve", lambda: nc.vector.memset(self.epscol[:], EPS), writes=[self.epscol])
        self.pow2_d = dt("pow2", [128, 17], F32, kind="ExternalInput").ap()
        self.pow2 = kb.sb("pow2_sb", [128, 17], F32)
        kb.dma("sp", self.pow2[:], self.pow2_d, writes=[self.pow2], sem_of=self.pow2)
        self.qa_s = T(dt("qa_s", [2 * D, S], BF16, kind="Internal").ap(), "qa_s")
        self.mT_s = T(dt("mT_s", [S, S], BF16, kind="Internal").ap(), "mT_s")
        self.psb = [kb.ps("psb%d" % i, [128, 512], F32) for i in range(7)]
        self.pst = kb.ps("pst", [128, 1024], BF16)
        self.k_mm = 0

        src = self.xT
        for kind, li in layers:
            if kind == "b":
                self.fox_layer(li, src, self.hT)
            else:
                self.dsa_layer(li, src, self.hT)
            src = self.hT
        if do_final:
            self.norm_phase(src, 4, final=True)
        kb.wait_all_on("sp")
        kb.close()

    def mmps(self):
        self.k_mm += 1
        return self.psb[self.k_mm % 2]

    def load_w_cols(self, wt, w_ap, col0, ncols, nchunk):
        src = w_ap.rearrange("(c p) n -> p c n", p=128)[:, :, col0:col0 + ncols]
        self.kb.dma("pool", wt[:, 0:nchunk, 0:ncols], src, writes=[wt], sem_of=wt, partial=False)

    def norm_phase(self, hsrc, gi, final=False, hnT=None):
        kb, nc, S = self.kb, self.nc, self.S
        kb.push()
        hb = [kb.sb("n_hb%d" % i, [128, 512], F32) for i in range(3)]
        sq = [kb.sb("n_sq%d" % i, [128, 512], F32) for i in range(2)]
        lnv = kb.sb("n_lnv", [128, 512], F32)
        rstd = kb.sb("n_rstd", [128, 512], F32)
        ob = [kb.sb("n_ob%d" % i, [128, 512], F32) for i in range(2)] if final else None
        ones_f = self.consts
        pstat = self.psb[6]
        k = 0
        for tb in range(self.NTB):
            ts = slice(tb * 512, (tb + 1) * 512)
            for c in range(NC_):
                h = hb[k % 3]; s = sq[k % 2]; k += 1
                kb.dma("sp", h[:], hsrc[c * 128:(c + 1) * 128, ts], reads=[hsrc], writes=[h], sem_of=h, partial=False)
                kb.op("act", lambda: nc.scalar.activation(out=s[:], in_=h[:], func=AF.Square), reads=[h], writes=[s])
                kb.op("pe", lambda: nc.tensor.matmul(pstat[:], lhsT=ones_f[:, 3, :], rhs=s[:], start=(c == 0), stop=(c == NC_ - 1)),
                      reads=[s, ones_f], writes=[pstat], partial=(c > 0), inc=True)
            kb.op("act", lambda: nc.scalar.activation(out=lnv[:], in_=pstat[:], func=AF.Ln, scale=1.0 / D, bias=self.epscol[:]),
                  reads=[pstat, self.epscol], writes=[lnv])
            kb.op("act", lambda: nc.scalar.activation(out=rstd[:], in_=lnv[:], func=AF.Exp, scale=-0.5), reads=[lnv], writes=[rstd])
            for c in range(NC_):
                h = hb[k % 3]; k += 1
                kb.dma("sp", h[:], hsrc[c * 128:(c + 1) * 128, ts], reads=[hsrc], writes=[h], sem_of=h, partial=False)
                if final:
                    o = ob[c % 2]
                    kb.op("dve", lambda: nc.vector.scalar_tensor_tensor(out=o[:], in0=h[:], scalar=self.gcols[:, gi, c:c + 1], in1=rstd[:], op0=ALU.mult, op1=ALU.mult),
                          reads=[h, rstd, self.gcols], writes=[o])
                    kb.dma("sp", self.outT[c * 128:(c + 1) * 128, ts], o[:], reads=[o], writes=[self.outT], sem_of=o)
                else:
                    kb.op("dve", lambda: nc.vector.scalar_tensor_tensor(out=hnT[:, c, ts], in0=h[:], scalar=self.gcols[:, gi, c:c + 1], in1=rstd[:], op0=ALU.mult, op1=ALU.mult),
                          reads=[h, rstd, self.gcols], writes=[hnT], partial=True)
        kb.pop()

    def proj_cols(self, hnT, w_ap, col0, ncols, dst, scale=1.0, token_major=False, wbuf=None, obuf=None):
        kb, nc, S = self.kb, self.nc, self.S
        for cc in range(ncols // 128):
            wt = wbuf[cc % 2]
            ob = obuf[cc % 2]
            self.load_w_cols(wt, w_ap, col0 + cc * 128, 128, NC_)
            if not token_major:
                for tb in range(self.NTB):
                    ts = slice(tb * 512, (tb + 1) * 512)
                    ps = self.mmps()
                    for c in range(NC_):
                        kb.op("pe", lambda: nc.tensor.matmul(ps[:], lhsT=wt[:, c, :], rhs=hnT[:, c, ts], start=(c == 0), stop=(c == NC_ - 1)),
                              reads=[wt, hnT], writes=[ps], partial=(c > 0), inc=(c == NC_ - 1))
                    self.evac(ob[:, ts], ps[:], scale, [ps], ob)
                kb.dma("sp", dst[cc * 128:(cc + 1) * 128, :], ob[:], reads=[ob], writes=[dst], sem_of=ob)
            else:
                for tq in range(self.NTK // 4):
                    ps = self.mmps()
                    for q in range(4):
                        tk = tq * 4 + q
                        for c in range(NC_):
                            kb.op("pe", lambda: nc.tensor.matmul(ps[:, q * 128:(q + 1) * 128], lhsT=hnT[:, c, tk * 128:(tk + 1) * 128], rhs=wt[:, c, :],
                                                                 start=(c == 0), stop=(c == NC_ - 1)),
                                  reads=[wt, hnT], writes=[ps], partial=not (c == 0 and q == 0), inc=(c == NC_ - 1 and q == 3))
                    self.evac(ob[:, tq * 512:(tq + 1) * 512], ps[:], scale, [ps], ob)
                dv = dst.t.rearrange("(k p) n -> p k n", p=128)[:, :, cc * 128:(cc + 1) * 128]
                kb.dma("sp", dv, ob[:].rearrange("p (k n) -> p k n", n=128), reads=[ob], writes=[dst], sem_of=ob)

    def evac(self, out_ap, in_ap, scale, reads, wtile, partial=True):
        kb, nc = self.kb, self.nc
        self.k_ev = getattr(self, "k_ev", 0) + 1
        if self.k_ev % 2 == 0:
            kb.op("act", lambda: nc.scalar.activation(out=out_ap, in_=in_ap, func=AF.Copy, scale=float(scale)), reads=reads, writes=[wtile], partial=partial)
        else:
            kb.op("dve", lambda: nc.vector.tensor_scalar(out=out_ap, in0=in_ap, scalar1=float(scale), scalar2=None, op0=ALU.mult), reads=reads, writes=[wtile], partial=partial)

    def wout_block(self, tb, og, wo, hsrc, hdst, hres, psY):
        kb, nc = self.kb, self.nc
        ts = slice(tb * 512, (tb + 1) * 512)
        for dc in range(NC_):
            hr = hres[dc % 2]
            kb.dma("sp", hr[:], hsrc[dc * 128:(dc + 1) * 128, ts], reads=[hsrc], writes=[hr], sem_of=hr, partial=False)
            for h in range(H):
                kb.op("pe", lambda: nc.tensor.matmul(psY[:], lhsT=wo[:, h, dc * 128:(dc + 1) * 128], rhs=og[:, h, :], start=(h == 0), stop=(h == H - 1)),
                      reads=[wo, og], writes=[psY], partial=(h > 0), inc=(h == H - 1))
            kb.op("dve", lambda: nc.vector.tensor_tensor(out=hr[:], in0=psY[:], in1=hr[:], op=ALU.add), reads=[psY, hr], writes=[hr])
            kb.dma("sp", hdst[dc * 128:(dc + 1) * 128, ts], hr[:], reads=[hr], writes=[hdst], sem_of=hr)

    def fox_layer(self, li, hsrc, hdst):
        kb, nc, S = self.kb, self.nc, self.S
        W = self.w[li]
        NTB, NTK = self.NTB, self.NTK
        kb.push()
        csT = kb.sb("csT", [128, NTK, 16], F32)
        csl = kb.sb("csl", [128, NTK, 16], F32)
        kb.push()
        hnT = kb.sb("hnT", [128, NC_, S], BF16)
        self.norm_phase(hsrc, li, hnT=hnT)
        kb.push()
        wbuf = [kb.sb("wbuf%d" % i, [128, NC_, 128], BF16) for i in range(2)]
        obuf = [kb.sb("obuf%d" % i, [128, S], BF16) for i in range(2)]
        self.proj_cols(hnT, W["w_in"], 0, D, self.qT_s, scale=DH ** -0.5, wbuf=wbuf, obuf=obuf)
        self.proj_cols(hnT, W["w_in"], D, D, self.kT_s, wbuf=wbuf, obuf=obuf)
        self.proj_cols(hnT, W["w_in"], 3 * D + H, D, self.gT_s, wbuf=wbuf, obuf=obuf)
        self.proj_cols(hnT, W["w_in"], 2 * D, D, self.v_s, token_major=True, wbuf=wbuf, obuf=obuf)
        kb.pop()
        wf = kb.sb("wf", [128, NC_, 16], BF16)
        self.load_w_cols(wf, W["w_in"], 3 * D, 16, NC_)
        fb = kb.sb("fb", [16, 1], F32)
        kb.dma("sp", fb[:], W["f_bias"], writes=[fb], sem_of=fb, partial=False)
        nfb = kb.sb("nfb", [16, 1], F32)
        kb.op("dve", lambda: nc.vector.tensor_scalar(out=nfb[:], in0=fb[:], scalar1=-1.0, scalar2=None, op0=ALU.mult), reads=[fb], writes=[nfb])
        lf = kb.sb("lf", [16, S], F32)
        cs = kb.sb("cs", [16, S], F32)
        onesr = kb.sb("onesr", [16, S], F32)
        kb.op("dve", lambda: nc.vector.memset(onesr[:], 1.0), writes=[onesr])
        for tb in range(NTB):
            ts = slice(tb * 512, (tb + 1) * 512)
            ps = self.mmps()
            for c in range(NC_):
                kb.op("pe", lambda: nc.tensor.matmul(ps[0:16, :], lhsT=wf[:, c, :], rhs=hnT[:, c, ts], start=(c == 0), stop=(c == NC_ - 1)),
                      reads=[wf, hnT], writes=[ps], partial=(c > 0), inc=(c == NC_ - 1))
            kb.op("act", lambda: nc.scalar.activation(out=lf[:, ts], in_=ps[0:16, :], func=AF.Exp, scale=-1.0, bias=nfb[:]),
                  reads=[ps, nfb], writes=[lf], partial=True)
        one16 = kb.sb("one16", [16, 1], F32)
        kb.op("dve", lambda: nc.vector.memset(one16[:], 1.0), writes=[one16])
        kb.op("act", lambda: nc.scalar.activation(out=lf[:], in_=lf[:], func=AF.Ln, scale=1.0, bias=one16[:]), reads=[lf, one16], writes=[lf])
        kb.op("dve", lambda: nc.vector.tensor_tensor_scan(out=cs[:], data0=onesr[:], data1=lf[:], initial=0.0, op0=ALU.mult, op1=ALU.add),
              reads=[onesr, lf], writes=[cs])
        pT = self.psb[5]
        for tk in range(NTK):
            kb.op("pe", lambda: nc.tensor.transpose(out=pT[:, (tk % 32) * 16:(tk % 32) * 16 + 16], in_=cs[0:16, tk * 128:(tk + 1) * 128], identity=self.consts[0:16, 0, 0:16]),
                  reads=[cs, self.consts], writes=[pT], partial=(tk > 0), inc=(tk == NTK - 1))
        kb.op("dve", lambda: nc.vector.tensor_copy(out=csT[:].rearrange("p k h -> p (k h)"), in_=pT[:, 0:NTK * 16]), reads=[pT], writes=[csT])
        kb.op("pe", lambda: nc.tensor.matmul(pT[:, 0:NTK * 16], lhsT=self.consts[:, 2, :], rhs=csT[:].rearrange("p k h -> p (k h)"), start=True, stop=True),
              reads=[csT, self.consts], writes=[pT])
        kb.op("dve", lambda: nc.vector.tensor_copy(out=csl[:].rearrange("p k h -> p (k h)"), in_=pT[:, 0:NTK * 16]), reads=[pT], writes=[csl])
        kb.pop()

        kb.push()
        wo = kb.sb("wo", [128, H, D], BF16)
        for h4 in range(4):
            src = W["w_out"].rearrange("(c p) n -> p c n", p=128)[:, h4 * 4:(h4 + 1) * 4, :]
            kb.dma("pool", wo[:, h4 * 4:(h4 + 1) * 4, :], src, writes=[wo], sem_of=wo, partial=True)
        kbuf = [kb.sb("kbuf%d" % i, [128, S], BF16) for i in range(2)]
        vbuf = [kb.sb("vbuf%d" % i, [128, NTK, 128], BF16) for i in range(2)]
        qbuf = [kb.sb("qbuf%d" % i, [128, 512], BF16) for i in range(2)]
        gbuf = [kb.sb("gbuf%d" % i, [128, 512], BF16) for i in range(2)]
        pbuf = [kb.sb("pbuf%d" % i, [128, 512], BF16) for i in range(4)]
        bc = [kb.sb("bc%d" % i, [128, NTK], F32) for i in range(2)]
        rd = kb.sb("rd", [128, 512], F32)
        sg = kb.sb("sg", [128, 512], F32)
        og = kb.sb("og", [128, H, 512], BF16)
        hres = [kb.sb("hres%d" % i, [128, 512], F32) for i in range(2)]
        psO = [self.psb[2], self.psb[3]]
        psD = [self.psb[4], self.psb[5]]
        psY = self.psb[6]
        kp = 0
        v_view = self.v_s.t.rearrange("(k p) n -> p k n", p=128)
        prev = None

        def fox_pv(h, i, nch, c0, pt, vt, O, Dn, gt):
            kb.op("pe", lambda: nc.tensor.matmul(O[:, c0:512], lhsT=vt[:, i, :], rhs=pt[:, c0:512], start=(i == 0), stop=(i == nch - 1)),
                  reads=[vt, pt], writes=[O], partial=(i > 0), inc=False)
            kb.op("pe", lambda: nc.tensor.matmul(Dn[:, c0:512], lhsT=self.ones_bf[:], rhs=pt[:, c0:512], start=(i == 0), stop=(i == nch - 1)),
                  reads=[self.ones_bf, pt], writes=[Dn], partial=(i > 0), inc=True)
            if i == nch - 1:
                kb.op("dve", lambda: nc.vector.reciprocal(out=rd[:], in_=Dn[:]), reads=[Dn], writes=[rd])
                kb.op("act", lambda: nc.scalar.activation(out=sg[:], in_=gt[:], func=AF.Silu), reads=[gt], writes=[sg])
                kb.op("dve", lambda: nc.vector.tensor_tensor(out=sg[:], in0=sg[:], in1=rd[:], op=ALU.mult), reads=[sg, rd], writes=[sg])
                kb.op("dve", lambda: nc.vector.tensor_tensor(out=og[:, h, :], in0=O[:], in1=sg[:], op=ALU.mult), reads=[O, sg], writes=[og], partial=True)

        for tb in range(NTB):
            ts = slice(tb * 512, (tb + 1) * 512)
            nch = 4 * (tb + 1)
            for h in range(H):
                hs = slice(h * 128, (h + 1) * 128)
                kt = kbuf[h % 2]; vt = vbuf[h % 2]; qt = qbuf[h % 2]; gt = gbuf[h % 2]; b = bc[h % 2]
                kb.dma("sp", kt[:, 0:nch * 128], self.kT_s[hs, 0:nch * 128], reads=[self.kT_s], writes=[kt], sem_of=kt, partial=False)
                kb.dma("sp", vt[:, 0:nch, :], v_view[:, 0:nch, hs], reads=[self.v_s], writes=[vt], sem_of=vt, partial=False)
                kb.dma("sp", qt[:], self.qT_s[hs, ts], reads=[self.qT_s], writes=[qt], sem_of=qt, partial=False)
                kb.dma("sp", gt[:], self.gT_s[hs, ts], reads=[self.gT_s], writes=[gt], sem_of=gt, partial=False)
                kb.op("dve", lambda: nc.vector.tensor_scalar(out=b[:, 0:nch], in0=csT[:, 0:nch, h], scalar1=csl[:, nch - 1, h:h + 1], scalar2=None, op0=ALU.subtract),
                      reads=[csT, csl], writes=[b])
                O = psO[h % 2]; Dn = psD[h % 2]
                for i in range(nch):
                    a = i - 4 * tb
                    c0 = max(a, 0) * 128
                    ps = self.mmps()
                    kb.op("pe", lambda: nc.tensor.matmul(ps[:, c0:512], lhsT=kt[:, i * 128:(i + 1) * 128], rhs=qt[:, c0:512], start=True, stop=(a < 0)),
                          reads=[kt, qt], writes=[ps], inc=(a < 0))
                    if a >= 0:
                        kb.op("pe", lambda: nc.tensor.matmul(ps[:, c0:c0 + 128], lhsT=self.ident_bf[:], rhs=self.tri_bf[:], start=False, stop=True),
                              reads=[self.ident_bf, self.tri_bf], writes=[ps], partial=True)
                    pt = pbuf[kp % 4]; kp += 1
                    kb.op("act", lambda: nc.scalar.activation(out=pt[:, c0:512], in_=ps[:, c0:512], func=AF.Exp, bias=b[:, i:i + 1], scale=1.0),
                          reads=[ps, b], writes=[pt])
                    if prev is not None:
                        fox_pv(*prev)
                    prev = (h, i, nch, c0, pt, vt, O, Dn, gt)
            fox_pv(*prev)
            prev = None
            self.wout_block(tb, og, wo, hsrc, hdst, hres, psY)
        kb.pop()
        kb.pop()

    def rms_block(self, src, nchk, gcol, dst, nfeat):
        kb, nc = self.kb, self.nc
        pstat = self.psb[6]
        for c in range(nchk):
            s_ = self.r_sq[c % 2]
            kb.op("act", lambda: nc.scalar.activation(out=s_[:], in_=src[:, c, :], func=AF.Square), reads=[src], writes=[s_])
            kb.op("pe", lambda: nc.tensor.matmul(pstat[:], lhsT=self.consts[:, 3, :], rhs=s_[:], start=(c == 0), stop=(c == nchk - 1)),
                  reads=[s_, self.consts], writes=[pstat], partial=(c > 0), inc=True)
        kb.op("act", lambda: nc.scalar.activation(out=self.r_ln[:], in_=pstat[:], func=AF.Ln, scale=1.0 / nfeat, bias=self.epscol[:]),
              reads=[pstat, self.epscol], writes=[self.r_ln])
        kb.op("act", lambda: nc.scalar.activation(out=self.r_rstd[:], in_=self.r_ln[:], func=AF.Exp, scale=-0.5), reads=[self.r_ln], writes=[self.r_rstd])
        for c in range(nchk):
            d_ap, d_t = dst(c)
            kb.op("dve", lambda: nc.vector.scalar_tensor_tensor(out=d_ap, in0=src[:, c, :], scalar=gcol[:, c:c + 1], in1=self.r_rstd[:], op0=ALU.mult, op1=ALU.mult),
                  reads=[src, gcol, self.r_rstd], writes=[d_t], partial=True)

    def dsa_layer(self, li, hsrc, hdst):
        kb, nc, S = self.kb, self.nc, self.S
        W = self.w[li]
        NTB, NTK = self.NTB, self.NTK
        KIT = 14
        TOPK = min(256, S // 4)
        c_s = self.kT_s
        iq_s = self.qT_s
        kb.push()
        absw = kb.sb("absw", [128, NTK, 16], F32)
        sgn = kb.sb("sgn", [128, NTK, 16], F32)
        ckvnT = kb.sb("ckvnT", [128, 2, S], BF16)
        ckvtok = kb.sb("ckvtok", [128, NTK, 256], BF16)
        kb.push()
        hnT = kb.sb("hnT", [128, NC_, S], BF16)
        self.norm_phase(hsrc, li, hnT=hnT)
        wbuf = [kb.sb("wbuf%d" % i, [128, NC_, 128], BF16) for i in range(2)]
        obuf = [kb.sb("obuf%d" % i, [128, S], BF16) for i in range(2)]
        self.proj_cols(hnT, W["w_in"], QR + KVR + IDXD + IDXH, D, self.gT_s, wbuf=wbuf, obuf=obuf)
        self.proj_cols(hnT, W["w_in"], 0, 896, c_s, wbuf=wbuf, obuf=obuf)
        wiw = kb.sb("wiw", [128, NC_, 16], BF16)
        self.load_w_cols(wiw, W["w_in"], QR + KVR + IDXD, 16, NC_)
        pw = self.psb[5]
        for tk in range(NTK):
            for c in range(NC_):
                kb.op("pe", lambda: nc.tensor.matmul(pw[:, tk * 16:(tk + 1) * 16], lhsT=hnT[:, c, tk * 128:(tk + 1) * 128], rhs=wiw[:, c, :], start=(c == 0), stop=(c == NC_ - 1)),
                      reads=[hnT, wiw], writes=[pw], partial=not (tk == 0 and c == 0), inc=(c == NC_ - 1 and tk == NTK - 1))
        kb.op("act", lambda: nc.scalar.activation(out=absw[:].rearrange("p k h -> p (k h)"), in_=pw[:, 0:NTK * 16], func=AF.Abs, scale=1.0 / 32.0),
              reads=[pw], writes=[absw])
        kb.op("act", lambda: nc.scalar.activation(out=sgn[:].rearrange("p k h -> p (k h)"), in_=pw[:, 0:NTK * 16], func=AF.Sign), reads=[pw], writes=[sgn])
        kb.pop()
        kb.push()
        wq = kb.sb("wq", [128, 4, D], BF16)
        kb.dma("pool", wq[:], W["w_q_up"].rearrange("(c p) n -> p c n", p=128), writes=[wq], sem_of=wq, partial=False)
        wiq = kb.sb("wiq", [128, 4, 1024], BF16)
        kb.dma("pool", wiq[:], W["w_iq"].rearrange("(c p) n -> p c n", p=128), writes=[wiq], sem_of=wiq, partial=False)
        wuk = kb.sb("wuk", [128, H, KVR], BF16)
        kb.dma("pool", wuk[:], W["w_ukT"].rearrange("h d r -> d h r"), writes=[wuk], sem_of=wuk, partial=False)
        qn = kb.sb("qn", [128, 4], F32)
        kvn = kb.sb("kvn", [128, 2], F32)
        kb.dma("sp", qn[:], W["qn"], writes=[qn], sem_of=qn, partial=False)
        kb.dma("sp", kvn[:], W["kvn"], writes=[kvn], sem_of=kvn, partial=False)
        self.r_sq = [kb.sb("r_sq%d" % i, [128, 512], F32) for i in range(2)]
        self.r_ln = kb.sb("r_ln", [128, 512], F32)
        self.r_rstd = kb.sb("r_rstd", [128, 512], F32)
        cin = [kb.sb("cin%d" % i, [128, 6, 512], BF16) for i in range(2)]
        cqn = [kb.sb("cqn%d" % i, [128, 4, 512], BF16) for i in range(2)]
        qh = [kb.sb("qh%d" % i, [128, 512], BF16) for i in range(2)]
        qst = [kb.sb("qst%d" % i, [128, 2, 512], BF16) for i in range(2)]
        ist = [kb.sb("ist%d" % i, [128, 512], BF16) for i in range(2)]
        tst = kb.sb("tst", [128, 1024], BF16)
        qa_view = self.qa_s.t.rearrange("(h r p) t -> p h r t", r=2, p=128)
        c_view = c_s.t.rearrange("(c p) t -> p c t", p=128)
        for tb in range(NTB):
            ts = slice(tb * 512, (tb + 1) * 512)
            ci = cin[tb % 2]; cn = cqn[tb % 2]
            kb.dma("sp", ci[:], c_view[:, 0:6, ts], reads=[c_s], writes=[ci], sem_of=ci, partial=False)
            self.rms_block(ci, 4, qn, lambda c: (cn[:, c, :], cn), QR)
            ckv_src = T(ci.t[:, 4:6, :], "x"); ckv_src.dep = ci.dep
            self.rms_block(ckv_src, 2, kvn, lambda c: (ckvnT[:, c, ts], ckvnT), KVR)
            for q4 in range(4):
                tk = tb * 4 + q4
                for rc in range(2):
                    kb.op("pe", lambda: nc.tensor.transpose(out=self.pst[:, (q4 * 2 + rc) * 128:(q4 * 2 + rc + 1) * 128], in_=ckvnT[:, rc, tk * 128:(tk + 1) * 128], identity=self.ident_bf[:]),
                          reads=[ckvnT, self.ident_bf], writes=[self.pst], partial=not (q4 == 0 and rc == 0), inc=(q4 == 3 and rc == 1))
            kb.op("dve", lambda: nc.vector.tensor_copy(out=ckvtok[:, tb * 4:(tb + 1) * 4, :].rearrange("p k r -> p (k r)"), in_=self.pst[:, :]), reads=[self.pst], writes=[ckvtok], partial=True)
            for h in range(H):
                ps = self.mmps()
                for rc in range(4):
                    kb.op("pe", lambda: nc.tensor.matmul(ps[:], lhsT=wq[:, rc, h * 128:(h + 1) * 128], rhs=cn[:, rc, :], start=(rc == 0), stop=(rc == 3)),
                          reads=[wq, cn], writes=[ps], partial=(rc > 0), inc=(rc == 3))
                qt = qh[h % 2]
                self.evac(qt[:], ps[:], DH ** -0.5, [ps], qt, partial=False)
                st = qst[h % 2]
                for r2 in range(2):
                    ps2 = self.mmps()
                    kb.op("pe", lambda: nc.tensor.matmul(ps2[:], lhsT=wuk[:, h, r2 * 128:(r2 + 1) * 128], rhs=qt[:], start=True, stop=True), reads=[wuk, qt], writes=[ps2])
                    self.evac(st[:, r2, :], ps2[:], 1.0, [ps2], st, partial=(r2 > 0))
                kb.dma("sp", qa_view[:, h, :, ts], st[:], reads=[st], writes=[self.qa_s], sem_of=st)
            for m in range(8):
                ps = self.mmps()
                for rc in range(4):
                    kb.op("pe", lambda: nc.tensor.matmul(ps[:], lhsT=wiq[:, rc, m * 128:(m + 1) * 128], rhs=cn[:, rc, :], start=(rc == 0), stop=(rc == 3)),
                          reads=[wiq, cn], writes=[ps], partial=(rc > 0), inc=(rc == 3))
                it = ist[m % 2]
                self.evac(it[:], ps[:], 1.0, [ps], it, partial=False)
                kb.dma("sp", iq_s[m * 128:(m + 1) * 128, ts], it[:], reads=[it], writes=[iq_s], sem_of=it)
        kb.pop()
        kb.push()
        ikT = kb.sb("ikT", [64, S], BF16)
        kb.dma("sp", ikT[:], c_s[768:832, :], reads=[c_s], writes=[ikT], sem_of=ikT, partial=False)
        iqb = [kb.sb("iqb%d" % i, [64, 16, 128], BF16) for i in range(2)]
        accb = [kb.sb("acc%d" % i, [128, S], F32) for i in range(2)]
        junk = kb.sb("junk", [128, S], BF16)
        mk = kb.sb("mk", [128, S], BF16)
        rb = [kb.sb("rb%d" % i, [128, 512], BF16) for i in range(4)]
        Dgb = [kb.sb("Dg%d" % i, [128, IDXH, 128], BF16) for i in range(2)]
        Mx = kb.sb("Mx", [128, 1], F32)
        stepv = kb.sb("stepv", [128, KIT + 1], F32)
        mid = kb.sb("mid", [128, KIT + 1], F32)
        cnt = kb.sb("cnt", [128, KIT + 1], F32)
        uu = kb.sb("uu", [128, 1], F32)
        thr = kb.sb("thr", [128, 1], F32)
        mst = [kb.sb("mst%d" % i, [128, NTK, 128], BF16) for i in range(2)]
        L01 = kb.sb("L01", [128, 128], BF16)
        kb.op("dve", lambda: nc.vector.tensor_copy(out=L01[:], in_=self.consts[:, 4, :]), reads=[self.consts], writes=[L01])
        iq_view = iq_s.t[0:1024, :].rearrange("(h d) t -> d h t", d=64)
        m_view = self.mT_s.t.rearrange("(c p) t -> p c t", p=128)
        kr = 0
        ka = 0
        paccb = [self.psb[2], self.psb[3]]

        def idx_acc(h, pacc, wd, r_, Dg, acc, ss):
            kb.op("pe", lambda: nc.tensor.matmul(pacc[:, 0:wd], lhsT=Dg[:, h, :], rhs=r_[:, 0:wd], start=(h == 0), stop=(h == IDXH - 1)),
                  reads=[Dg, r_], writes=[pacc], partial=(h > 0), inc=True)
            if h == IDXH - 1:
                self.evac(acc[:, ss], pacc[:, 0:wd], 1.0, [pacc], acc)

        for qb in range(NTK):
            n = (qb + 1) * 128
            tq = slice(qb * 128, (qb + 1) * 128)
            if n > TOPK:
                acc = accb[qb % 2]
                Dg = Dgb[qb % 2]
                iqt = iqb[qb % 2]
                kb.dma("sp", iqt[:], iq_view[:, :, tq], reads=[iq_s], writes=[iqt], sem_of=iqt, partial=False)
                for h in range(IDXH):
                    kb.op("pool", lambda: nc.gpsimd.tensor_scalar(out=Dg[:, h, :], in0=self.ident_bf[:], scalar1=sgn[:, qb, h:h + 1], scalar2=1.0, op0=ALU.mult, op1=ALU.mult),
                          reads=[self.ident_bf, sgn], writes=[Dg], partial=(h > 0))
                pend = []
                for sb_ in range((n + 511) // 512):
                    wd = min(512, n - sb_ * 512)
                    ss = slice(sb_ * 512, sb_ * 512 + wd)
                    pacc = paccb[ka % 2]; ka += 1
                    for h in range(IDXH):
                        ps = self.mmps()
                        kb.op("pe", lambda: nc.tensor.matmul(ps[:, 0:wd], lhsT=iqt[:, h, :], rhs=ikT[:, ss], start=True, stop=True), reads=[iqt, ikT], writes=[ps])
                        r_ = rb[kr % 4]; kr += 1
                        kb.op("act", lambda: nc.scalar.activation(out=r_[:, 0:wd], in_=ps[:, 0:wd], func=AF.Relu, scale=absw[:, qb, h:h + 1]), reads=[ps, absw], writes=[r_])
                        pend.append((h, pacc, wd, r_, Dg, acc, ss))
                        if len(pend) > 1:
                            idx_acc(*pend.pop(0))
                while pend:
                    idx_acc(*pend.pop(0))
                kb.op("dve", lambda: nc.vector.tensor_reduce(out=Mx[:], in_=acc[:, 0:n], axis=AX.X, op=ALU.max, apply_absolute_value=True), reads=[acc], writes=[Mx])
                kb.op("dve", lambda: nc.vector.tensor_tensor(out=acc[:, tq], in0=acc[:, tq], in1=self.consts[:, 5, :], op=ALU.add), reads=[acc, self.consts], writes=[acc])
                kb.op("dve", lambda: nc.vector.tensor_scalar(out=stepv[:], in0=self.pow2[:, 0:KIT + 1], scalar1=Mx[:, 0:1], scalar2=None, op0=ALU.mult), reads=[self.pow2, Mx], writes=[stepv])
                kb.op("dve", lambda: nc.vector.memset(mid[:, 0:1], 0.0), writes=[mid])
                for k in range(KIT):
                    kb.op("dve", lambda: nc.vector.tensor_scalar(out=junk[:, 0:n], in0=acc[:, 0:n], scalar1=mid[:, k:k + 1], scalar2=None, op0=ALU.is_ge, op1=ALU.add, accum_out=cnt[:, k:k + 1]),
                          reads=[acc, mid], writes=[junk, cnt])
                    kb.op("dve", lambda: nc.vector.tensor_scalar(out=uu[:], in0=cnt[:, k:k + 1], scalar1=TOPK - 0.5, scalar2=stepv[:, k:k + 1], op0=ALU.is_ge, op1=ALU.mult),
                          reads=[cnt, stepv], writes=[uu])
                    kb.op("dve", lambda: nc.vector.scalar_tensor_tensor(out=mid[:, k + 1:k + 2], in0=uu[:], scalar=stepv[:, k + 1:k + 2], in1=mid[:, k:k + 1], op0=ALU.subtract, op1=ALU.add),
                          reads=[uu, stepv, mid], writes=[mid])
                kb.op("dve", lambda: nc.vector.tensor_tensor(out=thr[:], in0=mid[:, KIT:KIT + 1], in1=stepv[:, KIT:KIT + 1], op=ALU.subtract), reads=[mid, stepv], writes=[thr])
                kb.op("dve", lambda: nc.vector.tensor_scalar(out=mk[:, 0:n], in0=acc[:, 0:n], scalar1=thr[:, 0:1], scalar2=None, op0=ALU.is_ge), reads=[acc, thr], writes=[mk])
            else:
                if qb > 0:
                    kb.op("dve", lambda: nc.vector.memset(mk[:, 0:qb * 128], 1.0), writes=[mk])
                kb.op("dve", lambda: nc.vector.tensor_copy(out=mk[:, tq], in_=L01[:]), reads=[L01], writes=[mk], partial=(qb > 0))
            ms = mst[qb % 2]
            for c0 in range(0, qb + 1, 8):
                ncb = min(8, qb + 1 - c0)
                for c in range(ncb):
                    kb.op("pe", lambda: nc.tensor.transpose(out=self.pst[:, c * 128:(c + 1) * 128], in_=mk[:, (c0 + c) * 128:(c0 + c + 1) * 128], identity=self.ident_bf[:]),
                          reads=[mk, self.ident_bf], writes=[self.pst], partial=(c > 0), inc=(c == ncb - 1))
                kb.op("act", lambda: nc.scalar.activation(out=ms[:, c0:c0 + ncb, :].rearrange("p c t -> p (c t)"), in_=self.pst[:, 0:ncb * 128], func=AF.Copy), reads=[self.pst], writes=[ms], partial=(c0 > 0))
            kb.dma("sp", m_view[:, 0:qb + 1, tq], ms[:, 0:qb + 1, :], reads=[ms], writes=[self.mT_s], sem_of=ms)
        kb.pop()
        kb.push()
        wo = kb.sb("wo", [128, H, D], BF16)
        for h4 in range(4):
            src = W["w_out"].rearrange("(c p) n -> p c n", p=128)[:, h4 * 4:(h4 + 1) * 4, :]
            kb.dma("pool", wo[:, h4 * 4:(h4 + 1) * 4, :], src, writes=[wo], sem_of=wo, partial=True)
        wuv = kb.sb("wuv", [128, 2, D], BF16)
        kb.dma("pool", wuv[:], W["w_uv"].rearrange("(c p) n -> p c n", p=128), writes=[wuv], sem_of=wuv, partial=False)
        b31 = kb.sb("b31", [128, 16], F32)
        kb.dma("sp", b31[:], W["rb31"], writes=[b31], sem_of=b31, partial=False)
        Dt = kb.sb("Dt", [128, H, 2, 128], BF16)
        dtmp = kb.sb("dtmp", [128, H, 2, 128], F32)
        kb.dma("sp", dtmp[:], W["rbD"], writes=[dtmp], sem_of=dtmp, partial=False)
        for h in range(H):
            kb.op("dve", lambda: nc.vector.tensor_scalar(out=Dt[:, h, :, :], in0=dtmp[:, h, :, :], scalar1=b31[:, h:h + 1], scalar2=None, op0=ALU.subtract),
                  reads=[dtmp, b31], writes=[Dt], partial=(h > 0))
        mT = kb.sb("mT", [128, NTK, 512], BF16)
        qab = [kb.sb("qab%d" % i, [128, 2, 512], BF16) for i in range(2)]
        gbuf = [kb.sb("gbuf%d" % i, [128, 512], BF16) for i in range(2)]
        pbuf = [kb.sb("pbuf%d" % i, [128, 512], BF16) for i in range(5)]
        ol = kb.sb("ol", [128, 2, 512], BF16)
        rd = kb.sb("rd", [128, 512], F32)
        sg = kb.sb("sg", [128, 512], F32)
        og = kb.sb("og", [128, H, 512], BF16)
        hres = [kb.sb("hres%d" % i, [128, 512], F32) for i in range(2)]
        O0, O1, Dn, psU, psY = self.psb[2], self.psb[3], self.psb[4], self.psb[6], self.psb[6]
        stb = [self.psb[0], self.psb[1], self.psb[5]]
        kp = 0
        pend = []
        LOOK = 2

        def dsa_pv(h, i, nch, c0, pt, gt):
            hs = slice(h * 128, (h + 1) * 128)
            kb.op("pe", lambda: nc.tensor.matmul(O0[:, c0:512], lhsT=ckvtok[:, i, 0:128], rhs=pt[:, c0:512], start=(i == 0), stop=(i == nch - 1)),
                  reads=[ckvtok, pt], writes=[O0], partial=(i > 0), inc=False)
            kb.op("pe", lambda: nc.tensor.matmul(O1[:, c0:512], lhsT=ckvtok[:, i, 128:256], rhs=pt[:, c0:512], start=(i == 0), stop=(i == nch - 1)),
                  reads=[ckvtok, pt], writes=[O1], partial=(i > 0), inc=False)
            kb.op("pe", lambda: nc.tensor.matmul(Dn[:, c0:512], lhsT=self.ones_bf[:], rhs=pt[:, c0:512], start=(i == 0), stop=(i == nch - 1)),
                  reads=[self.ones_bf, pt], writes=[Dn], partial=(i > 0), inc=True)
            if i == nch - 1:
                kb.op("act", lambda: nc.scalar.activation(out=ol[:, 0, :], in_=O0[:], func=AF.Copy), reads=[O0], writes=[ol])
                kb.op("dve", lambda: nc.vector.tensor_copy(out=ol[:, 1, :], in_=O1[:]), reads=[O1], writes=[ol], partial=True)
                kb.op("dve", lambda: nc.vector.reciprocal(out=rd[:], in_=Dn[:]), reads=[Dn], writes=[rd])
                kb.op("pe", lambda: nc.tensor.matmul(psU[:], lhsT=wuv[:, 0, hs], rhs=ol[:, 0, :], start=True, stop=False), reads=[wuv, ol], writes=[psU], inc=False)
                kb.op("pe", lambda: nc.tensor.matmul(psU[:], lhsT=wuv[:, 1, hs], rhs=ol[:, 1, :], start=False, stop=True), reads=[wuv, ol], writes=[psU], partial=True)
                kb.op("act", lambda: nc.scalar.activation(out=sg[:], in_=gt[:], func=AF.Silu), reads=[gt], writes=[sg])
                kb.op("dve", lambda: nc.vector.tensor_tensor(out=sg[:], in0=sg[:], in1=rd[:], op=ALU.mult), reads=[sg, rd], writes=[sg])
                kb.op("dve", lambda: nc.vector.tensor_tensor(out=og[:, h, :], in0=psU[:], in1=sg[:], op=ALU.mult), reads=[psU, sg], writes=[og], partial=True)

        for tb in range(NTB):
            ts = slice(tb * 512, (tb + 1) * 512)
            nch = 4 * (tb + 1)
            kb.dma("sp", mT[:, 0:nch, :], m_view[:, 0:nch, ts], reads=[self.mT_s], writes=[mT], sem_of=mT, partial=False)
            for h in range(H):
                hs = slice(h * 128, (h + 1) * 128)
                qa = qab[h % 2]; gt = gbuf[h % 2]
                kb.dma("sp", qa[:], qa_view[:, h, :, ts], reads=[self.qa_s], writes=[qa], sem_of=qa, partial=False)
                kb.dma("sp", gt[:], self.gT_s[hs, ts], reads=[self.gT_s], writes=[gt], sem_of=gt, partial=False)
                for i in range(nch):
                    a = i - 4 * tb
                    c0 = max(a, 0) * 128
                    extra = [(c, 4 * tb + c - i) for c in range(4) if (4 * tb + c - i) in (0, 1) and c * 128 >= c0]
                    ps = stb[kp % 3]
                    kb.op("pe", lambda: nc.tensor.matmul(ps[:, c0:512], lhsT=ckvnT[:, 0, i * 128:(i + 1) * 128], rhs=qa[:, 0, c0:512], start=True, stop=False),
                          reads=[ckvnT, qa], writes=[ps], inc=False)
                    kb.op("pe", lambda: nc.tensor.matmul(ps[:, c0:512], lhsT=ckvnT[:, 1, i * 128:(i + 1) * 128], rhs=qa[:, 1, c0:512], start=False, stop=(len(extra) == 0)),
                          reads=[ckvnT, qa], writes=[ps], partial=True, inc=(len(extra) == 0))
                    for ei, (c, df) in enumerate(extra):
                        kb.op("pe", lambda: nc.tensor.matmul(ps[:, c * 128:(c + 1) * 128], lhsT=self.ident_bf[:], rhs=Dt[:, h, df, :], start=False, stop=(ei == len(extra) - 1)),
                              reads=[self.ident_bf, Dt], writes=[ps], partial=True, inc=(ei == len(extra) - 1))
                    pt = pbuf[kp % 5]; kp += 1
                    kb.op("act", lambda: nc.scalar.activation(out=pt[:, c0:512], in_=ps[:, c0:512], func=AF.Exp, bias=b31[:, h:h + 1], scale=1.0),
                          reads=[ps, b31], writes=[pt])
                    kb.op("dve", lambda: nc.vector.tensor_tensor(out=pt[:, c0:512], in0=pt[:, c0:512], in1=mT[:, i, c0:512], op=ALU.mult), reads=[pt, mT], writes=[pt])
                    pend.append((h, i, nch, c0, pt, gt))
                    if len(pend) > LOOK:
                        dsa_pv(*pend.pop(0))
            while pend:
                dsa_pv(*pend.pop(0))
            self.wout_block(tb, og, wo, hsrc, hdst, hres, psY)
        kb.pop()
        kb.pop()


def make_consts():
    c = np.zeros((128, 6, 128), np.float32)
    pp = np.arange(128)[:, None]; jj = np.arange(128)[None, :]
    c[:, 4, :] = np.where(jj <= pp, 1.0, 0.0)
    c[:, 5, :] = np.where(jj <= pp, 0.0, -1e30)
    c[:, 0, :] = np.eye(128, dtype=np.float32)
    sp = np.arange(128)[:, None]; tp = np.arange(128)[None, :]
    c[:, 1, :] = np.where(sp <= tp, 0.0, NEG)
    c[127, 2, :] = 1.0
    c[:, 3, :] = 1.0
    return c


def cols16(v):
    return np.ascontiguousarray(v.reshape(16, 128).T)


def t5_bucket_np(dist):
    import math
    max_exact = 16
    d = np.maximum(dist, 0)
    df = np.maximum(d, 1).astype(np.float32)
    large = max_exact + (np.log(df / max_exact) / math.log(128 / max_exact) * (32 - max_exact)).astype(np.int32)
    large = np.minimum(large, 31)
    return np.where(d < max_exact, d, large)


def shared_inputs(p, layers, do_final=True):
    im = {"consts": make_consts()}
    g = np.zeros((128, 5, 16), np.float32)
    for i in range(4):
        g[:, i, :] = cols16(p["norm_g"][i])
    g[:, 4, :] = cols16(p["final_g"])
    im["gcols"] = g
    im["pow2"] = np.ascontiguousarray(np.broadcast_to((2.0 ** -np.arange(17, dtype=np.float64)).astype(np.float32), (128, 17)))
    sp = np.arange(128)[:, None]; tp = np.arange(128)[None, :]
    bk = np.stack([t5_bucket_np(tp - sp), t5_bucket_np(128 + tp - sp)], 0)
    for kind, li in layers:
        j = li // 2
        if kind == "b":
            im["w_in%d" % li] = np.ascontiguousarray(p["b_w_in"][j])
            im["f_bias%d" % li] = np.ascontiguousarray(p["b_f_bias"][j].reshape(16, 1))
            im["w_out%d" % li] = np.ascontiguousarray(p["b_w_out"][j])
        else:
            im["w_in%d" % li] = np.ascontiguousarray(p["a_w_in"][j])
            im["qn%d" % li] = np.ascontiguousarray(p["a_q_norm"][j].reshape(4, 128).T)
            im["kvn%d" % li] = np.ascontiguousarray(p["a_kv_norm"][j].reshape(2, 128).T)
            im["w_q_up%d" % li] = np.ascontiguousarray(p["a_w_q_up"][j])
            im["w_ukT%d" % li] = np.ascontiguousarray(np.transpose(p["a_w_uk"][j], (1, 2, 0)))
            im["w_uv%d" % li] = np.ascontiguousarray(p["a_w_uv"][j].reshape(KVR, D))
            im["w_iq%d" % li] = np.ascontiguousarray(p["a_w_iq"][j])
            im["w_out%d" % li] = np.ascontiguousarray(p["a_w_out"][j])
            rb = p["rel_bias"]
            im["rb31_%d" % li] = np.ascontiguousarray(np.broadcast_to(rb[31][None, :], (128, 16)))
            gath = rb[bk]
            im["rbD_%d" % li] = np.ascontiguousarray(np.transpose(gath, (1, 3, 0, 2)))
    return im


_PROG_CACHE = {}


def get_prog(S, layers, do_final):
    key = (S, tuple(layers), do_final)
    if key not in _PROG_CACHE:
        _PROG_CACHE[key] = Prog(S, list(layers), do_final=do_final)
    return _PROG_CACHE[key]


LAYERS = [("a", 0), ("b", 1), ("a", 2), ("b", 3)]
FUSED = True


def run_layers(xT_list, p, layers, do_final):
    S = xT_list[0].shape[1]
    prog = get_prog(S, layers, do_final)
    sh = shared_inputs(p, layers, do_final)
    in_maps = []
    for xT in xT_list:
        m = dict(sh)
        m["xT"] = xT
        in_maps.append(m)
    res = run_bass_kernel_spmd(prog.nc, in_maps, core_ids=list(range(len(xT_list))))
    key = "outT" if do_final else "hT"
    return [r[key] for r in res.results]


def kernel(**inputs):
    p = {k: np.asarray(v) for k, v in inputs.items()}
    x = p["x"]
    B = x.shape[0]
    xT = [np.ascontiguousarray(x[b].T) for b in range(B)]
    if FUSED:
        outT = run_layers(xT, p, LAYERS, True)
    else:
        cur = xT
        for n, lay in enumerate(LAYERS):
            cur = run_layers(cur, p, [lay], n == len(LAYERS) - 1)
        outT = cur
    return np.stack([np.ascontiguousarray(o.T) for o in outT], 0).astype(np.float32)
```

```python
import numpy as np
import concourse.bass as bass
import concourse.mybir as mybir
from concourse.bass_utils import run_bass_kernel_spmd

F32 = mybir.dt.float32
BF16 = mybir.dt.bfloat16
AF = mybir.ActivationFunctionType
ALU = mybir.AluOpType
AX = mybir.AxisListType

D = 2048
H = 16
DH = 128
NC_ = 16
QR = 512
KVR = 256
IDXH = 16
IDXD = 64
A_IN = QR + KVR + IDXD + IDXH + D
B_IN = 3 * D + H + D
EPS = 1e-6
NEG = -30000.0


class Dep:
    __slots__ = ("name", "w", "r", "dsem", "dcnt")

    def __init__(self, name):
        self.name = name
        self.w = {}
        self.r = {}
        self.dsem = None


class T:
    def __init__(self, t, name):
        self.t = t
        self.dep = Dep(name)

    def __getitem__(self, idx):
        return self.t[idx]


def _dep(d):
    return d.dep if isinstance(d, T) else d


class KB:
    NDMA = 48

    def __init__(self, nc):
        self.nc = nc
        self.engs = {"pe": nc.tensor, "act": nc.scalar, "dve": nc.vector,
                     "pool": nc.gpsimd, "sp": nc.sync}
        self._stack = [[]]
        self.semobj = {}
        self.semval = {}
        self.sems = {}
        for e in self.engs:
            key = "e_" + e
            self.semobj[key] = self._enter(nc.semaphore("s_" + e))
            self.semval[key] = 0
            self.sems[e] = key
        self.dfree = []
        for i in range(self.NDMA):
            key = "d_%d" % i
            self.semobj[key] = self._enter(nc.semaphore("sd_%d" % i))
            self.semval[key] = 0
            self.dfree.append(key)
        self.seen = {e: {} for e in self.engs}
        self.scope_deps = [[]]

    def _enter(self, cm):
        obj = cm.__enter__()
        self._stack[-1].append(cm)
        return obj

    def push(self):
        self._stack.append([])
        self.scope_deps.append([])

    def pop(self):
        self.barrier()
        for d in self.scope_deps.pop():
            if d.dsem is not None:
                self.dfree.append(d.dsem)
                d.dsem = None
        for cm in reversed(self._stack.pop()):
            cm.__exit__(None, None, None)

    def close(self):
        while len(self._stack) > 1:
            self.pop()
        for cm in reversed(self._stack[0]):
            cm.__exit__(None, None, None)

    def sb(self, name, shape, dt):
        self.uid = getattr(self, "uid", 0) + 1
        name = "%s_u%d" % (name, self.uid)
        t = T(self._enter(self.nc.sbuf_tensor(name, list(shape), dt)), name)
        self.scope_deps[-1].append(t.dep)
        return t

    def ps(self, name, shape, dt):
        t = T(self._enter(self.nc.psum_tensor(name, list(shape), dt)), name)
        self.scope_deps[-1].append(t.dep)
        return t

    def region(self, name):
        return Dep(name)

    def _wait(self, eng, key, val):
        if self.seen[eng].get(key, 0) >= val:
            return
        if key == self.sems[eng] and val <= self.semval[key] - 2:
            return
        self.engs[eng].wait_ge(self.semobj[key], val)
        self.seen[eng][key] = val

    def _deps(self, eng, reads, writes, partial):
        own = self.sems[eng]
        skip_own = (eng == "pe")
        for d in reads:
            d = _dep(d)
            for k, (v, _p) in d.w.items():
                if skip_own and k == own:
                    continue
                self._wait(eng, k, v)
        for d in writes:
            d = _dep(d)
            for k, v in d.r.items():
                if skip_own and k == own:
                    continue
                self._wait(eng, k, v)
            for k, (v, p) in d.w.items():
                if partial and p:
                    continue
                if skip_own and k == own:
                    continue
                self._wait(eng, k, v)

    def _record(self, key, val, reads, writes, partial):
        for d in reads:
            d = _dep(d)
            if d.r.get(key, 0) < val:
                d.r[key] = val
        for d in writes:
            d = _dep(d)
            if partial and not d.r and all(p for (_v, p) in d.w.values()):
                d.w[key] = (val, True)
            else:
                d.w = {key: (val, partial)}
                d.r = {}

    def op(self, eng, fn, reads=(), writes=(), partial=False, inc=True):
        self._deps(eng, reads, writes, partial)
        ins = fn()
        key = self.sems[eng]
        if inc:
            self.semval[key] += 1
            ins.then_inc(self.semobj[key], 1)
            val = self.semval[key]
        else:
            val = self.semval[key] + 1
        self._record(key, val, reads, writes, partial)
        return ins

    def dma(self, q, out, in_, reads=(), writes=(), sem_of=None, partial=True, **kw):
        self._deps(q, reads, writes, partial)
        d = _dep(sem_of)
        if d.dsem is None:
            d.dsem = self.dfree.pop()
        key = d.dsem
        self.semval[key] += 16
        ins = self.engs[q].dma_start(out=out, in_=in_, **kw)
        ins.then_inc(self.semobj[key], 16)
        self._record(key, self.semval[key], reads, writes, partial)
        return ins

    def barrier(self):
        for e in self.engs:
            for key, v in self.semval.items():
                if v > 0:
                    self._wait(e, key, v)

    def wait_all_on(self, eng):
        for key, v in self.semval.items():
            if v > 0:
                self._wait(eng, key, v)


class Prog:
    def __init__(self, S, layers, first_src_is_x=True, do_final=True):
        self.S = S
        self.NTB = S // 512
        self.NTK = S // 128
        self.layers = layers
        self.do_final = do_final
        nc = self.nc = bass.Bass("TRN2", target_bir_lowering=False)
        self.kb = KB(nc)
        kb = self.kb
        dt = nc.dram_tensor
        self.xT = T(dt("xT", [D, S], F32, kind="ExternalInput").ap(), "xT")
        if do_final:
            self.outT = T(dt("outT", [D, S], F32, kind="ExternalOutput").ap(), "outT")
        self.hT = T(dt("hT", [D, S], F32, kind="Internal" if do_final else "ExternalOutput").ap(), "hT")
        self.consts_d = dt("consts", [128, 6, 128], F32, kind="ExternalInput").ap()
        self.gcols_d = dt("gcols", [128, 5, 16], F32, kind="ExternalInput").ap()
        self.w = {}
        for kind, li in layers:
            if kind == "b":
                self.w[li] = dict(
                    w_in=dt("w_in%d" % li, [D, B_IN], F32, kind="ExternalInput").ap(),
                    f_bias=dt("f_bias%d" % li, [16, 1], F32, kind="ExternalInput").ap(),
                    w_out=dt("w_out%d" % li, [D, D], F32, kind="ExternalInput").ap(),
                )
            else:
                self.w[li] = dict(
                    w_in=dt("w_in%d" % li, [D, A_IN], F32, kind="ExternalInput").ap(),
                    qn=dt("qn%d" % li, [128, 4], F32, kind="ExternalInput").ap(),
                    kvn=dt("kvn%d" % li, [128, 2], F32, kind="ExternalInput").ap(),
                    w_q_up=dt("w_q_up%d" % li, [QR, D], F32, kind="ExternalInput").ap(),
                    w_ukT=dt("w_ukT%d" % li, [H, DH, KVR], F32, kind="ExternalInput").ap(),
                    w_uv=dt("w_uv%d" % li, [KVR, D], F32, kind="ExternalInput").ap(),
                    w_iq=dt("w_iq%d" % li, [QR, IDXH * IDXD], F32, kind="ExternalInput").ap(),
                    w_out=dt("w_out%d" % li, [D, D], F32, kind="ExternalInput").ap(),
                    rb31=dt("rb31_%d" % li, [128, 16], F32, kind="ExternalInput").ap(),
                    rbD=dt("rbD_%d" % li, [128, 16, 2, 128], F32, kind="ExternalInput").ap(),
                )
        self.qT_s = T(dt("qT_s", [D, S], BF16, kind="Internal").ap(), "qT_s")
        self.kT_s = T(dt("kT_s", [D, S], BF16, kind="Internal").ap(), "kT_s")
        self.v_s = T(dt("v_s", [S, D], BF16, kind="Internal").ap(), "v_s")
        self.gT_s = T(dt("gT_s", [D, S], BF16, kind="Internal").ap(), "gT_s")

        self.consts = kb.sb("consts_sb", [128, 6, 128], F32)
        self.gcols = kb.sb("gcols_sb", [128, 5, 16], F32)
        kb.dma("sp", self.consts[:], self.consts_d, writes=[self.consts], sem_of=self.consts)
        kb.dma("sp", self.gcols[:], self.gcols_d, writes=[self.gcols], sem_of=self.gcols)
        self.ident_bf = kb.sb("ident_bf", [128, 128], BF16)
        self.tri_bf = kb.sb("tri_bf", [128, 128], BF16)
        self.ones_bf = kb.sb("ones_bf", [128, 128], BF16)
        for dst, ci in ((self.ident_bf, 0), (self.tri_bf, 1), (self.ones_bf, 3)):
            kb.op("dve", lambda: nc.vector.tensor_copy(out=dst[:], in_=self.consts[:, ci, :]),
                  reads=[self.consts], writes=[dst])
        self.epscol = kb.sb("epscol", [128, 1], F32)
        kb.op("dve", lambda: nc.vector.memset(self.epscol[:], EPS), writes=[self.epscol])
        self.pow2_d = dt("pow2", [128, 17], F32, kind="ExternalInput").ap()
        self.pow2 = kb.sb("pow2_sb", [128, 17], F32)
        kb.dma("sp", self.pow2[:], self.pow2_d, writes=[self.pow2], sem_of=self.pow2)
        self.qa_s = T(dt("qa_s", [2 * D, S], BF16, kind="Internal").ap(), "qa_s")
        self.mT_s = T(dt("mT_s", [S, S], BF16, kind="Internal").ap(), "mT_s")
        self.psb = [kb.ps("psb%d" % i, [128, 512], F32) for i in range(7)]
        self.pst = kb.ps("pst", [128, 1024], BF16)
        self.k_mm = 0

        src = self.xT
        for kind, li in layers:
            if kind == "b":
                self.fox_layer(li, src, self.hT)
            else:
                self.dsa_layer(li, src, self.hT)
            src = self.hT
        if do_final:
            self.norm_phase(src, 4, final=True)
        kb.wait_all_on("sp")
        kb.close()

    def mmps(self):
        self.k_mm += 1
        return self.psb[self.k_mm % 2]

    def load_w_cols(self, wt, w_ap, col0, ncols, nchunk):
        src = w_ap.rearrange("(c p) n -> p c n", p=128)[:, :, col0:col0 + ncols]
        self.kb.dma("pool", wt[:, 0:nchunk, 0:ncols], src, writes=[wt], sem_of=wt, partial=False)

    def norm_phase(self, hsrc, gi, final=False, hnT=None):
        kb, nc, S = self.kb, self.nc, self.S
        kb.push()
        hb = [kb.sb("n_hb%d" % i, [128, 512], F32) for i in range(3)]
        sq = [kb.sb("n_sq%d" % i, [128, 512], F32) for i in range(2)]
        lnv = kb.sb("n_lnv", [128, 512], F32)
        rstd = kb.sb("n_rstd", [128, 512], F32)
        ob = [kb.sb("n_ob%d" % i, [128, 512], F32) for i in range(2)] if final else None
        ones_f = self.consts
        pstat = self.psb[6]
        k = 0
        for tb in range(self.NTB):
            ts = slice(tb * 512, (tb + 1) * 512)
            for c in range(NC_):
                h = hb[k % 3]; s = sq[k % 2]; k += 1
                kb.dma("sp", h[:], hsrc[c * 128:(c + 1) * 128, ts], reads=[hsrc], writes=[h], sem_of=h, partial=False)
                kb.op("act", lambda: nc.scalar.activation(out=s[:], in_=h[:], func=AF.Square), reads=[h], writes=[s])
                kb.op("pe", lambda: nc.tensor.matmul(pstat[:], lhsT=ones_f[:, 3, :], rhs=s[:], start=(c == 0), stop=(c == NC_ - 1)),
                      reads=[s, ones_f], writes=[pstat], partial=(c > 0), inc=True)
            kb.op("act", lambda: nc.scalar.activation(out=lnv[:], in_=pstat[:], func=AF.Ln, scale=1.0 / D, bias=self.epscol[:]),
                  reads=[pstat, self.epscol], writes=[lnv])
            kb.op("act", lambda: nc.scalar.activation(out=rstd[:], in_=lnv[:], func=AF.Exp, scale=-0.5), reads=[lnv], writes=[rstd])
            for c in range(NC_):
                h = hb[k % 3]; k += 1
                kb.dma("sp", h[:], hsrc[c * 128:(c + 1) * 128, ts], reads=[hsrc], writes=[h], sem_of=h, partial=False)
                if final:
                    o = ob[c % 2]
                    kb.op("dve", lambda: nc.vector.scalar_tensor_tensor(out=o[:], in0=h[:], scalar=self.gcols[:, gi, c:c + 1], in1=rstd[:], op0=ALU.mult, op1=ALU.mult),
                          reads=[h, rstd, self.gcols], writes=[o])
                    kb.dma("sp", self.outT[c * 128:(c + 1) * 128, ts], o[:], reads=[o], writes=[self.outT], sem_of=o)
                else:
                    kb.op("dve", lambda: nc.vector.scalar_tensor_tensor(out=hnT[:, c, ts], in0=h[:], scalar=self.gcols[:, gi, c:c + 1], in1=rstd[:], op0=ALU.mult, op1=ALU.mult),
                          reads=[h, rstd, self.gcols], writes=[hnT], partial=True)
        kb.pop()

    def proj_cols(self, hnT, w_ap, col0, ncols, dst, scale=1.0, token_major=False, wbuf=None, obuf=None):
        kb, nc, S = self.kb, self.nc, self.S
        for cc in range(ncols // 128):
            wt = wbuf[cc % 2]
            ob = obuf[cc % 2]
            self.load_w_cols(wt, w_ap, col0 + cc * 128, 128, NC_)
            if not token_major:
                for tb in range(self.NTB):
                    ts = slice(tb * 512, (tb + 1) * 512)
                    ps = self.mmps()
                    for c in range(NC_):
                        kb.op("pe", lambda: nc.tensor.matmul(ps[:], lhsT=wt[:, c, :], rhs=hnT[:, c, ts], start=(c == 0), stop=(c == NC_ - 1)),
                              reads=[wt, hnT], writes=[ps], partial=(c > 0), inc=(c == NC_ - 1))
                    self.evac(ob[:, ts], ps[:], scale, [ps], ob)
                kb.dma("sp", dst[cc * 128:(cc + 1) * 128, :], ob[:], reads=[ob], writes=[dst], sem_of=ob)
            else:
                for tq in range(self.NTK // 4):
                    ps = self.mmps()
                    for q in range(4):
                        tk = tq * 4 + q
                        for c in range(NC_):
                            kb.op("pe", lambda: nc.tensor.matmul(ps[:, q * 128:(q + 1) * 128], lhsT=hnT[:, c, tk * 128:(tk + 1) * 128], rhs=wt[:, c, :],
                                                                 start=(c == 0), stop=(c == NC_ - 1)),
                                  reads=[wt, hnT], writes=[ps], partial=not (c == 0 and q == 0), inc=(c == NC_ - 1 and q == 3))
                    self.evac(ob[:, tq * 512:(tq + 1) * 512], ps[:], scale, [ps], ob)
                dv = dst.t.rearrange("(k p) n -> p k n", p=128)[:, :, cc * 128:(cc + 1) * 128]
                kb.dma("sp", dv, ob[:].rearrange("p (k n) -> p k n", n=128), reads=[ob], writes=[dst], sem_of=ob)

    def evac(self, out_ap, in_ap, scale, reads, wtile, partial=True):
        kb, nc = self.kb, self.nc
        self.k_ev = getattr(self, "k_ev", 0) + 1
        if self.k_ev % 2 == 0:
            kb.op("act", lambda: nc.scalar.activation(out=out_ap, in_=in_ap, func=AF.Copy, scale=float(scale)), reads=reads, writes=[wtile], partial=partial)
        else:
            kb.op("dve", lambda: nc.vector.tensor_scalar(out=out_ap, in0=in_ap, scalar1=float(scale), scalar2=None, op0=ALU.mult), reads=reads, writes=[wtile], partial=partial)

    def wout_block(self, tb, og, wo, hsrc, hdst, hres, psY):
        kb, nc = self.kb, self.nc
        ts = slice(tb * 512, (tb + 1) * 512)
        for dc in range(NC_):
            hr = hres[dc % 2]
            kb.dma("sp", hr[:], hsrc[dc * 128:(dc + 1) * 128, ts], reads=[hsrc], writes=[hr], sem_of=hr, partial=False)
            for h in range(H):
                kb.op("pe", lambda: nc.tensor.matmul(psY[:], lhsT=wo[:, h, dc * 128:(dc + 1) * 128], rhs=og[:, h, :], start=(h == 0), stop=(h == H - 1)),
                      reads=[wo, og], writes=[psY], partial=(h > 0), inc=(h == H - 1))
            kb.op("dve", lambda: nc.vector.tensor_tensor(out=hr[:], in0=psY[:], in1=hr[:], op=ALU.add), reads=[psY, hr], writes=[hr])
            kb.dma("sp", hdst[dc * 128:(dc + 1) * 128, ts], hr[:], reads=[hr], writes=[hdst], sem_of=hr)

    def fox_layer(self, li, hsrc, hdst):
        kb, nc, S = self.kb, self.nc, self.S
        W = self.w[li]
        NTB, NTK = self.NTB, self.NTK
        kb.push()
        csT = kb.sb("csT", [128, NTK, 16], F32)
        csl = kb.sb("csl", [128, NTK, 16], F32)
        kb.push()
        hnT = kb.sb("hnT", [128, NC_, S], BF16)
        self.norm_phase(hsrc, li, hnT=hnT)
        kb.push()
        wbuf = [kb.sb("wbuf%d" % i, [128, NC_, 128], BF16) for i in range(2)]
        obuf = [kb.sb("obuf%d" % i, [128, S], BF16) for i in range(2)]
        self.proj_cols(hnT, W["w_in"], 0, D, self.qT_s, scale=DH ** -0.5, wbuf=wbuf, obuf=obuf)
        self.proj_cols(hnT, W["w_in"], D, D, self.kT_s, wbuf=wbuf, obuf=obuf)
        self.proj_cols(hnT, W["w_in"], 3 * D + H, D, self.gT_s, wbuf=wbuf, obuf=obuf)
        self.proj_cols(hnT, W["w_in"], 2 * D, D, self.v_s, token_major=True, wbuf=wbuf, obuf=obuf)
        kb.pop()
        wf = kb.sb("wf", [128, NC_, 16], BF16)
        self.load_w_cols(wf, W["w_in"], 3 * D, 16, NC_)
        fb = kb.sb("fb", [16, 1], F32)
        kb.dma("sp", fb[:], W["f_bias"], writes=[fb], sem_of=fb, partial=False)
        nfb = kb.sb("nfb", [16, 1], F32)
        kb.op("dve", lambda: nc.vector.tensor_scalar(out=nfb[:], in0=fb[:], scalar1=-1.0, scalar2=None, op0=ALU.mult), reads=[fb], writes=[nfb])
        lf = kb.sb("lf", [16, S], F32)
        cs = kb.sb("cs", [16, S], F32)
        onesr = kb.sb("onesr", [16, S], F32)
        kb.op("dve", lambda: nc.vector.memset(onesr[:], 1.0), writes=[onesr])
        for tb in range(NTB):
            ts = slice(tb * 512, (tb + 1) * 512)
            ps = self.mmps()
            for c in range(NC_):
                kb.op("pe", lambda: nc.tensor.matmul(ps[0:16, :], lhsT=wf[:, c, :], rhs=hnT[:, c, ts], start=(c == 0), stop=(c == NC_ - 1)),
                      reads=[wf, hnT], writes=[ps], partial=(c > 0), inc=(c == NC_ - 1))
            kb.op("act", lambda: nc.scalar.activation(out=lf[:, ts], in_=ps[0:16, :], func=AF.Exp, scale=-1.0, bias=nfb[:]),
                  reads=[ps, nfb], writes=[lf], partial=True)
        one16 = kb.sb("one16", [16, 1], F32)
        kb.op("dve", lambda: nc.vector.memset(one16[:], 1.0), writes=[one16])
        kb.op("act", lambda: nc.scalar.activation(out=lf[:], in_=lf[:], func=AF.Ln, scale=1.0, bias=one16[:]), reads=[lf, one16], writes=[lf])
        kb.op("dve", lambda: nc.vector.tensor_tensor_scan(out=cs[:], data0=onesr[:], data1=lf[:], initial=0.0, op0=ALU.mult, op1=ALU.add),
              reads=[onesr, lf], writes=[cs])
        pT = self.psb[5]
        for tk in range(NTK):
            kb.op("pe", lambda: nc.tensor.transpose(out=pT[:, (tk % 32) * 16:(tk % 32) * 16 + 16], in_=cs[0:16, tk * 128:(tk + 1) * 128], identity=self.consts[0:16, 0, 0:16]),
                  reads=[cs, self.consts], writes=[pT], partial=(tk > 0), inc=(tk == NTK - 1))
        kb.op("dve", lambda: nc.vector.tensor_copy(out=csT[:].rearrange("p k h -> p (k h)"), in_=pT[:, 0:NTK * 16]), reads=[pT], writes=[csT])
        kb.op("pe", lambda: nc.tensor.matmul(pT[:, 0:NTK * 16], lhsT=self.consts[:, 2, :], rhs=csT[:].rearrange("p k h -> p (k h)"), start=True, stop=True),
              reads=[csT, self.consts], writes=[pT])
        kb.op("dve", lambda: nc.vector.tensor_copy(out=csl[:].rearrange("p k h -> p (k h)"), in_=pT[:, 0:NTK * 16]), reads=[pT], writes=[csl])
        kb.pop()

        kb.push()
        wo = kb.sb("wo", [128, H, D], BF16)
        for h4 in range(4):
            src = W["w_out"].rearrange("(c p) n -> p c n", p=128)[:, h4 * 4:(h4 + 1) * 4, :]
            kb.dma("pool", wo[:, h4 * 4:(h4 + 1) * 4, :], src, writes=[wo], sem_of=wo, partial=True)
        kbuf = [kb.sb("kbuf%d" % i, [128, S], BF16) for i in range(2)]
        vbuf = [kb.sb("vbuf%d" % i, [128, NTK, 128], BF16) for i in range(2)]
        qbuf = [kb.sb("qbuf%d" % i, [128, 512], BF16) for i in range(2)]
        gbuf = [kb.sb("gbuf%d" % i, [128, 512], BF16) for i in range(2)]
        pbuf = [kb.sb("pbuf%d" % i, [128, 512], BF16) for i in range(4)]
        bc = [kb.sb("bc%d" % i, [128, NTK], F32) for i in range(2)]
        rd = kb.sb("rd", [128, 512], F32)
        sg = kb.sb("sg", [128, 512], F32)
        og = kb.sb("og", [128, H, 512], BF16)
        hres = [kb.sb("hres%d" % i, [128, 512], F32) for i in range(2)]
        psO = [self.psb[2], self.psb[3]]
        psD = [self.psb[4], self.psb[5]]
        psY = self.psb[6]
        kp = 0
        v_view = self.v_s.t.rearrange("(k p) n -> p k n", p=128)
        prev = None

        def fox_pv(h, i, nch, c0, pt, vt, O, Dn, gt):
            kb.op("pe", lambda: nc.tensor.matmul(O[:, c0:512], lhsT=vt[:, i, :], rhs=pt[:, c0:512], start=(i == 0), stop=(i == nch - 1)),
                  reads=[vt, pt], writes=[O], partial=(i > 0), inc=False)
            kb.op("pe", lambda: nc.tensor.matmul(Dn[:, c0:512], lhsT=self.ones_bf[:], rhs=pt[:, c0:512], start=(i == 0), stop=(i == nch - 1)),
                  reads=[self.ones_bf, pt], writes=[Dn], partial=(i > 0), inc=True)
            if i == nch - 1:
                kb.op("dve", lambda: nc.vector.reciprocal(out=rd[:], in_=Dn[:]), reads=[Dn], writes=[rd])
                kb.op("act", lambda: nc.scalar.activation(out=sg[:], in_=gt[:], func=AF.Silu), reads=[gt], writes=[sg])
                kb.op("dve", lambda: nc.vector.tensor_tensor(out=sg[:], in0=sg[:], in1=rd[:], op=ALU.mult), reads=[sg, rd], writes=[sg])
                kb.op("dve", lambda: nc.vector.tensor_tensor(out=og[:, h, :], in0=O[:], in1=sg[:], op=ALU.mult), reads=[O, sg], writes=[og], partial=True)

        for tb in range(NTB):
            ts = slice(tb * 512, (tb + 1) * 512)
            nch = 4 * (tb + 1)
            for h in range(H):
                hs = slice(h * 128, (h + 1) * 128)
                kt = kbuf[h % 2]; vt = vbuf[h % 2]; qt = qbuf[h % 2]; gt = gbuf[h % 2]; b = bc[h % 2]
                kb.dma("sp", kt[:, 0:nch * 128], self.kT_s[hs, 0:nch * 128], reads=[self.kT_s], writes=[kt], sem_of=kt, partial=False)
                kb.dma("sp", vt[:, 0:nch, :], v_view[:, 0:nch, hs], reads=[self.v_s], writes=[vt], sem_of=vt, partial=False)
                kb.dma("sp", qt[:], self.qT_s[hs, ts], reads=[self.qT_s], writes=[qt], sem_of=qt, partial=False)
                kb.dma("sp", gt[:], self.gT_s[hs, ts], reads=[self.gT_s], writes=[gt], sem_of=gt, partial=False)
                kb.op("dve", lambda: nc.vector.tensor_scalar(out=b[:, 0:nch], in0=csT[:, 0:nch, h], scalar1=csl[:, nch - 1, h:h + 1], scalar2=None, op0=ALU.subtract),
                      reads=[csT, csl], writes=[b])
                O = psO[h % 2]; Dn = psD[h % 2]
                for i in range(nch):
                    a = i - 4 * tb
                    c0 = max(a, 0) * 128
                    ps = self.mmps()
                    kb.op("pe", lambda: nc.tensor.matmul(ps[:, c0:512], lhsT=kt[:, i * 128:(i + 1) * 128], rhs=qt[:, c0:512], start=True, stop=(a < 0)),
                          reads=[kt, qt], writes=[ps], inc=(a < 0))
                    if a >= 0:
                        kb.op("pe", lambda: nc.tensor.matmul(ps[:, c0:c0 + 128], lhsT=self.ident_bf[:], rhs=self.tri_bf[:], start=False, stop=True),
                              reads=[self.ident_bf, self.tri_bf], writes=[ps], partial=True)
                    pt = pbuf[kp % 4]; kp += 1
                    kb.op("act", lambda: nc.scalar.activation(out=pt[:, c0:512], in_=ps[:, c0:512], func=AF.Exp, bias=b[:, i:i + 1], scale=1.0),
                          reads=[ps, b], writes=[pt])
                    if prev is not None:
                        fox_pv(*prev)
                    prev = (h, i, nch, c0, pt, vt, O, Dn, gt)
            fox_pv(*prev)
            prev = None
            self.wout_block(tb, og, wo, hsrc, hdst, hres, psY)
        kb.pop()
        kb.pop()

    def rms_block(self, src, nchk, gcol, dst, nfeat):
        kb, nc = self.kb, self.nc
        pstat = self.psb[6]
        for c in range(nchk):
            s_ = self.r_sq[c % 2]
            kb.op("act", lambda: nc.scalar.activation(out=s_[:], in_=src[:, c, :], func=AF.Square), reads=[src], writes=[s_])
            kb.op("pe", lambda: nc.tensor.matmul(pstat[:], lhsT=self.consts[:, 3, :], rhs=s_[:], start=(c == 0), stop=(c == nchk - 1)),
                  reads=[s_, self.consts], writes=[pstat], partial=(c > 0), inc=True)
        kb.op("act", lambda: nc.scalar.activation(out=self.r_ln[:], in_=pstat[:], func=AF.Ln, scale=1.0 / nfeat, bias=self.epscol[:]),
              reads=[pstat, self.epscol], writes=[self.r_ln])
        kb.op("act", lambda: nc.scalar.activation(out=self.r_rstd[:], in_=self.r_ln[:], func=AF.Exp, scale=-0.5), reads=[self.r_ln], writes=[self.r_rstd])
        for c in range(nchk):
            d_ap, d_t = dst(c)
            kb.op("dve", lambda: nc.vector.scalar_tensor_tensor(out=d_ap, in0=src[:, c, :], scalar=gcol[:, c:c + 1], in1=self.r_rstd[:], op0=ALU.mult, op1=ALU.mult),
                  reads=[src, gcol, self.r_rstd], writes=[d_t], partial=True)

    def dsa_layer(self, li, hsrc, hdst):
        kb, nc, S = self.kb, self.nc, self.S
        W = self.w[li]
        NTB, NTK = self.NTB, self.NTK
        KIT = 14
        TOPK = min(256, S // 4)
        c_s = self.kT_s
        iq_s = self.qT_s
        kb.push()
        absw = kb.sb("absw", [128, NTK, 16], F32)
        sgn = kb.sb("sgn", [128, NTK, 16], F32)
        ckvnT = kb.sb("ckvnT", [128, 2, S], BF16)
        ckvtok = kb.sb("ckvtok", [128, NTK, 256], BF16)
        kb.push()
        hnT = kb.sb("hnT", [128, NC_, S], BF16)
        self.norm_phase(hsrc, li, hnT=hnT)
        wbuf = [kb.sb("wbuf%d" % i, [128, NC_, 128], BF16) for i in range(2)]
        obuf = [kb.sb("obuf%d" % i, [128, S], BF16) for i in range(2)]
        self.proj_cols(hnT, W["w_in"], QR + KVR + IDXD + IDXH, D, self.gT_s, wbuf=wbuf, obuf=obuf)
        self.proj_cols(hnT, W["w_in"], 0, 896, c_s, wbuf=wbuf, obuf=obuf)
        wiw = kb.sb("wiw", [128, NC_, 16], BF16)
        self.load_w_cols(wiw, W["w_in"], QR + KVR + IDXD, 16, NC_)
        pw = self.psb[5]
        for tk in range(NTK):
            for c in range(NC_):
                kb.op("pe", lambda: nc.tensor.matmul(pw[:, tk * 16:(tk + 1) * 16], lhsT=hnT[:, c, tk * 128:(tk + 1) * 128], rhs=wiw[:, c, :], start=(c == 0), stop=(c == NC_ - 1)),
                      reads=[hnT, wiw], writes=[pw], partial=not (tk == 0 and c == 0), inc=(c == NC_ - 1 and tk == NTK - 1))
        kb.op("act", lambda: nc.scalar.activation(out=absw[:].rearrange("p k h -> p (k h)"), in_=pw[:, 0:NTK * 16], func=AF.Abs, scale=1.0 / 32.0),
              reads=[pw], writes=[absw])
        kb.op("act", lambda: nc.scalar.activation(out=sgn[:].rearrange("p k h -> p (k h)"), in_=pw[:, 0:NTK * 16], func=AF.Sign), reads=[pw], writes=[sgn])
        kb.pop()
        kb.push()
        wq = kb.sb("wq", [128, 4, D], BF16)
        kb.dma("pool", wq[:], W["w_q_up"].rearrange("(c p) n -> p c n", p=128), writes=[wq], sem_of=wq, partial=False)
        wiq = kb.sb("wiq", [128, 4, 1024], BF16)
        kb.dma("pool", wiq[:], W["w_iq"].rearrange("(c p) n -> p c n", p=128), writes=[wiq], sem_of=wiq, partial=False)
        wuk = kb.sb("wuk", [128, H, KVR], BF16)
        kb.dma("pool", wuk[:], W["w_ukT"].rearrange("h d r -> d h r"), writes=[wuk], sem_of=wuk, partial=False)
        qn = kb.sb("qn", [128, 4], F32)
        kvn = kb.sb("kvn", [128, 2], F32)
        kb.dma("sp", qn[:], W["qn"], writes=[qn], sem_of=qn, partial=False)
        kb.dma("sp", kvn[:], W["kvn"], writes=[kvn], sem_of=kvn, partial=False)
        self.r_sq = [kb.sb("r_sq%d" % i, [128, 512], F32) for i in range(2)]
        self.r_ln = kb.sb("r_ln", [128, 512], F32)
        self.r_rstd = kb.sb("r_rstd", [128, 512], F32)
        cin = [kb.sb("cin%d" % i, [128, 6, 512], BF16) for i in range(2)]
        cqn = [kb.sb("cqn%d" % i, [128, 4, 512], BF16) for i in range(2)]
        qh = [kb.sb("qh%d" % i, [128, 512], BF16) for i in range(2)]
        qst = [kb.sb("qst%d" % i, [128, 2, 512], BF16) for i in range(2)]
        ist = [kb.sb("ist%d" % i, [128, 512], BF16) for i in range(2)]
        tst = kb.sb("tst", [128, 1024], BF16)
        qa_view = self.qa_s.t.rearrange("(h r p) t -> p h r t", r=2, p=128)
        c_view = c_s.t.rearrange("(c p) t -> p c t", p=128)
        for tb in range(NTB):
            ts = slice(tb * 512, (tb + 1) * 512)
            ci = cin[tb % 2]; cn = cqn[tb % 2]
            kb.dma("sp", ci[:], c_view[:, 0:6, ts], reads=[c_s], writes=[ci], sem_of=ci, partial=False)
            self.rms_block(ci, 4, qn, lambda c: (cn[:, c, :], cn), QR)
            ckv_src = T(ci.t[:, 4:6, :], "x"); ckv_src.dep = ci.dep
            self.rms_block(ckv_src, 2, kvn, lambda c: (ckvnT[:, c, ts], ckvnT), KVR)
            for q4 in range(4):
                tk = tb * 4 + q4
                for rc in range(2):
                    kb.op("pe", lambda: nc.tensor.transpose(out=self.pst[:, (q4 * 2 + rc) * 128:(q4 * 2 + rc + 1) * 128], in_=ckvnT[:, rc, tk * 128:(tk + 1) * 128], identity=self.ident_bf[:]),
                          reads=[ckvnT, self.ident_bf], writes=[self.pst], partial=not (q4 == 0 and rc == 0), inc=(q4 == 3 and rc == 1))
            kb.op("dve", lambda: nc.vector.tensor_copy(out=ckvtok[:, tb * 4:(tb + 1) * 4, :].rearrange("p k r -> p (k r)"), in_=self.pst[:, :]), reads=[self.pst], writes=[ckvtok], partial=True)
            for h in range(H):
                ps = self.mmps()
                for rc in range(4):
                    kb.op("pe", lambda: nc.tensor.matmul(ps[:], lhsT=wq[:, rc, h * 128:(h + 1) * 128], rhs=cn[:, rc, :], start=(rc == 0), stop=(rc == 3)),
                          reads=[wq, cn], writes=[ps], partial=(rc > 0), inc=(rc == 3))
                qt = qh[h % 2]
                self.evac(qt[:], ps[:], DH ** -0.5, [ps], qt, partial=False)
                st = qst[h % 2]
                for r2 in range(2):
                    ps2 = self.mmps()
                    kb.op("pe", lambda: nc.tensor.matmul(ps2[:], lhsT=wuk[:, h, r2 * 128:(r2 + 1) * 128], rhs=qt[:], start=True, stop=True), reads=[wuk, qt], writes=[ps2])
                    self.evac(st[:, r2, :], ps2[:], 1.0, [ps2], st, partial=(r2 > 0))
                kb.dma("sp", qa_view[:, h, :, ts], st[:], reads=[st], writes=[self.qa_s], sem_of=st)
            for m in range(8):
                ps = self.mmps()
                for rc in range(4):
                    kb.op("pe", lambda: nc.tensor.matmul(ps[:], lhsT=wiq[:, rc, m * 128:(m + 1) * 128], rhs=cn[:, rc, :], start=(rc == 0), stop=(rc == 3)),
                          reads=[wiq, cn], writes=[ps], partial=(rc > 0), inc=(rc == 3))
                it = ist[m % 2]
                self.evac(it[:], ps[:], 1.0, [ps], it, partial=False)
                kb.dma("sp", iq_s[m * 128:(m + 1) * 128, ts], it[:], reads=[it], writes=[iq_s], sem_of=it)
        kb.pop()
        kb.push()
        ikT = kb.sb("ikT", [64, S], BF16)
        kb.dma("sp", ikT[:], c_s[768:832, :], reads=[c_s], writes=[ikT], sem_of=ikT, partial=False)
        iqb = [kb.sb("iqb%d" % i, [64, 16, 128], BF16) for i in range(2)]
        accb = [kb.sb("acc%d" % i, [128, S], F32) for i in range(2)]
        junk = kb.sb("junk", [128, S], BF16)
        mkb = [kb.sb("mk%d" % i, [128, S], BF16) for i in range(2)]
        rb = [kb.sb("rb%d" % i, [128, 512], BF16) for i in range(4)]
        Dgb = [kb.sb("Dg%d" % i, [128, IDXH, 128], BF16) for i in range(2)]
        Mx = kb.sb("Mx", [128, 1], F32)
        stepv = kb.sb("stepv", [128, KIT + 1], F32)
        mid = kb.sb("mid", [128, KIT + 1], F32)
        cnt = kb.sb("cnt", [128, KIT + 1], F32)
        uu = kb.sb("uu", [128, 1], F32)
        thr = kb.sb("thr", [128, 1], F32)
        mst = [kb.sb("mst%d" % i, [128, NTK, 128], BF16) for i in range(2)]
        L01 = kb.sb("L01", [128, 128], BF16)
        kb.op("dve", lambda: nc.vector.tensor_copy(out=L01[:], in_=self.consts[:, 4, :]), reads=[self.consts], writes=[L01])
        iq_view = iq_s.t[0:1024, :].rearrange("(h d) t -> d h t", d=64)
        m_view = self.mT_s.t.rearrange("(c p) t -> p c t", p=128)
        kr = 0
        ka = 0
        paccb = [self.psb[2], self.psb[3]]

        def idx_acc(h, pacc, wd, r_, Dg, acc, ss):
            kb.op("pe", lambda: nc.tensor.matmul(pacc[:, 0:wd], lhsT=Dg[:, h, :], rhs=r_[:, 0:wd], start=(h == 0), stop=(h == IDXH - 1)),
                  reads=[Dg, r_], writes=[pacc], partial=(h > 0), inc=True)
            if h == IDXH - 1:
                self.evac(acc[:, ss], pacc[:, 0:wd], 1.0, [pacc], acc)

        def emit_mask(qb):
            mk = mkb[qb % 2]
            tq = slice(qb * 128, (qb + 1) * 128)
            ms = mst[qb % 2]
            for c0 in range(0, qb + 1, 8):
                ncb = min(8, qb + 1 - c0)
                for c in range(ncb):
                    kb.op("pe", lambda: nc.tensor.transpose(out=self.pst[:, c * 128:(c + 1) * 128], in_=mk[:, (c0 + c) * 128:(c0 + c + 1) * 128], identity=self.ident_bf[:]),
                          reads=[mk, self.ident_bf], writes=[self.pst], partial=(c > 0), inc=(c == ncb - 1))
                kb.op("act", lambda: nc.scalar.activation(out=ms[:, c0:c0 + ncb, :].rearrange("p c t -> p (c t)"), in_=self.pst[:, 0:ncb * 128], func=AF.Copy), reads=[self.pst], writes=[ms], partial=(c0 > 0))
            kb.dma("sp", m_view[:, 0:qb + 1, tq], ms[:, 0:qb + 1, :], reads=[ms], writes=[self.mT_s], sem_of=ms)

        for qb in range(NTK):
            n = (qb + 1) * 128
            tq = slice(qb * 128, (qb + 1) * 128)
            mk = mkb[qb % 2]
            if n > TOPK:
                acc = accb[qb % 2]
                Dg = Dgb[qb % 2]
                iqt = iqb[qb % 2]
                kb.dma("sp", iqt[:], iq_view[:, :, tq], reads=[iq_s], writes=[iqt], sem_of=iqt, partial=False)
                for h in range(IDXH):
                    kb.op("pool", lambda: nc.gpsimd.tensor_scalar(out=Dg[:, h, :], in0=self.ident_bf[:], scalar1=sgn[:, qb, h:h + 1], scalar2=1.0, op0=ALU.mult, op1=ALU.mult),
                          reads=[self.ident_bf, sgn], writes=[Dg], partial=(h > 0))
                pend = []
                for sb_ in range((n + 511) // 512):
                    wd = min(512, n - sb_ * 512)
                    ss = slice(sb_ * 512, sb_ * 512 + wd)
                    pacc = paccb[ka % 2]; ka += 1
                    for h in range(IDXH):
                        ps = self.mmps()
                        kb.op("pe", lambda: nc.tensor.matmul(ps[:, 0:wd], lhsT=iqt[:, h, :], rhs=ikT[:, ss], start=True, stop=True), reads=[iqt, ikT], writes=[ps])
                        r_ = rb[kr % 4]; kr += 1
                        kb.op("act", lambda: nc.scalar.activation(out=r_[:, 0:wd], in_=ps[:, 0:wd], func=AF.Relu, scale=absw[:, qb, h:h + 1]), reads=[ps, absw], writes=[r_])
                        pend.append((h, pacc, wd, r_, Dg, acc, ss))
                        if len(pend) > 1:
                            idx_acc(*pend.pop(0))
                while pend:
                    idx_acc(*pend.pop(0))
                kb.op("dve", lambda: nc.vector.tensor_reduce(out=Mx[:], in_=acc[:, 0:n], axis=AX.X, op=ALU.max, apply_absolute_value=True), reads=[acc], writes=[Mx])
                kb.op("dve", lambda: nc.vector.tensor_tensor(out=acc[:, tq], in0=acc[:, tq], in1=self.consts[:, 5, :], op=ALU.add), reads=[acc, self.consts], writes=[acc])
                kb.op("dve", lambda: nc.vector.tensor_scalar(out=stepv[:], in0=self.pow2[:, 0:KIT + 1], scalar1=Mx[:, 0:1], scalar2=None, op0=ALU.mult), reads=[self.pow2, Mx], writes=[stepv])
                kb.op("dve", lambda: nc.vector.memset(mid[:, 0:1], 0.0), writes=[mid])
                for k in range(KIT):
                    kb.op("dve", lambda: nc.vector.tensor_scalar(out=junk[:, 0:n], in0=acc[:, 0:n], scalar1=mid[:, k:k + 1], scalar2=None, op0=ALU.is_ge, op1=ALU.add, accum_out=cnt[:, k:k + 1]),
                          reads=[acc, mid], writes=[junk, cnt])
                    kb.op("dve", lambda: nc.vector.tensor_scalar(out=uu[:], in0=cnt[:, k:k + 1], scalar1=TOPK - 0.5, scalar2=stepv[:, k:k + 1], op0=ALU.is_ge, op1=ALU.mult),
                          reads=[cnt, stepv], writes=[uu])
                    kb.op("dve", lambda: nc.vector.scalar_tensor_tensor(out=mid[:, k + 1:k + 2], in0=uu[:], scalar=stepv[:, k + 1:k + 2], in1=mid[:, k:k + 1], op0=ALU.subtract, op1=ALU.add),
                          reads=[uu, stepv, mid], writes=[mid])
                kb.op("dve", lambda: nc.vector.tensor_tensor(out=thr[:], in0=mid[:, KIT:KIT + 1], in1=stepv[:, KIT:KIT + 1], op=ALU.subtract), reads=[mid, stepv], writes=[thr])
                kb.op("dve", lambda: nc.vector.tensor_scalar(out=mk[:, 0:n], in0=acc[:, 0:n], scalar1=thr[:, 0:1], scalar2=None, op0=ALU.is_ge), reads=[acc, thr], writes=[mk])
            else:
                if qb > 0:
                    kb.op("dve", lambda: nc.vector.memset(mk[:, 0:qb * 128], 1.0), writes=[mk])
                kb.op("dve", lambda: nc.vector.tensor_copy(out=mk[:, tq], in_=L01[:]), reads=[L01], writes=[mk], partial=(qb > 0))
            if qb >= 1:
                emit_mask(qb - 1)
        emit_mask(NTK - 1)
        kb.pop()
        kb.push()
        wo = kb.sb("wo", [128, H, D], BF16)
        for h4 in range(4):
            src = W["w_out"].rearrange("(c p) n -> p c n", p=128)[:, h4 * 4:(h4 + 1) * 4, :]
            kb.dma("pool", wo[:, h4 * 4:(h4 + 1) * 4, :], src, writes=[wo], sem_of=wo, partial=True)
        wuv = kb.sb("wuv", [128, 2, D], BF16)
        kb.dma("pool", wuv[:], W["w_uv"].rearrange("(c p) n -> p c n", p=128), writes=[wuv], sem_of=wuv, partial=False)
        b31 = kb.sb("b31", [128, 16], F32)
        kb.dma("sp", b31[:], W["rb31"], writes=[b31], sem_of=b31, partial=False)
        Dt = kb.sb("Dt", [128, H, 2, 128], BF16)
        dtmp = kb.sb("dtmp", [128, H, 2, 128], F32)
        kb.dma("sp", dtmp[:], W["rbD"], writes=[dtmp], sem_of=dtmp, partial=False)
        for h in range(H):
            kb.op("dve", lambda: nc.vector.tensor_scalar(out=Dt[:, h, :, :], in0=dtmp[:, h, :, :], scalar1=b31[:, h:h + 1], scalar2=None, op0=ALU.subtract),
                  reads=[dtmp, b31], writes=[Dt], partial=(h > 0))
        mT = kb.sb("mT", [128, NTK, 512], BF16)
        qab = [kb.sb("qab%d" % i, [128, 2, 512], BF16) for i in range(2)]
        gbuf = [kb.sb("gbuf%d" % i, [128, 512], BF16) for i in range(2)]
        pbuf = [kb.sb("pbuf%d" % i, [128, 512], BF16) for i in range(5)]
        ol = kb.sb("ol", [128, 2, 512], BF16)
        rd = kb.sb("rd", [128, 512], F32)
        sg = kb.sb("sg", [128, 512], F32)
        og = kb.sb("og", [128, H, 512], BF16)
        hres = [kb.sb("hres%d" % i, [128, 512], F32) for i in range(2)]
        O0, O1, Dn, psU, psY = self.psb[2], self.psb[3], self.psb[4], self.psb[6], self.psb[6]
        stb = [self.psb[0], self.psb[1], self.psb[5]]
        kp = 0
        pend = []
        LOOK = 2

        def dsa_pv(h, i, nch, c0, pt, gt):
            hs = slice(h * 128, (h + 1) * 128)
            kb.op("pe", lambda: nc.tensor.matmul(O0[:, c0:512], lhsT=ckvtok[:, i, 0:128], rhs=pt[:, c0:512], start=(i == 0), stop=(i == nch - 1)),
                  reads=[ckvtok, pt], writes=[O0], partial=(i > 0), inc=False)
            kb.op("pe", lambda: nc.tensor.matmul(O1[:, c0:512], lhsT=ckvtok[:, i, 128:256], rhs=pt[:, c0:512], start=(i == 0), stop=(i == nch - 1)),
                  reads=[ckvtok, pt], writes=[O1], partial=(i > 0), inc=False)
            kb.op("pe", lambda: nc.tensor.matmul(Dn[:, c0:512], lhsT=self.ones_bf[:], rhs=pt[:, c0:512], start=(i == 0), stop=(i == nch - 1)),
                  reads=[self.ones_bf, pt], writes=[Dn], partial=(i > 0), inc=True)
            if i == nch - 1:
                kb.op("act", lambda: nc.scalar.activation(out=ol[:, 0, :], in_=O0[:], func=AF.Copy), reads=[O0], writes=[ol])
                kb.op("dve", lambda: nc.vector.tensor_copy(out=ol[:, 1, :], in_=O1[:]), reads=[O1], writes=[ol], partial=True)
                kb.op("dve", lambda: nc.vector.reciprocal(out=rd[:], in_=Dn[:]), reads=[Dn], writes=[rd])
                kb.op("pe", lambda: nc.tensor.matmul(psU[:], lhsT=wuv[:, 0, hs], rhs=ol[:, 0, :], start=True, stop=False), reads=[wuv, ol], writes=[psU], inc=False)
                kb.op("pe", lambda: nc.tensor.matmul(psU[:], lhsT=wuv[:, 1, hs], rhs=ol[:, 1, :], start=False, stop=True), reads=[wuv, ol], writes=[psU], partial=True)
                kb.op("act", lambda: nc.scalar.activation(out=sg[:], in_=gt[:], func=AF.Silu), reads=[gt], writes=[sg])
                kb.op("dve", lambda: nc.vector.tensor_tensor(out=sg[:], in0=sg[:], in1=rd[:], op=ALU.mult), reads=[sg, rd], writes=[sg])
                kb.op("dve", lambda: nc.vector.tensor_tensor(out=og[:, h, :], in0=psU[:], in1=sg[:], op=ALU.mult), reads=[psU, sg], writes=[og], partial=True)

        for tb in range(NTB):
            ts = slice(tb * 512, (tb + 1) * 512)
            nch = 4 * (tb + 1)
            kb.dma("sp", mT[:, 0:nch, :], m_view[:, 0:nch, ts], reads=[self.mT_s], writes=[mT], sem_of=mT, partial=False)
            for h in range(H):
                hs = slice(h * 128, (h + 1) * 128)
                qa = qab[h % 2]; gt = gbuf[h % 2]
                kb.dma("sp", qa[:], qa_view[:, h, :, ts], reads=[self.qa_s], writes=[qa], sem_of=qa, partial=False)
                kb.dma("sp", gt[:], self.gT_s[hs, ts], reads=[self.gT_s], writes=[gt], sem_of=gt, partial=False)
                for i in range(nch):
                    a = i - 4 * tb
                    c0 = max(a, 0) * 128
                    extra = [(c, 4 * tb + c - i) for c in range(4) if (4 * tb + c - i) in (0, 1) and c * 128 >= c0]
                    ps = stb[kp % 3]
                    kb.op("pe", lambda: nc.tensor.matmul(ps[:, c0:512], lhsT=ckvnT[:, 0, i * 128:(i + 1) * 128], rhs=qa[:, 0, c0:512], start=True, stop=False),
                          reads=[ckvnT, qa], writes=[ps], inc=False)
                    kb.op("pe", lambda: nc.tensor.matmul(ps[:, c0:512], lhsT=ckvnT[:, 1, i * 128:(i + 1) * 128], rhs=qa[:, 1, c0:512], start=False, stop=(len(extra) == 0)),
                          reads=[ckvnT, qa], writes=[ps], partial=True, inc=(len(extra) == 0))
                    for ei, (c, df) in enumerate(extra):
                        kb.op("pe", lambda: nc.tensor.matmul(ps[:, c * 128:(c + 1) * 128], lhsT=self.ident_bf[:], rhs=Dt[:, h, df, :], start=False, stop=(ei == len(extra) - 1)),
                              reads=[self.ident_bf, Dt], writes=[ps], partial=True, inc=(ei == len(extra) - 1))
                    pt = pbuf[kp % 5]; kp += 1
                    kb.op("act", lambda: nc.scalar.activation(out=pt[:, c0:512], in_=ps[:, c0:512], func=AF.Exp, bias=b31[:, h:h + 1], scale=1.0),
                          reads=[ps, b31], writes=[pt])
                    if kp % 2 == 0:
                        kb.op("dve", lambda: nc.vector.tensor_tensor(out=pt[:, c0:512], in0=pt[:, c0:512], in1=mT[:, i, c0:512], op=ALU.mult), reads=[pt, mT], writes=[pt])
                    else:
                        kb.op("pool", lambda: nc.gpsimd.tensor_tensor(out=pt[:, c0:512], in0=pt[:, c0:512], in1=mT[:, i, c0:512], op=ALU.mult), reads=[pt, mT], writes=[pt])
                    pend.append((h, i, nch, c0, pt, gt))
                    if len(pend) > LOOK:
                        dsa_pv(*pend.pop(0))
            while pend:
                dsa_pv(*pend.pop(0))
            self.wout_block(tb, og, wo, hsrc, hdst, hres, psY)
        kb.pop()
        kb.pop()


def make_consts():
    c = np.zeros((128, 6, 128), np.float32)
    pp = np.arange(128)[:, None]; jj = np.arange(128)[None, :]
    c[:, 4, :] = np.where(jj <= pp, 1.0, 0.0)
    c[:, 5, :] = np.where(jj <= pp, 0.0, -1e30)
    c[:, 0, :] = np.eye(128, dtype=np.float32)
    sp = np.arange(128)[:, None]; tp = np.arange(128)[None, :]
    c[:, 1, :] = np.where(sp <= tp, 0.0, NEG)
    c[127, 2, :] = 1.0
    c[:, 3, :] = 1.0
    return c


def cols16(v):
    return np.ascontiguousarray(v.reshape(16, 128).T)


def t5_bucket_np(dist):
    import math
    max_exact = 16
    d = np.maximum(dist, 0)
    df = np.maximum(d, 1).astype(np.float32)
    large = max_exact + (np.log(df / max_exact) / math.log(128 / max_exact) * (32 - max_exact)).astype(np.int32)
    large = np.minimum(large, 31)
    return np.where(d < max_exact, d, large)


def shared_inputs(p, layers, do_final=True):
    im = {"consts": make_consts()}
    g = np.zeros((128, 5, 16), np.float32)
    for i in range(4):
        g[:, i, :] = cols16(p["norm_g"][i])
    g[:, 4, :] = cols16(p["final_g"])
    im["gcols"] = g
    im["pow2"] = np.ascontiguousarray(np.broadcast_to((2.0 ** -np.arange(17, dtype=np.float64)).astype(np.float32), (128, 17)))
    sp = np.arange(128)[:, None]; tp = np.arange(128)[None, :]
    bk = np.stack([t5_bucket_np(tp - sp), t5_bucket_np(128 + tp - sp)], 0)
    for kind, li in layers:
        j = li // 2
        if kind == "b":
            im["w_in%d" % li] = np.ascontiguousarray(p["b_w_in"][j])
            im["f_bias%d" % li] = np.ascontiguousarray(p["b_f_bias"][j].reshape(16, 1))
            im["w_out%d" % li] = np.ascontiguousarray(p["b_w_out"][j])
        else:
            im["w_in%d" % li] = np.ascontiguousarray(p["a_w_in"][j])
            im["qn%d" % li] = np.ascontiguousarray(p["a_q_norm"][j].reshape(4, 128).T)
            im["kvn%d" % li] = np.ascontiguousarray(p["a_kv_norm"][j].reshape(2, 128).T)
            im["w_q_up%d" % li] = np.ascontiguousarray(p["a_w_q_up"][j])
            im["w_ukT%d" % li] = np.ascontiguousarray(np.transpose(p["a_w_uk"][j], (1, 2, 0)))
            im["w_uv%d" % li] = np.ascontiguousarray(p["a_w_uv"][j].reshape(KVR, D))
            im["w_iq%d" % li] = np.ascontiguousarray(p["a_w_iq"][j])
            im["w_out%d" % li] = np.ascontiguousarray(p["a_w_out"][j])
            rb = p["rel_bias"]
            im["rb31_%d" % li] = np.ascontiguousarray(np.broadcast_to(rb[31][None, :], (128, 16)))
            gath = rb[bk]
            im["rbD_%d" % li] = np.ascontiguousarray(np.transpose(gath, (1, 3, 0, 2)))
    return im


_PROG_CACHE = {}


def get_prog(S, layers, do_final):
    key = (S, tuple(layers), do_final)
    if key not in _PROG_CACHE:
        _PROG_CACHE[key] = Prog(S, list(layers), do_final=do_final)
    return _PROG_CACHE[key]


LAYERS = [("a", 0), ("b", 1), ("a", 2), ("b", 3)]
FUSED = True


def run_layers(xT_list, p, layers, do_final):
    S = xT_list[0].shape[1]
    prog = get_prog(S, layers, do_final)
    sh = shared_inputs(p, layers, do_final)
    in_maps = []
    for xT in xT_list:
        m = dict(sh)
        m["xT"] = xT
        in_maps.append(m)
    res = run_bass_kernel_spmd(prog.nc, in_maps, core_ids=list(range(len(xT_list))))
    key = "outT" if do_final else "hT"
    return [r[key] for r in res.results]


def kernel(**inputs):
    p = {k: np.asarray(v) for k, v in inputs.items()}
    x = p["x"]
    B = x.shape[0]
    xT = [np.ascontiguousarray(x[b].T) for b in range(B)]
    if FUSED:
        outT = run_layers(xT, p, LAYERS, True)
    else:
        cur = xT
        for n, lay in enumerate(LAYERS):
            cur = run_layers(cur, p, [lay], n == len(LAYERS) - 1)
        outT = cur
    return np.stack([np.ascontiguousarray(o.T) for o in outT], 0).astype(np.float32)
```

```python
import numpy as np
import concourse.bass as bass
import concourse.mybir as mybir
from concourse.bass_utils import run_bass_kernel_spmd

F32 = mybir.dt.float32
BF16 = mybir.dt.bfloat16
AF = mybir.ActivationFunctionType
ALU = mybir.AluOpType
AX = mybir.AxisListType

D = 2048
H = 16
DH = 128
NC_ = 16
QR = 512
KVR = 256
IDXH = 16
IDXD = 64
A_IN = QR + KVR + IDXD + IDXH + D
B_IN = 3 * D + H + D
EPS = 1e-6
NEG = -30000.0


class Dep:
    __slots__ = ("name", "w", "r", "dsem", "dcnt")

    def __init__(self, name):
        self.name = name
        self.w = {}
        self.r = {}
        self.dsem = None


class T:
    def __init__(self, t, name):
        self.t = t
        self.dep = Dep(name)

    def __getitem__(self, idx):
        return self.t[idx]


def _dep(d):
    return d.dep if isinstance(d, T) else d


class KB:
    NDMA = 48

    def __init__(self, nc):
        self.nc = nc
        self.engs = {"pe": nc.tensor, "act": nc.scalar, "dve": nc.vector,
                     "pool": nc.gpsimd, "sp": nc.sync}
        self._stack = [[]]
        self.semobj = {}
        self.semval = {}
        self.sems = {}
        for e in self.engs:
            key = "e_" + e
            self.semobj[key] = self._enter(nc.semaphore("s_" + e))
            self.semval[key] = 0
            self.sems[e] = key
        self.dfree = []
        for i in range(self.NDMA):
            key = "d_%d" % i
            self.semobj[key] = self._enter(nc.semaphore("sd_%d" % i))
            self.semval[key] = 0
            self.dfree.append(key)
        self.seen = {e: {} for e in self.engs}
        self.scope_deps = [[]]

    def _enter(self, cm):
        obj = cm.__enter__()
        self._stack[-1].append(cm)
        return obj

    def push(self):
        self._stack.append([])
        self.scope_deps.append([])

    def pop(self):
        self.barrier()
        for d in self.scope_deps.pop():
            if d.dsem is not None:
                self.dfree.append(d.dsem)
                d.dsem = None
        for cm in reversed(self._stack.pop()):
            cm.__exit__(None, None, None)

    def close(self):
        while len(self._stack) > 1:
            self.pop()
        for cm in reversed(self._stack[0]):
            cm.__exit__(None, None, None)

    def sb(self, name, shape, dt):
        self.uid = getattr(self, "uid", 0) + 1
        name = "%s_u%d" % (name, self.uid)
        t = T(self._enter(self.nc.sbuf_tensor(name, list(shape), dt)), name)
        self.scope_deps[-1].append(t.dep)
        return t

    def ps(self, name, shape, dt):
        t = T(self._enter(self.nc.psum_tensor(name, list(shape), dt)), name)
        self.scope_deps[-1].append(t.dep)
        return t

    def region(self, name):
        return Dep(name)

    def _wait(self, eng, key, val):
        if self.seen[eng].get(key, 0) >= val:
            return
        if key == self.sems[eng] and val <= self.semval[key] - 2:
            return
        self.engs[eng].wait_ge(self.semobj[key], val)
        self.seen[eng][key] = val

    def _deps(self, eng, reads, writes, partial):
        own = self.sems[eng]
        skip_own = (eng == "pe")
        for d in reads:
            d = _dep(d)
            for k, (v, _p) in d.w.items():
                if skip_own and k == own:
                    continue
                self._wait(eng, k, v)
        for d in writes:
            d = _dep(d)
            for k, v in d.r.items():
                if skip_own and k == own:
                    continue
                self._wait(eng, k, v)
            for k, (v, p) in d.w.items():
                if partial and p:
                    continue
                if skip_own and k == own:
                    continue
                self._wait(eng, k, v)

    def _record(self, key, val, reads, writes, partial):
        for d in reads:
            d = _dep(d)
            if d.r.get(key, 0) < val:
                d.r[key] = val
        for d in writes:
            d = _dep(d)
            if partial and not d.r and all(p for (_v, p) in d.w.values()):
                d.w[key] = (val, True)
            else:
                d.w = {key: (val, partial)}
                d.r = {}

    def op(self, eng, fn, reads=(), writes=(), partial=False, inc=True):
        self._deps(eng, reads, writes, partial)
        ins = fn()
        key = self.sems[eng]
        if inc:
            self.semval[key] += 1
            ins.then_inc(self.semobj[key], 1)
            val = self.semval[key]
        else:
            val = self.semval[key] + 1
        self._record(key, val, reads, writes, partial)
        return ins

    def dma(self, q, out, in_, reads=(), writes=(), sem_of=None, partial=True, **kw):
        self._deps(q, reads, writes, partial)
        d = _dep(sem_of)
        if d.dsem is None:
            d.dsem = self.dfree.pop()
        key = d.dsem
        self.semval[key] += 16
        ins = self.engs[q].dma_start(out=out, in_=in_, **kw)
        ins.then_inc(self.semobj[key], 16)
        self._record(key, self.semval[key], reads, writes, partial)
        return ins

    def barrier(self):
        for e in self.engs:
            for key, v in self.semval.items():
                if v > 0:
                    self._wait(e, key, v)

    def wait_all_on(self, eng):
        for key, v in self.semval.items():
            if v > 0:
                self._wait(eng, key, v)


class Prog:
    def __init__(self, S, layers, first_src_is_x=True, do_final=True):
        self.S = S
        self.NTB = S // 512
        self.NTK = S // 128
        self.layers = layers
        self.do_final = do_final
        nc = self.nc = bass.Bass("TRN2", target_bir_lowering=False)
        self.kb = KB(nc)
        kb = self.kb
        dt = nc.dram_tensor
        self.xT = T(dt("xT", [D, S], F32, kind="ExternalInput").ap(), "xT")
        if do_final:
            self.outT = T(dt("outT", [D, S], F32, kind="ExternalOutput").ap(), "outT")
        self.hT = T(dt("hT", [D, S], F32, kind="Internal" if do_final else "ExternalOutput").ap(), "hT")
        self.consts_d = dt("consts", [128, 6, 128], F32, kind="ExternalInput").ap()
        self.gcols_d = dt("gcols", [128, 5, 16], F32, kind="ExternalInput").ap()
        self.w = {}
        for kind, li in layers:
            if kind == "b":
                self.w[li] = dict(
                    w_in=dt("w_in%d" % li, [D, B_IN], F32, kind="ExternalInput").ap(),
                    f_bias=dt("f_bias%d" % li, [16, 1], F32, kind="ExternalInput").ap(),
                    w_out=dt("w_out%d" % li, [D, D], F32, kind="ExternalInput").ap(),
                )
            else:
                self.w[li] = dict(
                    w_in=dt("w_in%d" % li, [D, A_IN], F32, kind="ExternalInput").ap(),
                    qn=dt("qn%d" % li, [128, 4], F32, kind="ExternalInput").ap(),
                    kvn=dt("kvn%d" % li, [128, 2], F32, kind="ExternalInput").ap(),
                    w_q_up=dt("w_q_up%d" % li, [QR, D], F32, kind="ExternalInput").ap(),
                    w_ukT=dt("w_ukT%d" % li, [H, DH, KVR], F32, kind="ExternalInput").ap(),
                    w_uv=dt("w_uv%d" % li, [KVR, D], F32, kind="ExternalInput").ap(),
                    w_iq=dt("w_iq%d" % li, [QR, IDXH * IDXD], F32, kind="ExternalInput").ap(),
                    w_out=dt("w_out%d" % li, [D, D], F32, kind="ExternalInput").ap(),
                    rb31=dt("rb31_%d" % li, [128, 16], F32, kind="ExternalInput").ap(),
                    rbD=dt("rbD_%d" % li, [128, 16, 2, 128], F32, kind="ExternalInput").ap(),
                )
        self.qT_s = T(dt("qT_s", [D, S], BF16, kind="Internal").ap(), "qT_s")
        self.kT_s = T(dt("kT_s", [D, S], BF16, kind="Internal").ap(), "kT_s")
        self.v_s = T(dt("v_s", [S, D], BF16, kind="Internal").ap(), "v_s")
        self.gT_s = T(dt("gT_s", [D, S], BF16, kind="Internal").ap(), "gT_s")

        self.consts = kb.sb("consts_sb", [128, 6, 128], F32)
        self.gcols = kb.sb("gcols_sb", [128, 5, 16], F32)
        kb.dma("sp", self.consts[:], self.consts_d, writes=[self.consts], sem_of=self.consts)
        kb.dma("sp", self.gcols[:], self.gcols_d, writes=[self.gcols], sem_of=self.gcols)
        self.ident_bf = kb.sb("ident_bf", [128, 128], BF16)
        self.tri_bf = kb.sb("tri_bf", [128, 128], BF16)
        self.ones_bf = kb.sb("ones_bf", [128, 128], BF16)
        for dst, ci in ((self.ident_bf, 0), (self.tri_bf, 1), (self.ones_bf, 3)):
            kb.op("dve", lambda: nc.vector.tensor_copy(out=dst[:], in_=self.consts[:, ci, :]),
                  reads=[self.consts], writes=[dst])
        self.epscol = kb.sb("epscol", [128, 1], F32)
        kb.op("dve", lambda: nc.vector.memset(self.epscol[:], EPS), writes=[self.epscol])
        self.pow2_d = dt("pow2", [128, 17], F32, kind="ExternalInput").ap()
        self.pow2 = kb.sb("pow2_sb", [128, 17], F32)
        kb.dma("sp", self.pow2[:], self.pow2_d, writes=[self.pow2], sem_of=self.pow2)
        self.qa_s = T(dt("qa_s", [2 * D, S], BF16, kind="Internal").ap(), "qa_s")
        self.mT_s = T(dt("mT_s", [S, S], BF16, kind="Internal").ap(), "mT_s")
        self.psb = [kb.ps("psb%d" % i, [128, 512], F32) for i in range(7)]
        self.pst = kb.ps("pst", [128, 1024], BF16)
        self.k_mm = 0

        src = self.xT
        for kind, li in layers:
            if kind == "b":
                self.fox_layer(li, src, self.hT)
            else:
                self.dsa_layer(li, src, self.hT)
            src = self.hT
        if do_final:
            self.norm_phase(src, 4, final=True)
        kb.wait_all_on("sp")
        kb.close()

    def mmps(self):
        self.k_mm += 1
        return self.psb[self.k_mm % 2]

    def load_w_cols(self, wt, w_ap, col0, ncols, nchunk):
        src = w_ap.rearrange("(c p) n -> p c n", p=128)[:, :, col0:col0 + ncols]
        self.kb.dma("pool", wt[:, 0:nchunk, 0:ncols], src, writes=[wt], sem_of=wt, partial=False)

    def norm_phase(self, hsrc, gi, final=False, hnT=None):
        kb, nc, S = self.kb, self.nc, self.S
        kb.push()
        hb = [kb.sb("n_hb%d" % i, [128, 512], F32) for i in range(3)]
        sq = [kb.sb("n_sq%d" % i, [128, 512], F32) for i in range(2)]
        lnv = kb.sb("n_lnv", [128, 512], F32)
        rstd = kb.sb("n_rstd", [128, 512], F32)
        ob = [kb.sb("n_ob%d" % i, [128, 512], F32) for i in range(2)] if final else None
        ones_f = self.consts
        pstat = self.psb[6]
        k = 0
        for tb in range(self.NTB):
            ts = slice(tb * 512, (tb + 1) * 512)
            for c in range(NC_):
                h = hb[k % 3]; s = sq[k % 2]; k += 1
                kb.dma("sp", h[:], hsrc[c * 128:(c + 1) * 128, ts], reads=[hsrc], writes=[h], sem_of=h, partial=False)
                kb.op("act", lambda: nc.scalar.activation(out=s[:], in_=h[:], func=AF.Square), reads=[h], writes=[s])
                kb.op("pe", lambda: nc.tensor.matmul(pstat[:], lhsT=ones_f[:, 3, :], rhs=s[:], start=(c == 0), stop=(c == NC_ - 1)),
                      reads=[s, ones_f], writes=[pstat], partial=(c > 0), inc=True)
            kb.op("act", lambda: nc.scalar.activation(out=lnv[:], in_=pstat[:], func=AF.Ln, scale=1.0 / D, bias=self.epscol[:]),
                  reads=[pstat, self.epscol], writes=[lnv])
            kb.op("act", lambda: nc.scalar.activation(out=rstd[:], in_=lnv[:], func=AF.Exp, scale=-0.5), reads=[lnv], writes=[rstd])
            for c in range(NC_):
                h = hb[k % 3]; k += 1
                kb.dma("sp", h[:], hsrc[c * 128:(c + 1) * 128, ts], reads=[hsrc], writes=[h], sem_of=h, partial=False)
                if final:
                    o = ob[c % 2]
                    kb.op("dve", lambda: nc.vector.scalar_tensor_tensor(out=o[:], in0=h[:], scalar=self.gcols[:, gi, c:c + 1], in1=rstd[:], op0=ALU.mult, op1=ALU.mult),
                          reads=[h, rstd, self.gcols], writes=[o])
                    kb.dma("sp", self.outT[c * 128:(c + 1) * 128, ts], o[:], reads=[o], writes=[self.outT], sem_of=o)
                else:
                    kb.op("dve", lambda: nc.vector.scalar_tensor_tensor(out=hnT[:, c, ts], in0=h[:], scalar=self.gcols[:, gi, c:c + 1], in1=rstd[:], op0=ALU.mult, op1=ALU.mult),
                          reads=[h, rstd, self.gcols], writes=[hnT], partial=True)
        kb.pop()

    def proj_cols(self, hnT, w_ap, col0, ncols, dst, scale=1.0, token_major=False, wbuf=None, obuf=None):
        kb, nc, S = self.kb, self.nc, self.S
        for cc in range(ncols // 128):
            wt = wbuf[cc % 2]
            ob = obuf[cc % 2]
            self.load_w_cols(wt, w_ap, col0 + cc * 128, 128, NC_)
            if not token_major:
                for tb in range(self.NTB):
                    ts = slice(tb * 512, (tb + 1) * 512)
                    ps = self.mmps()
                    for c in range(NC_):
                        kb.op("pe", lambda: nc.tensor.matmul(ps[:], lhsT=wt[:, c, :], rhs=hnT[:, c, ts], start=(c == 0), stop=(c == NC_ - 1)),
                              reads=[wt, hnT], writes=[ps], partial=(c > 0), inc=(c == NC_ - 1))
                    self.evac(ob[:, ts], ps[:], scale, [ps], ob)
                kb.dma("sp", dst[cc * 128:(cc + 1) * 128, :], ob[:], reads=[ob], writes=[dst], sem_of=ob)
            else:
                for tq in range(self.NTB):
                    ts = slice(tq * 512, (tq + 1) * 512)
                    ps = self.mmps()
                    for c in range(NC_):
                        kb.op("pe", lambda: nc.tensor.matmul(ps[:], lhsT=wt[:, c, :], rhs=hnT[:, c, ts], start=(c == 0), stop=(c == NC_ - 1)),
                              reads=[wt, hnT], writes=[ps], partial=(c > 0), inc=(c == NC_ - 1))
                    vt = self.vtb[tq % 2]
                    self.evac(vt[:], ps[:], scale, [ps], vt, partial=False)
                    for q in range(4):
                        kb.op("pe", lambda: nc.tensor.transpose(out=self.pst[:, q * 128:(q + 1) * 128], in_=vt[:, q * 128:(q + 1) * 128], identity=self.ident_bf[:]),
                              reads=[vt, self.ident_bf], writes=[self.pst], partial=(q > 0), inc=(q == 3))
                    kb.op("dve", lambda: nc.vector.tensor_copy(out=ob[:, ts], in_=self.pst[:, 0:512]), reads=[self.pst], writes=[ob], partial=True)
                dv = dst.t.rearrange("(k p) n -> p k n", p=128)[:, :, cc * 128:(cc + 1) * 128]
                kb.dma("sp", dv, ob[:].rearrange("p (k n) -> p k n", n=128), reads=[ob], writes=[dst], sem_of=ob)

    def evac(self, out_ap, in_ap, scale, reads, wtile, partial=True):
        kb, nc = self.kb, self.nc
        self.k_ev = getattr(self, "k_ev", 0) + 1
        if self.k_ev % 2 == 0:
            kb.op("act", lambda: nc.scalar.activation(out=out_ap, in_=in_ap, func=AF.Copy, scale=float(scale)), reads=reads, writes=[wtile], partial=partial)
        else:
            kb.op("dve", lambda: nc.vector.tensor_scalar(out=out_ap, in0=in_ap, scalar1=float(scale), scalar2=None, op0=ALU.mult), reads=reads, writes=[wtile], partial=partial)

    def wout_block(self, tb, og, wo, hsrc, hdst, hres, psY):
        kb, nc = self.kb, self.nc
        ts = slice(tb * 512, (tb + 1) * 512)
        for dc in range(NC_):
            hr = hres[dc % 2]
            kb.dma("sp", hr[:], hsrc[dc * 128:(dc + 1) * 128, ts], reads=[hsrc], writes=[hr], sem_of=hr, partial=False)
            for h in range(H):
                kb.op("pe", lambda: nc.tensor.matmul(psY[:], lhsT=wo[:, h, dc * 128:(dc + 1) * 128], rhs=og[:, h, :], start=(h == 0), stop=(h == H - 1)),
                      reads=[wo, og], writes=[psY], partial=(h > 0), inc=(h == H - 1))
            kb.op("dve", lambda: nc.vector.tensor_tensor(out=hr[:], in0=psY[:], in1=hr[:], op=ALU.add), reads=[psY, hr], writes=[hr])
            kb.dma("sp", hdst[dc * 128:(dc + 1) * 128, ts], hr[:], reads=[hr], writes=[hdst], sem_of=hr)

    def fox_layer(self, li, hsrc, hdst):
        kb, nc, S = self.kb, self.nc, self.S
        W = self.w[li]
        NTB, NTK = self.NTB, self.NTK
        kb.push()
        csT = kb.sb("csT", [128, NTK, 16], F32)
        csl = kb.sb("csl", [128, NTK, 16], F32)
        kb.push()
        hnT = kb.sb("hnT", [128, NC_, S], BF16)
        self.norm_phase(hsrc, li, hnT=hnT)
        kb.push()
        wbuf = [kb.sb("wbuf%d" % i, [128, NC_, 128], BF16) for i in range(2)]
        obuf = [kb.sb("obuf%d" % i, [128, S], BF16) for i in range(2)]
        self.vtb = [kb.sb("vtb%d" % i, [128, 512], BF16) for i in range(2)]
        self.proj_cols(hnT, W["w_in"], 0, D, self.qT_s, scale=DH ** -0.5, wbuf=wbuf, obuf=obuf)
        self.proj_cols(hnT, W["w_in"], D, D, self.kT_s, wbuf=wbuf, obuf=obuf)
        self.proj_cols(hnT, W["w_in"], 3 * D + H, D, self.gT_s, wbuf=wbuf, obuf=obuf)
        self.proj_cols(hnT, W["w_in"], 2 * D, D, self.v_s, token_major=True, wbuf=wbuf, obuf=obuf)
        kb.pop()
        wf = kb.sb("wf", [128, NC_, 16], BF16)
        self.load_w_cols(wf, W["w_in"], 3 * D, 16, NC_)
        fb = kb.sb("fb", [16, 1], F32)
        kb.dma("sp", fb[:], W["f_bias"], writes=[fb], sem_of=fb, partial=False)
        nfb = kb.sb("nfb", [16, 1], F32)
        kb.op("dve", lambda: nc.vector.tensor_scalar(out=nfb[:], in0=fb[:], scalar1=-1.0, scalar2=None, op0=ALU.mult), reads=[fb], writes=[nfb])
        lf = kb.sb("lf", [16, S], F32)
        cs = kb.sb("cs", [16, S], F32)
        onesr = kb.sb("onesr", [16, S], F32)
        kb.op("dve", lambda: nc.vector.memset(onesr[:], 1.0), writes=[onesr])
        for tb in range(NTB):
            ts = slice(tb * 512, (tb + 1) * 512)
            ps = self.mmps()
            for c in range(NC_):
                kb.op("pe", lambda: nc.tensor.matmul(ps[0:16, :], lhsT=wf[:, c, :], rhs=hnT[:, c, ts], start=(c == 0), stop=(c == NC_ - 1)),
                      reads=[wf, hnT], writes=[ps], partial=(c > 0), inc=(c == NC_ - 1))
            kb.op("act", lambda: nc.scalar.activation(out=lf[:, ts], in_=ps[0:16, :], func=AF.Exp, scale=-1.0, bias=nfb[:]),
                  reads=[ps, nfb], writes=[lf], partial=True)
        one16 = kb.sb("one16", [16, 1], F32)
        kb.op("dve", lambda: nc.vector.memset(one16[:], 1.0), writes=[one16])
        kb.op("act", lambda: nc.scalar.activation(out=lf[:], in_=lf[:], func=AF.Ln, scale=1.0, bias=one16[:]), reads=[lf, one16], writes=[lf])
        kb.op("dve", lambda: nc.vector.tensor_tensor_scan(out=cs[:], data0=onesr[:], data1=lf[:], initial=0.0, op0=ALU.mult, op1=ALU.add),
              reads=[onesr, lf], writes=[cs])
        pT = self.psb[5]
        for tk in range(NTK):
            kb.op("pe", lambda: nc.tensor.transpose(out=pT[:, (tk % 32) * 16:(tk % 32) * 16 + 16], in_=cs[0:16, tk * 128:(tk + 1) * 128], identity=self.consts[0:16, 0, 0:16]),
                  reads=[cs, self.consts], writes=[pT], partial=(tk > 0), inc=(tk == NTK - 1))
        kb.op("dve", lambda: nc.vector.tensor_copy(out=csT[:].rearrange("p k h -> p (k h)"), in_=pT[:, 0:NTK * 16]), reads=[pT], writes=[csT])
        kb.op("pe", lambda: nc.tensor.matmul(pT[:, 0:NTK * 16], lhsT=self.consts[:, 2, :], rhs=csT[:].rearrange("p k h -> p (k h)"), start=True, stop=True),
              reads=[csT, self.consts], writes=[pT])
        kb.op("dve", lambda: nc.vector.tensor_copy(out=csl[:].rearrange("p k h -> p (k h)"), in_=pT[:, 0:NTK * 16]), reads=[pT], writes=[csl])
        kb.pop()

        kb.push()
        wo = kb.sb("wo", [128, H, D], BF16)
        for h4 in range(4):
            src = W["w_out"].rearrange("(c p) n -> p c n", p=128)[:, h4 * 4:(h4 + 1) * 4, :]
            kb.dma("pool", wo[:, h4 * 4:(h4 + 1) * 4, :], src, writes=[wo], sem_of=wo, partial=True)
        kbuf = [kb.sb("kbuf%d" % i, [128, S], BF16) for i in range(2)]
        vbuf = [kb.sb("vbuf%d" % i, [128, NTK, 128], BF16) for i in range(2)]
        qbuf = [kb.sb("qbuf%d" % i, [128, 512], BF16) for i in range(2)]
        gbuf = [kb.sb("gbuf%d" % i, [128, 512], BF16) for i in range(2)]
        pbuf = [kb.sb("pbuf%d" % i, [128, 512], BF16) for i in range(4)]
        bc = [kb.sb("bc%d" % i, [128, NTK], F32) for i in range(2)]
        rd = kb.sb("rd", [128, 512], F32)
        sg = kb.sb("sg", [128, 512], F32)
        og = kb.sb("og", [128, H, 512], BF16)
        hres = [kb.sb("hres%d" % i, [128, 512], F32) for i in range(2)]
        psO = [self.psb[2], self.psb[3]]
        psD = [self.psb[4], self.psb[5]]
        psY = self.psb[6]
        kp = 0
        v_view = self.v_s.t.rearrange("(k p) n -> p k n", p=128)
        prev = None

        def fox_pv(h, i, nch, c0, pt, vt, O, Dn, gt):
            kb.op("pe", lambda: nc.tensor.matmul(O[:, c0:512], lhsT=vt[:, i, :], rhs=pt[:, c0:512], start=(i == 0), stop=(i == nch - 1)),
                  reads=[vt, pt], writes=[O], partial=(i > 0), inc=False)
            kb.op("pe", lambda: nc.tensor.matmul(Dn[:, c0:512], lhsT=self.ones_bf[:], rhs=pt[:, c0:512], start=(i == 0), stop=(i == nch - 1)),
                  reads=[self.ones_bf, pt], writes=[Dn], partial=(i > 0), inc=True)
            if i == nch - 1:
                kb.op("dve", lambda: nc.vector.reciprocal(out=rd[:], in_=Dn[:]), reads=[Dn], writes=[rd])
                kb.op("act", lambda: nc.scalar.activation(out=sg[:], in_=gt[:], func=AF.Silu), reads=[gt], writes=[sg])
                kb.op("dve", lambda: nc.vector.tensor_tensor(out=sg[:], in0=sg[:], in1=rd[:], op=ALU.mult), reads=[sg, rd], writes=[sg])
                kb.op("dve", lambda: nc.vector.tensor_tensor(out=og[:, h, :], in0=O[:], in1=sg[:], op=ALU.mult), reads=[O, sg], writes=[og], partial=True)

        for tb in range(NTB):
            ts = slice(tb * 512, (tb + 1) * 512)
            nch = 4 * (tb + 1)
            for h in range(H):
                hs = slice(h * 128, (h + 1) * 128)
                kt = kbuf[h % 2]; vt = vbuf[h % 2]; qt = qbuf[h % 2]; gt = gbuf[h % 2]; b = bc[h % 2]
                kb.dma("sp", kt[:, 0:nch * 128], self.kT_s[hs, 0:nch * 128], reads=[self.kT_s], writes=[kt], sem_of=kt, partial=False)
                kb.dma("sp", vt[:, 0:nch, :], v_view[:, 0:nch, hs], reads=[self.v_s], writes=[vt], sem_of=vt, partial=False)
                kb.dma("sp", qt[:], self.qT_s[hs, ts], reads=[self.qT_s], writes=[qt], sem_of=qt, partial=False)
                kb.dma("sp", gt[:], self.gT_s[hs, ts], reads=[self.gT_s], writes=[gt], sem_of=gt, partial=False)
                kb.op("dve", lambda: nc.vector.tensor_scalar(out=b[:, 0:nch], in0=csT[:, 0:nch, h], scalar1=csl[:, nch - 1, h:h + 1], scalar2=None, op0=ALU.subtract),
                      reads=[csT, csl], writes=[b])
                O = psO[h % 2]; Dn = psD[h % 2]
                for i in range(nch):
                    a = i - 4 * tb
                    c0 = max(a, 0) * 128
                    ps = self.mmps()
                    kb.op("pe", lambda: nc.tensor.matmul(ps[:, c0:512], lhsT=kt[:, i * 128:(i + 1) * 128], rhs=qt[:, c0:512], start=True, stop=(a < 0)),
                          reads=[kt, qt], writes=[ps], inc=(a < 0))
                    if a >= 0:
                        kb.op("pe", lambda: nc.tensor.matmul(ps[:, c0:c0 + 128], lhsT=self.ident_bf[:], rhs=self.tri_bf[:], start=False, stop=True),
                              reads=[self.ident_bf, self.tri_bf], writes=[ps], partial=True)
                    pt = pbuf[kp % 4]; kp += 1
                    kb.op("act", lambda: nc.scalar.activation(out=pt[:, c0:512], in_=ps[:, c0:512], func=AF.Exp, bias=b[:, i:i + 1], scale=1.0),
                          reads=[ps, b], writes=[pt])
                    if prev is not None:
                        fox_pv(*prev)
                    prev = (h, i, nch, c0, pt, vt, O, Dn, gt)
            fox_pv(*prev)
            prev = None
            self.wout_block(tb, og, wo, hsrc, hdst, hres, psY)
        kb.pop()
        kb.pop()

    def rms_block(self, src, nchk, gcol, dst, nfeat):
        kb, nc = self.kb, self.nc
        pstat = self.psb[6]
        for c in range(nchk):
            s_ = self.r_sq[c % 2]
            kb.op("act", lambda: nc.scalar.activation(out=s_[:], in_=src[:, c, :], func=AF.Square), reads=[src], writes=[s_])
            kb.op("pe", lambda: nc.tensor.matmul(pstat[:], lhsT=self.consts[:, 3, :], rhs=s_[:], start=(c == 0), stop=(c == nchk - 1)),
                  reads=[s_, self.consts], writes=[pstat], partial=(c > 0), inc=True)
        kb.op("act", lambda: nc.scalar.activation(out=self.r_ln[:], in_=pstat[:], func=AF.Ln, scale=1.0 / nfeat, bias=self.epscol[:]),
              reads=[pstat, self.epscol], writes=[self.r_ln])
        kb.op("act", lambda: nc.scalar.activation(out=self.r_rstd[:], in_=self.r_ln[:], func=AF.Exp, scale=-0.5), reads=[self.r_ln], writes=[self.r_rstd])
        for c in range(nchk):
            d_ap, d_t = dst(c)
            kb.op("dve", lambda: nc.vector.scalar_tensor_tensor(out=d_ap, in0=src[:, c, :], scalar=gcol[:, c:c + 1], in1=self.r_rstd[:], op0=ALU.mult, op1=ALU.mult),
                  reads=[src, gcol, self.r_rstd], writes=[d_t], partial=True)

    def dsa_layer(self, li, hsrc, hdst):
        kb, nc, S = self.kb, self.nc, self.S
        W = self.w[li]
        NTB, NTK = self.NTB, self.NTK
        KIT = 14
        TOPK = min(256, S // 4)
        c_s = self.kT_s
        iq_s = self.qT_s
        kb.push()
        absw = kb.sb("absw", [128, NTK, 16], F32)
        sgn = kb.sb("sgn", [128, NTK, 16], F32)
        ckvnT = kb.sb("ckvnT", [128, 2, S], BF16)
        ckvtok = kb.sb("ckvtok", [128, NTK, 256], BF16)
        kb.push()
        hnT = kb.sb("hnT", [128, NC_, S], BF16)
        self.norm_phase(hsrc, li, hnT=hnT)
        wbuf = [kb.sb("wbuf%d" % i, [128, NC_, 128], BF16) for i in range(2)]
        obuf = [kb.sb("obuf%d" % i, [128, S], BF16) for i in range(2)]
        self.proj_cols(hnT, W["w_in"], QR + KVR + IDXD + IDXH, D, self.gT_s, wbuf=wbuf, obuf=obuf)
        self.proj_cols(hnT, W["w_in"], 0, 896, c_s, wbuf=wbuf, obuf=obuf)
        wiw = kb.sb("wiw", [128, NC_, 16], BF16)
        self.load_w_cols(wiw, W["w_in"], QR + KVR + IDXD, 16, NC_)
        pw = self.psb[5]
        for tk in range(NTK):
            for c in range(NC_):
                kb.op("pe", lambda: nc.tensor.matmul(pw[:, tk * 16:(tk + 1) * 16], lhsT=hnT[:, c, tk * 128:(tk + 1) * 128], rhs=wiw[:, c, :], start=(c == 0), stop=(c == NC_ - 1)),
                      reads=[hnT, wiw], writes=[pw], partial=not (tk == 0 and c == 0), inc=(c == NC_ - 1 and tk == NTK - 1))
        kb.op("act", lambda: nc.scalar.activation(out=absw[:].rearrange("p k h -> p (k h)"), in_=pw[:, 0:NTK * 16], func=AF.Abs, scale=1.0 / 32.0),
              reads=[pw], writes=[absw])
        kb.op("act", lambda: nc.scalar.activation(out=sgn[:].rearrange("p k h -> p (k h)"), in_=pw[:, 0:NTK * 16], func=AF.Sign), reads=[pw], writes=[sgn])
        kb.pop()
        kb.push()
        wq = kb.sb("wq", [128, 4, D], BF16)
        kb.dma("pool", wq[:], W["w_q_up"].rearrange("(c p) n -> p c n", p=128), writes=[wq], sem_of=wq, partial=False)
        wiq = kb.sb("wiq", [128, 4, 1024], BF16)
        kb.dma("pool", wiq[:], W["w_iq"].rearrange("(c p) n -> p c n", p=128), writes=[wiq], sem_of=wiq, partial=False)
        wuk = kb.sb("wuk", [128, H, KVR], BF16)
        kb.dma("pool", wuk[:], W["w_ukT"].rearrange("h d r -> d h r"), writes=[wuk], sem_of=wuk, partial=False)
        qn = kb.sb("qn", [128, 4], F32)
        kvn = kb.sb("kvn", [128, 2], F32)
        kb.dma("sp", qn[:], W["qn"], writes=[qn], sem_of=qn, partial=False)
        kb.dma("sp", kvn[:], W["kvn"], writes=[kvn], sem_of=kvn, partial=False)
        self.r_sq = [kb.sb("r_sq%d" % i, [128, 512], F32) for i in range(2)]
        self.r_ln = kb.sb("r_ln", [128, 512], F32)
        self.r_rstd = kb.sb("r_rstd", [128, 512], F32)
        cin = [kb.sb("cin%d" % i, [128, 6, 512], BF16) for i in range(2)]
        cqn = [kb.sb("cqn%d" % i, [128, 4, 512], BF16) for i in range(2)]
        qh = [kb.sb("qh%d" % i, [128, 512], BF16) for i in range(2)]
        qst = [kb.sb("qst%d" % i, [128, 2, 512], BF16) for i in range(2)]
        ist = [kb.sb("ist%d" % i, [128, 512], BF16) for i in range(2)]
        tst = kb.sb("tst", [128, 1024], BF16)
        qa_view = self.qa_s.t.rearrange("(h r p) t -> p h r t", r=2, p=128)
        c_view = c_s.t.rearrange("(c p) t -> p c t", p=128)
        for tb in range(NTB):
            ts = slice(tb * 512, (tb + 1) * 512)
            ci = cin[tb % 2]; cn = cqn[tb % 2]
            kb.dma("sp", ci[:], c_view[:, 0:6, ts], reads=[c_s], writes=[ci], sem_of=ci, partial=False)
            self.rms_block(ci, 4, qn, lambda c: (cn[:, c, :], cn), QR)
            ckv_src = T(ci.t[:, 4:6, :], "x"); ckv_src.dep = ci.dep
            self.rms_block(ckv_src, 2, kvn, lambda c: (ckvnT[:, c, ts], ckvnT), KVR)
            for q4 in range(4):
                tk = tb * 4 + q4
                for rc in range(2):
                    kb.op("pe", lambda: nc.tensor.transpose(out=self.pst[:, (q4 * 2 + rc) * 128:(q4 * 2 + rc + 1) * 128], in_=ckvnT[:, rc, tk * 128:(tk + 1) * 128], identity=self.ident_bf[:]),
                          reads=[ckvnT, self.ident_bf], writes=[self.pst], partial=not (q4 == 0 and rc == 0), inc=(q4 == 3 and rc == 1))
            kb.op("dve", lambda: nc.vector.tensor_copy(out=ckvtok[:, tb * 4:(tb + 1) * 4, :].rearrange("p k r -> p (k r)"), in_=self.pst[:, :]), reads=[self.pst], writes=[ckvtok], partial=True)
            for h in range(H):
                ps = self.mmps()
                for rc in range(4):
                    kb.op("pe", lambda: nc.tensor.matmul(ps[:], lhsT=wq[:, rc, h * 128:(h + 1) * 128], rhs=cn[:, rc, :], start=(rc == 0), stop=(rc == 3)),
                          reads=[wq, cn], writes=[ps], partial=(rc > 0), inc=(rc == 3))
                qt = qh[h % 2]
                self.evac(qt[:], ps[:], DH ** -0.5, [ps], qt, partial=False)
                st = qst[h % 2]
                for r2 in range(2):
                    ps2 = self.mmps()
                    kb.op("pe", lambda: nc.tensor.matmul(ps2[:], lhsT=wuk[:, h, r2 * 128:(r2 + 1) * 128], rhs=qt[:], start=True, stop=True), reads=[wuk, qt], writes=[ps2])
                    self.evac(st[:, r2, :], ps2[:], 1.0, [ps2], st, partial=(r2 > 0))
                kb.dma("sp", qa_view[:, h, :, ts], st[:], reads=[st], writes=[self.qa_s], sem_of=st)
            for m in range(8):
                ps = self.mmps()
                for rc in range(4):
                    kb.op("pe", lambda: nc.tensor.matmul(ps[:], lhsT=wiq[:, rc, m * 128:(m + 1) * 128], rhs=cn[:, rc, :], start=(rc == 0), stop=(rc == 3)),
                          reads=[wiq, cn], writes=[ps], partial=(rc > 0), inc=(rc == 3))
                it = ist[m % 2]
                self.evac(it[:], ps[:], 1.0, [ps], it, partial=False)
                kb.dma("sp", iq_s[m * 128:(m + 1) * 128, ts], it[:], reads=[it], writes=[iq_s], sem_of=it)
        kb.pop()
        kb.push()
        ikT = kb.sb("ikT", [64, S], BF16)
        kb.dma("sp", ikT[:], c_s[768:832, :], reads=[c_s], writes=[ikT], sem_of=ikT, partial=False)
        iqb = [kb.sb("iqb%d" % i, [64, 16, 128], BF16) for i in range(2)]
        accb = [kb.sb("acc%d" % i, [128, S], F32) for i in range(2)]
        junk = kb.sb("junk", [128, S], BF16)
        mkb = [kb.sb("mk%d" % i, [128, S], BF16) for i in range(2)]
        rb = [kb.sb("rb%d" % i, [128, 512], BF16) for i in range(4)]
        Dgb = [kb.sb("Dg%d" % i, [128, IDXH, 128], BF16) for i in range(2)]
        Mx = kb.sb("Mx", [128, 1], F32)
        stepv = kb.sb("stepv", [128, KIT + 1], F32)
        mid = kb.sb("mid", [128, KIT + 1], F32)
        cnt = kb.sb("cnt", [128, KIT + 1], F32)
        uu = kb.sb("uu", [128, 1], F32)
        thr = kb.sb("thr", [128, 1], F32)
        mst = [kb.sb("mst%d" % i, [128, NTK, 128], BF16) for i in range(2)]
        L01 = kb.sb("L01", [128, 128], BF16)
        kb.op("dve", lambda: nc.vector.tensor_copy(out=L01[:], in_=self.consts[:, 4, :]), reads=[self.consts], writes=[L01])
        iq_view = iq_s.t[0:1024, :].rearrange("(h d) t -> d h t", d=64)
        m_view = self.mT_s.t.rearrange("(c p) t -> p c t", p=128)
        kr = 0
        ka = 0
        paccb = [self.psb[2], self.psb[3]]

        def idx_acc(h, pacc, wd, r_, Dg, acc, ss):
            kb.op("pe", lambda: nc.tensor.matmul(pacc[:, 0:wd], lhsT=Dg[:, h, :], rhs=r_[:, 0:wd], start=(h == 0), stop=(h == IDXH - 1)),
                  reads=[Dg, r_], writes=[pacc], partial=(h > 0), inc=True)
            if h == IDXH - 1:
                self.evac(acc[:, ss], pacc[:, 0:wd], 1.0, [pacc], acc)

        def emit_mask(qb):
            mk = mkb[qb % 2]
            tq = slice(qb * 128, (qb + 1) * 128)
            ms = mst[qb % 2]
            for c0 in range(0, qb + 1, 8):
                ncb = min(8, qb + 1 - c0)
                for c in range(ncb):
                    kb.op("pe", lambda: nc.tensor.transpose(out=self.pst[:, c * 128:(c + 1) * 128], in_=mk[:, (c0 + c) * 128:(c0 + c + 1) * 128], identity=self.ident_bf[:]),
                          reads=[mk, self.ident_bf], writes=[self.pst], partial=(c > 0), inc=(c == ncb - 1))
                kb.op("act", lambda: nc.scalar.activation(out=ms[:, c0:c0 + ncb, :].rearrange("p c t -> p (c t)"), in_=self.pst[:, 0:ncb * 128], func=AF.Copy), reads=[self.pst], writes=[ms], partial=(c0 > 0))
            kb.dma("sp", m_view[:, 0:qb + 1, tq], ms[:, 0:qb + 1, :], reads=[ms], writes=[self.mT_s], sem_of=ms)

        for qb in range(NTK):
            n = (qb + 1) * 128
            tq = slice(qb * 128, (qb + 1) * 128)
            mk = mkb[qb % 2]
            if n > TOPK:
                acc = accb[qb % 2]
                Dg = Dgb[qb % 2]
                iqt = iqb[qb % 2]
                kb.dma("sp", iqt[:], iq_view[:, :, tq], reads=[iq_s], writes=[iqt], sem_of=iqt, partial=False)
                for h in range(IDXH):
                    kb.op("pool", lambda: nc.gpsimd.tensor_scalar(out=Dg[:, h, :], in0=self.ident_bf[:], scalar1=sgn[:, qb, h:h + 1], scalar2=1.0, op0=ALU.mult, op1=ALU.mult),
                          reads=[self.ident_bf, sgn], writes=[Dg], partial=(h > 0))
                pend = []
                for sb_ in range((n + 511) // 512):
                    wd = min(512, n - sb_ * 512)
                    ss = slice(sb_ * 512, sb_ * 512 + wd)
                    pacc = paccb[ka % 2]; ka += 1
                    for h in range(IDXH):
                        ps = self.mmps()
                        kb.op("pe", lambda: nc.tensor.matmul(ps[:, 0:wd], lhsT=iqt[:, h, :], rhs=ikT[:, ss], start=True, stop=True), reads=[iqt, ikT], writes=[ps])
                        r_ = rb[kr % 4]; kr += 1
                        kb.op("act", lambda: nc.scalar.activation(out=r_[:, 0:wd], in_=ps[:, 0:wd], func=AF.Relu, scale=absw[:, qb, h:h + 1]), reads=[ps, absw], writes=[r_])
                        pend.append((h, pacc, wd, r_, Dg, acc, ss))
                        if len(pend) > 1:
                            idx_acc(*pend.pop(0))
                while pend:
                    idx_acc(*pend.pop(0))
                kb.op("dve", lambda: nc.vector.tensor_reduce(out=Mx[:], in_=acc[:, 0:n], axis=AX.X, op=ALU.max, apply_absolute_value=True), reads=[acc], writes=[Mx])
                kb.op("dve", lambda: nc.vector.tensor_tensor(out=acc[:, tq], in0=acc[:, tq], in1=self.consts[:, 5, :], op=ALU.add), reads=[acc, self.consts], writes=[acc])
                kb.op("dve", lambda: nc.vector.tensor_scalar(out=stepv[:], in0=self.pow2[:, 0:KIT + 1], scalar1=Mx[:, 0:1], scalar2=None, op0=ALU.mult), reads=[self.pow2, Mx], writes=[stepv])
                kb.op("dve", lambda: nc.vector.memset(mid[:, 0:1], 0.0), writes=[mid])
                for k in range(KIT):
                    kb.op("dve", lambda: nc.vector.tensor_scalar(out=junk[:, 0:n], in0=acc[:, 0:n], scalar1=mid[:, k:k + 1], scalar2=None, op0=ALU.is_ge, op1=ALU.add, accum_out=cnt[:, k:k + 1]),
                          reads=[acc, mid], writes=[junk, cnt])
                    kb.op("dve", lambda: nc.vector.tensor_scalar(out=uu[:], in0=cnt[:, k:k + 1], scalar1=TOPK - 0.5, scalar2=stepv[:, k:k + 1], op0=ALU.is_ge, op1=ALU.mult),
                          reads=[cnt, stepv], writes=[uu])
                    kb.op("dve", lambda: nc.vector.scalar_tensor_tensor(out=mid[:, k + 1:k + 2], in0=uu[:], scalar=stepv[:, k + 1:k + 2], in1=mid[:, k:k + 1], op0=ALU.subtract, op1=ALU.add),
                          reads=[uu, stepv, mid], writes=[mid])
                kb.op("dve", lambda: nc.vector.tensor_tensor(out=thr[:], in0=mid[:, KIT:KIT + 1], in1=stepv[:, KIT:KIT + 1], op=ALU.subtract), reads=[mid, stepv], writes=[thr])
                kb.op("dve", lambda: nc.vector.tensor_scalar(out=mk[:, 0:n], in0=acc[:, 0:n], scalar1=thr[:, 0:1], scalar2=None, op0=ALU.is_ge), reads=[acc, thr], writes=[mk])
            else:
                if qb > 0:
                    kb.op("dve", lambda: nc.vector.memset(mk[:, 0:qb * 128], 1.0), writes=[mk])
                kb.op("dve", lambda: nc.vector.tensor_copy(out=mk[:, tq], in_=L01[:]), reads=[L01], writes=[mk], partial=(qb > 0))
            if qb >= 1:
                emit_mask(qb - 1)
        emit_mask(NTK - 1)
        kb.pop()
        kb.push()
        wo = kb.sb("wo", [128, H, D], BF16)
        for h4 in range(4):
            src = W["w_out"].rearrange("(c p) n -> p c n", p=128)[:, h4 * 4:(h4 + 1) * 4, :]
            kb.dma("pool", wo[:, h4 * 4:(h4 + 1) * 4, :], src, writes=[wo], sem_of=wo, partial=True)
        wuv = kb.sb("wuv", [128, 2, D], BF16)
        kb.dma("pool", wuv[:], W["w_uv"].rearrange("(c p) n -> p c n", p=128), writes=[wuv], sem_of=wuv, partial=False)
        b31 = kb.sb("b31", [128, 16], F32)
        kb.dma("sp", b31[:], W["rb31"], writes=[b31], sem_of=b31, partial=False)
        Dt = kb.sb("Dt", [128, H, 2, 128], BF16)
        dtmp = kb.sb("dtmp", [128, H, 2, 128], F32)
        kb.dma("sp", dtmp[:], W["rbD"], writes=[dtmp], sem_of=dtmp, partial=False)
        for h in range(H):
            kb.op("dve", lambda: nc.vector.tensor_scalar(out=Dt[:, h, :, :], in0=dtmp[:, h, :, :], scalar1=b31[:, h:h + 1], scalar2=None, op0=ALU.subtract),
                  reads=[dtmp, b31], writes=[Dt], partial=(h > 0))
        mT = kb.sb("mT", [128, NTK, 512], BF16)
        qab = [kb.sb("qab%d" % i, [128, 2, 512], BF16) for i in range(2)]
        gbuf = [kb.sb("gbuf%d" % i, [128, 512], BF16) for i in range(2)]
        pbuf = [kb.sb("pbuf%d" % i, [128, 512], BF16) for i in range(5)]
        ol = kb.sb("ol", [128, 2, 512], BF16)
        rd = kb.sb("rd", [128, 512], F32)
        sg = kb.sb("sg", [128, 512], F32)
        og = kb.sb("og", [128, H, 512], BF16)
        hres = [kb.sb("hres%d" % i, [128, 512], F32) for i in range(2)]
        O0, O1, Dn, psU, psY = self.psb[2], self.psb[3], self.psb[4], self.psb[6], self.psb[6]
        stb = [self.psb[0], self.psb[1], self.psb[5]]
        kp = 0
        pend = []
        LOOK = 2

        def dsa_pv(h, i, nch, c0, pt, gt):
            hs = slice(h * 128, (h + 1) * 128)
            kb.op("pe", lambda: nc.tensor.matmul(O0[:, c0:512], lhsT=ckvtok[:, i, 0:128], rhs=pt[:, c0:512], start=(i == 0), stop=(i == nch - 1)),
                  reads=[ckvtok, pt], writes=[O0], partial=(i > 0), inc=False)
            kb.op("pe", lambda: nc.tensor.matmul(O1[:, c0:512], lhsT=ckvtok[:, i, 128:256], rhs=pt[:, c0:512], start=(i == 0), stop=(i == nch - 1)),
                  reads=[ckvtok, pt], writes=[O1], partial=(i > 0), inc=False)
            kb.op("pe", lambda: nc.tensor.matmul(Dn[:, c0:512], lhsT=self.ones_bf[:], rhs=pt[:, c0:512], start=(i == 0), stop=(i == nch - 1)),
                  reads=[self.ones_bf, pt], writes=[Dn], partial=(i > 0), inc=True)
            if i == nch - 1:
                kb.op("act", lambda: nc.scalar.activation(out=ol[:, 0, :], in_=O0[:], func=AF.Copy), reads=[O0], writes=[ol])
                kb.op("dve", lambda: nc.vector.tensor_copy(out=ol[:, 1, :], in_=O1[:]), reads=[O1], writes=[ol], partial=True)
                kb.op("dve", lambda: nc.vector.reciprocal(out=rd[:], in_=Dn[:]), reads=[Dn], writes=[rd])
                kb.op("pe", lambda: nc.tensor.matmul(psU[:], lhsT=wuv[:, 0, hs], rhs=ol[:, 0, :], start=True, stop=False), reads=[wuv, ol], writes=[psU], inc=False)
                kb.op("pe", lambda: nc.tensor.matmul(psU[:], lhsT=wuv[:, 1, hs], rhs=ol[:, 1, :], start=False, stop=True), reads=[wuv, ol], writes=[psU], partial=True)
                kb.op("act", lambda: nc.scalar.activation(out=sg[:], in_=gt[:], func=AF.Silu), reads=[gt], writes=[sg])
                kb.op("dve", lambda: nc.vector.tensor_tensor(out=sg[:], in0=sg[:], in1=rd[:], op=ALU.mult), reads=[sg, rd], writes=[sg])
                kb.op("dve", lambda: nc.vector.tensor_tensor(out=og[:, h, :], in0=psU[:], in1=sg[:], op=ALU.mult), reads=[psU, sg], writes=[og], partial=True)

        for tb in range(NTB):
            ts = slice(tb * 512, (tb + 1) * 512)
            nch = 4 * (tb + 1)
            kb.dma("sp", mT[:, 0:nch, :], m_view[:, 0:nch, ts], reads=[self.mT_s], writes=[mT], sem_of=mT, partial=False)
            for h in range(H):
                hs = slice(h * 128, (h + 1) * 128)
                qa = qab[h % 2]; gt = gbuf[h % 2]
                kb.dma("sp", qa[:], qa_view[:, h, :, ts], reads=[self.qa_s], writes=[qa], sem_of=qa, partial=False)
                kb.dma("sp", gt[:], self.gT_s[hs, ts], reads=[self.gT_s], writes=[gt], sem_of=gt, partial=False)
                for i in range(nch):
                    a = i - 4 * tb
                    c0 = max(a, 0) * 128
                    extra = [(c, 4 * tb + c - i) for c in range(4) if (4 * tb + c - i) in (0, 1) and c * 128 >= c0]
                    ps = stb[kp % 3]
                    kb.op("pe", lambda: nc.tensor.matmul(ps[:, c0:512], lhsT=ckvnT[:, 0, i * 128:(i + 1) * 128], rhs=qa[:, 0, c0:512], start=True, stop=False),
                          reads=[ckvnT, qa], writes=[ps], inc=False)
                    kb.op("pe", lambda: nc.tensor.matmul(ps[:, c0:512], lhsT=ckvnT[:, 1, i * 128:(i + 1) * 128], rhs=qa[:, 1, c0:512], start=False, stop=(len(extra) == 0)),
                          reads=[ckvnT, qa], writes=[ps], partial=True, inc=(len(extra) == 0))
                    for ei, (c, df) in enumerate(extra):
                        kb.op("pe", lambda: nc.tensor.matmul(ps[:, c * 128:(c + 1) * 128], lhsT=self.ident_bf[:], rhs=Dt[:, h, df, :], start=False, stop=(ei == len(extra) - 1)),
                              reads=[self.ident_bf, Dt], writes=[ps], partial=True, inc=(ei == len(extra) - 1))
                    pt = pbuf[kp % 5]; kp += 1
                    kb.op("act", lambda: nc.scalar.activation(out=pt[:, c0:512], in_=ps[:, c0:512], func=AF.Exp, bias=b31[:, h:h + 1], scale=1.0),
                          reads=[ps, b31], writes=[pt])
                    if kp % 2 == 0:
                        kb.op("dve", lambda: nc.vector.tensor_tensor(out=pt[:, c0:512], in0=pt[:, c0:512], in1=mT[:, i, c0:512], op=ALU.mult), reads=[pt, mT], writes=[pt])
                    else:
                        kb.op("pool", lambda: nc.gpsimd.tensor_tensor(out=pt[:, c0:512], in0=pt[:, c0:512], in1=mT[:, i, c0:512], op=ALU.mult), reads=[pt, mT], writes=[pt])
                    pend.append((h, i, nch, c0, pt, gt))
                    if len(pend) > LOOK:
                        dsa_pv(*pend.pop(0))
            while pend:
                dsa_pv(*pend.pop(0))
            self.wout_block(tb, og, wo, hsrc, hdst, hres, psY)
        kb.pop()
        kb.pop()


def make_consts():
    c = np.zeros((128, 6, 128), np.float32)
    pp = np.arange(128)[:, None]; jj = np.arange(128)[None, :]
    c[:, 4, :] = np.where(jj <= pp, 1.0, 0.0)
    c[:, 5, :] = np.where(jj <= pp, 0.0, -1e30)
    c[:, 0, :] = np.eye(128, dtype=np.float32)
    sp = np.arange(128)[:, None]; tp = np.arange(128)[None, :]
    c[:, 1, :] = np.where(sp <= tp, 0.0, NEG)
    c[127, 2, :] = 1.0
    c[:, 3, :] = 1.0
    return c


def cols16(v):
    return np.ascontiguousarray(v.reshape(16, 128).T)


def t5_bucket_np(dist):
    import math
    max_exact = 16
    d = np.maximum(dist, 0)
    df = np.maximum(d, 1).astype(np.float32)
    large = max_exact + (np.log(df / max_exact) / math.log(128 / max_exact) * (32 - max_exact)).astype(np.int32)
    large = np.minimum(large, 31)
    return np.where(d < max_exact, d, large)


def shared_inputs(p, layers, do_final=True):
    im = {"consts": make_consts()}
    g = np.zeros((128, 5, 16), np.float32)
    for i in range(4):
        g[:, i, :] = cols16(p["norm_g"][i])
    g[:, 4, :] = cols16(p["final_g"])
    im["gcols"] = g
    im["pow2"] = np.ascontiguousarray(np.broadcast_to((2.0 ** -np.arange(17, dtype=np.float64)).astype(np.float32), (128, 17)))
    sp = np.arange(128)[:, None]; tp = np.arange(128)[None, :]
    bk = np.stack([t5_bucket_np(tp - sp), t5_bucket_np(128 + tp - sp)], 0)
    for kind, li in layers:
        j = li // 2
        if kind == "b":
            im["w_in%d" % li] = np.ascontiguousarray(p["b_w_in"][j])
            im["f_bias%d" % li] = np.ascontiguousarray(p["b_f_bias"][j].reshape(16, 1))
            im["w_out%d" % li] = np.ascontiguousarray(p["b_w_out"][j])
        else:
            im["w_in%d" % li] = np.ascontiguousarray(p["a_w_in"][j])
            im["qn%d" % li] = np.ascontiguousarray(p["a_q_norm"][j].reshape(4, 128).T)
            im["kvn%d" % li] = np.ascontiguousarray(p["a_kv_norm"][j].reshape(2, 128).T)
            im["w_q_up%d" % li] = np.ascontiguousarray(p["a_w_q_up"][j])
            im["w_ukT%d" % li] = np.ascontiguousarray(np.transpose(p["a_w_uk"][j], (1, 2, 0)))
            im["w_uv%d" % li] = np.ascontiguousarray(p["a_w_uv"][j].reshape(KVR, D))
            im["w_iq%d" % li] = np.ascontiguousarray(p["a_w_iq"][j])
            im["w_out%d" % li] = np.ascontiguousarray(p["a_w_out"][j])
            rb = p["rel_bias"]
            im["rb31_%d" % li] = np.ascontiguousarray(np.broadcast_to(rb[31][None, :], (128, 16)))
            gath = rb[bk]
            im["rbD_%d" % li] = np.ascontiguousarray(np.transpose(gath, (1, 3, 0, 2)))
    return im


_PROG_CACHE = {}


def get_prog(S, layers, do_final):
    key = (S, tuple(layers), do_final)
    if key not in _PROG_CACHE:
        _PROG_CACHE[key] = Prog(S, list(layers), do_final=do_final)
    return _PROG_CACHE[key]


LAYERS = [("a", 0), ("b", 1), ("a", 2), ("b", 3)]
FUSED = True


def run_layers(xT_list, p, layers, do_final):
    S = xT_list[0].shape[1]
    prog = get_prog(S, layers, do_final)
    sh = shared_inputs(p, layers, do_final)
    in_maps = []
    for xT in xT_list:
        m = dict(sh)
        m["xT"] = xT
        in_maps.append(m)
    res = run_bass_kernel_spmd(prog.nc, in_maps, core_ids=list(range(len(xT_list))))
    key = "outT" if do_final else "hT"
    return [r[key] for r in res.results]


def kernel(**inputs):
    p = {k: np.asarray(v) for k, v in inputs.items()}
    x = p["x"]
    B = x.shape[0]
    xT = [np.ascontiguousarray(x[b].T) for b in range(B)]
    if FUSED:
        outT = run_layers(xT, p, LAYERS, True)
    else:
        cur = xT
        for n, lay in enumerate(LAYERS):
            cur = run_layers(cur, p, [lay], n == len(LAYERS) - 1)
        outT = cur
    return np.stack([np.ascontiguousarray(o.T) for o in outT], 0).astype(np.float32)
```

```python
import numpy as np
import concourse.bass as bass
import concourse.mybir as mybir
from concourse.bass_utils import run_bass_kernel_spmd

F32 = mybir.dt.float32
BF16 = mybir.dt.bfloat16
AF = mybir.ActivationFunctionType
ALU = mybir.AluOpType
AX = mybir.AxisListType

D = 2048
H = 16
DH = 128
NC_ = 16
QR = 512
KVR = 256
IDXH = 16
IDXD = 64
A_IN = QR + KVR + IDXD + IDXH + D
B_IN = 3 * D + H + D
EPS = 1e-6
NEG = -30000.0


class Dep:
    __slots__ = ("name", "w", "r", "dsem", "dcnt")

    def __init__(self, name):
        self.name = name
        self.w = {}
        self.r = {}
        self.dsem = None


class T:
    def __init__(self, t, name):
        self.t = t
        self.dep = Dep(name)

    def __getitem__(self, idx):
        return self.t[idx]


def _dep(d):
    return d.dep if isinstance(d, T) else d


class KB:
    NDMA = 48

    def __init__(self, nc):
        self.nc = nc
        self.engs = {"pe": nc.tensor, "act": nc.scalar, "dve": nc.vector,
                     "pool": nc.gpsimd, "sp": nc.sync}
        self._stack = [[]]
        self.semobj = {}
        self.semval = {}
        self.sems = {}
        for e in self.engs:
            key = "e_" + e
            self.semobj[key] = self._enter(nc.semaphore("s_" + e))
            self.semval[key] = 0
            self.sems[e] = key
        self.dfree = []
        for i in range(self.NDMA):
            key = "d_%d" % i
            self.semobj[key] = self._enter(nc.semaphore("sd_%d" % i))
            self.semval[key] = 0
            self.dfree.append(key)
        self.seen = {e: {} for e in self.engs}
        self.scope_deps = [[]]

    def _enter(self, cm):
        obj = cm.__enter__()
        self._stack[-1].append(cm)
        return obj

    def push(self):
        self._stack.append([])
        self.scope_deps.append([])

    def pop(self):
        self.barrier()
        for d in self.scope_deps.pop():
            if d.dsem is not None:
                self.dfree.append(d.dsem)
                d.dsem = None
        for cm in reversed(self._stack.pop()):
            cm.__exit__(None, None, None)

    def close(self):
        while len(self._stack) > 1:
            self.pop()
        for cm in reversed(self._stack[0]):
            cm.__exit__(None, None, None)

    def sb(self, name, shape, dt):
        self.uid = getattr(self, "uid", 0) + 1
        name = "%s_u%d" % (name, self.uid)
        t = T(self._enter(self.nc.sbuf_tensor(name, list(shape), dt)), name)
        self.scope_deps[-1].append(t.dep)
        return t

    def ps(self, name, shape, dt):
        t = T(self._enter(self.nc.psum_tensor(name, list(shape), dt)), name)
        self.scope_deps[-1].append(t.dep)
        return t

    def region(self, name):
        return Dep(name)

    def _wait(self, eng, key, val):
        if self.seen[eng].get(key, 0) >= val:
            return
        if key == self.sems[eng] and val <= self.semval[key] - 2:
            return
        self.engs[eng].wait_ge(self.semobj[key], val)
        self.seen[eng][key] = val

    def _deps(self, eng, reads, writes, partial):
        own = self.sems[eng]
        skip_own = (eng == "pe")
        for d in reads:
            d = _dep(d)
            for k, (v, _p) in d.w.items():
                if skip_own and k == own:
                    continue
                self._wait(eng, k, v)
        for d in writes:
            d = _dep(d)
            for k, v in d.r.items():
                if skip_own and k == own:
                    continue
                self._wait(eng, k, v)
            for k, (v, p) in d.w.items():
                if partial and p:
                    continue
                if skip_own and k == own:
                    continue
                self._wait(eng, k, v)

    def _record(self, key, val, reads, writes, partial):
        for d in reads:
            d = _dep(d)
            if d.r.get(key, 0) < val:
                d.r[key] = val
        for d in writes:
            d = _dep(d)
            if partial and not d.r and all(p for (_v, p) in d.w.values()):
                d.w[key] = (val, True)
            else:
                d.w = {key: (val, partial)}
                d.r = {}

    def op(self, eng, fn, reads=(), writes=(), partial=False, inc=True):
        self._deps(eng, reads, writes, partial)
        ins = fn()
        key = self.sems[eng]
        if inc:
            self.semval[key] += 1
            ins.then_inc(self.semobj[key], 1)
            val = self.semval[key]
        else:
            val = self.semval[key] + 1
        self._record(key, val, reads, writes, partial)
        return ins

    def dma(self, q, out, in_, reads=(), writes=(), sem_of=None, partial=True, **kw):
        self._deps(q, reads, writes, partial)
        d = _dep(sem_of)
        if d.dsem is None:
            d.dsem = self.dfree.pop()
        key = d.dsem
        self.semval[key] += 16
        ins = self.engs[q].dma_start(out=out, in_=in_, **kw)
        ins.then_inc(self.semobj[key], 16)
        self._record(key, self.semval[key], reads, writes, partial)
        return ins

    def barrier(self):
        for e in self.engs:
            for key, v in self.semval.items():
                if v > 0:
                    self._wait(e, key, v)

    def wait_all_on(self, eng):
        for key, v in self.semval.items():
            if v > 0:
                self._wait(eng, key, v)


class Prog:
    def __init__(self, S, layers, first_src_is_x=True, do_final=True):
        self.S = S
        self.NTB = S // 512
        self.NTK = S // 128
        self.layers = layers
        self.do_final = do_final
        nc = self.nc = bass.Bass("TRN2", target_bir_lowering=False)
        self.kb = KB(nc)
        kb = self.kb
        dt = nc.dram_tensor
        self.xT = T(dt("xT", [D, S], F32, kind="ExternalInput").ap(), "xT")
        if do_final:
            self.outT = T(dt("outT", [D, S], F32, kind="ExternalOutput").ap(), "outT")
        self.hT = T(dt("hT", [D, S], F32, kind="Internal" if do_final else "ExternalOutput").ap(), "hT")
        self.consts_d = dt("consts", [128, 6, 128], F32, kind="ExternalInput").ap()
        self.gcols_d = dt("gcols", [128, 5, 16], F32, kind="ExternalInput").ap()
        self.w = {}
        for kind, li in layers:
            if kind == "b":
                self.w[li] = dict(
                    w_in=dt("w_in%d" % li, [D, B_IN], F32, kind="ExternalInput").ap(),
                    f_bias=dt("f_bias%d" % li, [16, 1], F32, kind="ExternalInput").ap(),
                    w_out=dt("w_out%d" % li, [D, D], F32, kind="ExternalInput").ap(),
                )
            else:
                self.w[li] = dict(
                    w_in=dt("w_in%d" % li, [D, A_IN], F32, kind="ExternalInput").ap(),
                    qn=dt("qn%d" % li, [128, 4], F32, kind="ExternalInput").ap(),
                    kvn=dt("kvn%d" % li, [128, 2], F32, kind="ExternalInput").ap(),
                    w_q_up=dt("w_q_up%d" % li, [QR, D], F32, kind="ExternalInput").ap(),
                    w_ukT=dt("w_ukT%d" % li, [H, DH, KVR], F32, kind="ExternalInput").ap(),
                    w_uv=dt("w_uv%d" % li, [KVR, D], F32, kind="ExternalInput").ap(),
                    w_iq=dt("w_iq%d" % li, [QR, IDXH * IDXD], F32, kind="ExternalInput").ap(),
                    w_out=dt("w_out%d" % li, [D, D], F32, kind="ExternalInput").ap(),
                    rb31=dt("rb31_%d" % li, [128, 16], F32, kind="ExternalInput").ap(),
                    rbD=dt("rbD_%d" % li, [128, 16, 2, 128], F32, kind="ExternalInput").ap(),
                )
        self.qT_s = T(dt("qT_s", [D, S], BF16, kind="Internal").ap(), "qT_s")
        self.kT_s = T(dt("kT_s", [D, S], BF16, kind="Internal").ap(), "kT_s")
        self.v_s = T(dt("v_s", [S, D], BF16, kind="Internal").ap(), "v_s")
        self.gT_s = T(dt("gT_s", [D, S], BF16, kind="Internal").ap(), "gT_s")

        self.consts = kb.sb("consts_sb", [128, 6, 128], F32)
        self.gcols = kb.sb("gcols_sb", [128, 5, 16], F32)
        kb.dma("sp", self.consts[:], self.consts_d, writes=[self.consts], sem_of=self.consts)
        kb.dma("sp", self.gcols[:], self.gcols_d, writes=[self.gcols], sem_of=self.gcols)
        self.ident_bf = kb.sb("ident_bf", [128, 128], BF16)
        self.tri_bf = kb.sb("tri_bf", [128, 128], BF16)
        self.ones_bf = kb.sb("ones_bf", [128, 128], BF16)
        for dst, ci in ((self.ident_bf, 0), (self.tri_bf, 1), (self.ones_bf, 3)):
            kb.op("dve", lambda: nc.vector.tensor_copy(out=dst[:], in_=self.consts[:, ci, :]),
                  reads=[self.consts], writes=[dst])
        self.epscol = kb.sb("epscol", [128, 1], F32)
        kb.op("dve", lambda: nc.vector.memset(self.epscol[:], EPS), writes=[self.epscol])
        self.pow2_d = dt("pow2", [128, 17], F32, kind="ExternalInput").ap()
        self.pow2 = kb.sb("pow2_sb", [128, 17], F32)
        kb.dma("sp", self.pow2[:], self.pow2_d, writes=[self.pow2], sem_of=self.pow2)
        self.qa_s = T(dt("qa_s", [2 * D, S], BF16, kind="Internal").ap(), "qa_s")
        self.mT_s = T(dt("mT_s", [S, S], BF16, kind="Internal").ap(), "mT_s")
        self.psb = [kb.ps("psb%d" % i, [128, 512], F32) for i in range(7)]
        self.pst = kb.ps("pst", [128, 1024], BF16)
        self.k_mm = 0

        src = self.xT
        for kind, li in layers:
            if kind == "b":
                self.fox_layer(li, src, self.hT)
            else:
                self.dsa_layer(li, src, self.hT)
            src = self.hT
        if do_final:
            self.norm_phase(src, 4, final=True)
        kb.wait_all_on("sp")
        kb.close()

    def mmps(self):
        self.k_mm += 1
        return self.psb[self.k_mm % 2]

    def load_w_cols(self, wt, w_ap, col0, ncols, nchunk):
        src = w_ap.rearrange("(c p) n -> p c n", p=128)[:, :, col0:col0 + ncols]
        self.kb.dma("pool", wt[:, 0:nchunk, 0:ncols], src, writes=[wt], sem_of=wt, partial=False)

    def norm_phase(self, hsrc, gi, final=False, hnT=None):
        kb, nc, S = self.kb, self.nc, self.S
        kb.push()
        hb = [kb.sb("n_hb%d" % i, [128, 512], F32) for i in range(3)]
        sq = [kb.sb("n_sq%d" % i, [128, 512], F32) for i in range(2)]
        lnv = kb.sb("n_lnv", [128, 512], F32)
        rstd = kb.sb("n_rstd", [128, 512], F32)
        ob = [kb.sb("n_ob%d" % i, [128, 512], F32) for i in range(2)] if final else None
        ones_f = self.consts
        pstat = self.psb[6]
        k = 0
        for tb in range(self.NTB):
            ts = slice(tb * 512, (tb + 1) * 512)
            for c in range(NC_):
                h = hb[k % 3]; s = sq[k % 2]; k += 1
                kb.dma("sp", h[:], hsrc[c * 128:(c + 1) * 128, ts], reads=[hsrc], writes=[h], sem_of=h, partial=False)
                kb.op("act", lambda: nc.scalar.activation(out=s[:], in_=h[:], func=AF.Square), reads=[h], writes=[s])
                kb.op("pe", lambda: nc.tensor.matmul(pstat[:], lhsT=ones_f[:, 3, :], rhs=s[:], start=(c == 0), stop=(c == NC_ - 1)),
                      reads=[s, ones_f], writes=[pstat], partial=(c > 0), inc=True)
            kb.op("act", lambda: nc.scalar.activation(out=lnv[:], in_=pstat[:], func=AF.Ln, scale=1.0 / D, bias=self.epscol[:]),
                  reads=[pstat, self.epscol], writes=[lnv])
            kb.op("act", lambda: nc.scalar.activation(out=rstd[:], in_=lnv[:], func=AF.Exp, scale=-0.5), reads=[lnv], writes=[rstd])
            for c in range(NC_):
                h = hb[k % 3]; k += 1
                kb.dma("sp", h[:], hsrc[c * 128:(c + 1) * 128, ts], reads=[hsrc], writes=[h], sem_of=h, partial=False)
                if final:
                    o = ob[c % 2]
                    kb.op("dve", lambda: nc.vector.scalar_tensor_tensor(out=o[:], in0=h[:], scalar=self.gcols[:, gi, c:c + 1], in1=rstd[:], op0=ALU.mult, op1=ALU.mult),
                          reads=[h, rstd, self.gcols], writes=[o])
                    kb.dma("sp", self.outT[c * 128:(c + 1) * 128, ts], o[:], reads=[o], writes=[self.outT], sem_of=o)
                else:
                    kb.op("dve", lambda: nc.vector.scalar_tensor_tensor(out=hnT[:, c, ts], in0=h[:], scalar=self.gcols[:, gi, c:c + 1], in1=rstd[:], op0=ALU.mult, op1=ALU.mult),
                          reads=[h, rstd, self.gcols], writes=[hnT], partial=True)
        kb.pop()

    def proj_cols(self, hnT, w_ap, col0, ncols, dst, scale=1.0, token_major=False, wbuf=None, obuf=None):
        kb, nc, S = self.kb, self.nc, self.S
        for cc in range(ncols // 128):
            wt = wbuf[cc % 2]
            ob = obuf[cc % 2]
            self.load_w_cols(wt, w_ap, col0 + cc * 128, 128, NC_)
            if not token_major:
                for tb in range(self.NTB):
                    ts = slice(tb * 512, (tb + 1) * 512)
                    ps = self.mmps()
                    for c in range(NC_):
                        kb.op("pe", lambda: nc.tensor.matmul(ps[:], lhsT=wt[:, c, :], rhs=hnT[:, c, ts], start=(c == 0), stop=(c == NC_ - 1)),
                              reads=[wt, hnT], writes=[ps], partial=(c > 0), inc=(c == NC_ - 1))
                    self.evac(ob[:, ts], ps[:], scale, [ps], ob)
                kb.dma("sp", dst[cc * 128:(cc + 1) * 128, :], ob[:], reads=[ob], writes=[dst], sem_of=ob)
            else:
                for tq in range(self.NTK // 4):
                    ps = self.mmps()
                    for q in range(4):
                        tk = tq * 4 + q
                        for c in range(NC_):
                            kb.op("pe", lambda: nc.tensor.matmul(ps[:, q * 128:(q + 1) * 128], lhsT=hnT[:, c, tk * 128:(tk + 1) * 128], rhs=wt[:, c, :],
                                                                 start=(c == 0), stop=(c == NC_ - 1)),
                                  reads=[wt, hnT], writes=[ps], partial=not (c == 0 and q == 0), inc=(c == NC_ - 1 and q == 3))
                    self.evac(ob[:, tq * 512:(tq + 1) * 512], ps[:], scale, [ps], ob)
                dv = dst.t.rearrange("(k p) n -> p k n", p=128)[:, :, cc * 128:(cc + 1) * 128]
                kb.dma("sp", dv, ob[:].rearrange("p (k n) -> p k n", n=128), reads=[ob], writes=[dst], sem_of=ob)

    def evac(self, out_ap, in_ap, scale, reads, wtile, partial=True):
        kb, nc = self.kb, self.nc
        self.k_ev = getattr(self, "k_ev", 0) + 1
        if self.k_ev % 2 == 0:
            kb.op("act", lambda: nc.scalar.activation(out=out_ap, in_=in_ap, func=AF.Copy, scale=float(scale)), reads=reads, writes=[wtile], partial=partial)
        else:
            kb.op("dve", lambda: nc.vector.tensor_scalar(out=out_ap, in0=in_ap, scalar1=float(scale), scalar2=None, op0=ALU.mult), reads=reads, writes=[wtile], partial=partial)

    def wout_block(self, tb, og, wo, hsrc, hdst, hres, psY):
        kb, nc = self.kb, self.nc
        ts = slice(tb * 512, (tb + 1) * 512)
        for dc in range(NC_):
            hr = hres[dc % 2]
            kb.dma("sp", hr[:], hsrc[dc * 128:(dc + 1) * 128, ts], reads=[hsrc], writes=[hr], sem_of=hr, partial=False)
            for h in range(H):
                kb.op("pe", lambda: nc.tensor.matmul(psY[:], lhsT=wo[:, h, dc * 128:(dc + 1) * 128], rhs=og[:, h, :], start=(h == 0), stop=(h == H - 1)),
                      reads=[wo, og], writes=[psY], partial=(h > 0), inc=(h == H - 1))
            kb.op("dve", lambda: nc.vector.tensor_tensor(out=hr[:], in0=psY[:], in1=hr[:], op=ALU.add), reads=[psY, hr], writes=[hr])
            kb.dma("sp", hdst[dc * 128:(dc + 1) * 128, ts], hr[:], reads=[hr], writes=[hdst], sem_of=hr)

    def fox_layer(self, li, hsrc, hdst):
        kb, nc, S = self.kb, self.nc, self.S
        W = self.w[li]
        NTB, NTK = self.NTB, self.NTK
        kb.push()
        csT = kb.sb("csT", [128, NTK, 16], F32)
        csl = kb.sb("csl", [128, NTK, 16], F32)
        kb.push()
        hnT = kb.sb("hnT", [128, NC_, S], BF16)
        self.norm_phase(hsrc, li, hnT=hnT)
        kb.push()
        wbuf = [kb.sb("wbuf%d" % i, [128, NC_, 128], BF16) for i in range(2)]
        obuf = [kb.sb("obuf%d" % i, [128, S], BF16) for i in range(2)]
        self.proj_cols(hnT, W["w_in"], 0, D, self.qT_s, scale=DH ** -0.5, wbuf=wbuf, obuf=obuf)
        self.proj_cols(hnT, W["w_in"], D, D, self.kT_s, wbuf=wbuf, obuf=obuf)
        self.proj_cols(hnT, W["w_in"], 3 * D + H, D, self.gT_s, wbuf=wbuf, obuf=obuf)
        self.proj_cols(hnT, W["w_in"], 2 * D, D, self.v_s, token_major=True, wbuf=wbuf, obuf=obuf)
        kb.pop()
        wf = kb.sb("wf", [128, NC_, 16], BF16)
        self.load_w_cols(wf, W["w_in"], 3 * D, 16, NC_)
        fb = kb.sb("fb", [16, 1], F32)
        kb.dma("sp", fb[:], W["f_bias"], writes=[fb], sem_of=fb, partial=False)
        nfb = kb.sb("nfb", [16, 1], F32)
        kb.op("dve", lambda: nc.vector.tensor_scalar(out=nfb[:], in0=fb[:], scalar1=-1.0, scalar2=None, op0=ALU.mult), reads=[fb], writes=[nfb])
        lf = kb.sb("lf", [16, S], F32)
        cs = kb.sb("cs", [16, S], F32)
        onesr = kb.sb("onesr", [16, S], F32)
        kb.op("dve", lambda: nc.vector.memset(onesr[:], 1.0), writes=[onesr])
        for tb in range(NTB):
            ts = slice(tb * 512, (tb + 1) * 512)
            ps = self.mmps()
            for c in range(NC_):
                kb.op("pe", lambda: nc.tensor.matmul(ps[0:16, :], lhsT=wf[:, c, :], rhs=hnT[:, c, ts], start=(c == 0), stop=(c == NC_ - 1)),
                      reads=[wf, hnT], writes=[ps], partial=(c > 0), inc=(c == NC_ - 1))
            kb.op("act", lambda: nc.scalar.activation(out=lf[:, ts], in_=ps[0:16, :], func=AF.Exp, scale=-1.0, bias=nfb[:]),
                  reads=[ps, nfb], writes=[lf], partial=True)
        one16 = kb.sb("one16", [16, 1], F32)
        kb.op("dve", lambda: nc.vector.memset(one16[:], 1.0), writes=[one16])
        kb.op("act", lambda: nc.scalar.activation(out=lf[:], in_=lf[:], func=AF.Ln, scale=1.0, bias=one16[:]), reads=[lf, one16], writes=[lf])
        kb.op("dve", lambda: nc.vector.tensor_tensor_scan(out=cs[:], data0=onesr[:], data1=lf[:], initial=0.0, op0=ALU.mult, op1=ALU.add),
              reads=[onesr, lf], writes=[cs])
        pT = self.psb[5]
        for tk in range(NTK):
            kb.op("pe", lambda: nc.tensor.transpose(out=pT[:, (tk % 32) * 16:(tk % 32) * 16 + 16], in_=cs[0:16, tk * 128:(tk + 1) * 128], identity=self.consts[0:16, 0, 0:16]),
                  reads=[cs, self.consts], writes=[pT], partial=(tk > 0), inc=(tk == NTK - 1))
        kb.op("dve", lambda: nc.vector.tensor_copy(out=csT[:].rearrange("p k h -> p (k h)"), in_=pT[:, 0:NTK * 16]), reads=[pT], writes=[csT])
        kb.op("pe", lambda: nc.tensor.matmul(pT[:, 0:NTK * 16], lhsT=self.consts[:, 2, :], rhs=csT[:].rearrange("p k h -> p (k h)"), start=True, stop=True),
              reads=[csT, self.consts], writes=[pT])
        kb.op("dve", lambda: nc.vector.tensor_copy(out=csl[:].rearrange("p k h -> p (k h)"), in_=pT[:, 0:NTK * 16]), reads=[pT], writes=[csl])
        kb.pop()

        kb.push()
        wo = kb.sb("wo", [128, H, D], BF16)
        for h4 in range(4):
            src = W["w_out"].rearrange("(c p) n -> p c n", p=128)[:, h4 * 4:(h4 + 1) * 4, :]
            kb.dma("pool", wo[:, h4 * 4:(h4 + 1) * 4, :], src, writes=[wo], sem_of=wo, partial=True)
        kbuf = [kb.sb("kbuf%d" % i, [128, S], BF16) for i in range(2)]
        vbuf = [kb.sb("vbuf%d" % i, [128, NTK, 128], BF16) for i in range(2)]
        qbuf = [kb.sb("qbuf%d" % i, [128, 512], BF16) for i in range(2)]
        gbuf = [kb.sb("gbuf%d" % i, [128, 512], BF16) for i in range(2)]
        pbuf = [kb.sb("pbuf%d" % i, [128, 512], BF16) for i in range(4)]
        bc = [kb.sb("bc%d" % i, [128, NTK], F32) for i in range(2)]
        rd = kb.sb("rd", [128, 512], F32)
        sg = kb.sb("sg", [128, 512], F32)
        og = kb.sb("og", [128, H, 512], BF16)
        hres = [kb.sb("hres%d" % i, [128, 512], F32) for i in range(2)]
        psO = [self.psb[2], self.psb[3]]
        psD = [self.psb[4], self.psb[5]]
        psY = self.psb[6]
        kp = 0
        v_view = self.v_s.t.rearrange("(k p) n -> p k n", p=128)
        prev = None

        def fox_pv(h, i, nch, c0, pt, vt, O, Dn, gt):
            kb.op("pe", lambda: nc.tensor.matmul(O[:, c0:512], lhsT=vt[:, i, :], rhs=pt[:, c0:512], start=(i == 0), stop=(i == nch - 1)),
                  reads=[vt, pt], writes=[O], partial=(i > 0), inc=False)
            kb.op("pe", lambda: nc.tensor.matmul(Dn[:, c0:512], lhsT=self.ones_bf[:], rhs=pt[:, c0:512], start=(i == 0), stop=(i == nch - 1)),
                  reads=[self.ones_bf, pt], writes=[Dn], partial=(i > 0), inc=True)
            if i == nch - 1:
                kb.op("act", lambda: nc.scalar.activation(out=sg[:], in_=gt[:], func=AF.Exp, scale=-1.0), reads=[gt], writes=[sg])
                kb.op("dve", lambda: nc.vector.scalar_tensor_tensor(out=rd[:], in0=sg[:], scalar=1.0, in1=Dn[:], op0=ALU.add, op1=ALU.mult), reads=[sg, Dn], writes=[rd])
                kb.op("dve", lambda: nc.vector.reciprocal(out=rd[:], in_=rd[:]), reads=[rd], writes=[rd])
                kb.op("dve", lambda: nc.vector.tensor_tensor(out=sg[:], in0=gt[:], in1=rd[:], op=ALU.mult), reads=[gt, rd], writes=[sg])
                kb.op("dve", lambda: nc.vector.tensor_tensor(out=og[:, h, :], in0=O[:], in1=sg[:], op=ALU.mult), reads=[O, sg], writes=[og], partial=True)

        for tb in range(NTB):
            ts = slice(tb * 512, (tb + 1) * 512)
            nch = 4 * (tb + 1)
            for h in range(H):
                hs = slice(h * 128, (h + 1) * 128)
                kt = kbuf[h % 2]; vt = vbuf[h % 2]; qt = qbuf[h % 2]; gt = gbuf[h % 2]; b = bc[h % 2]
                kb.dma("sp", kt[:, 0:nch * 128], self.kT_s[hs, 0:nch * 128], reads=[self.kT_s], writes=[kt], sem_of=kt, partial=False)
                kb.dma("sp", vt[:, 0:nch, :], v_view[:, 0:nch, hs], reads=[self.v_s], writes=[vt], sem_of=vt, partial=False)
                kb.dma("sp", qt[:], self.qT_s[hs, ts], reads=[self.qT_s], writes=[qt], sem_of=qt, partial=False)
                kb.dma("sp", gt[:], self.gT_s[hs, ts], reads=[self.gT_s], writes=[gt], sem_of=gt, partial=False)
                kb.op("dve", lambda: nc.vector.tensor_scalar(out=b[:, 0:nch], in0=csT[:, 0:nch, h], scalar1=csl[:, nch - 1, h:h + 1], scalar2=None, op0=ALU.subtract),
                      reads=[csT, csl], writes=[b])
                O = psO[h % 2]; Dn = psD[h % 2]
                for i in range(nch):
                    a = i - 4 * tb
                    c0 = max(a, 0) * 128
                    ps = self.mmps()
                    kb.op("pe", lambda: nc.tensor.matmul(ps[:, c0:512], lhsT=kt[:, i * 128:(i + 1) * 128], rhs=qt[:, c0:512], start=True, stop=(a < 0)),
                          reads=[kt, qt], writes=[ps], inc=(a < 0))
                    if a >= 0:
                        kb.op("pe", lambda: nc.tensor.matmul(ps[:, c0:c0 + 128], lhsT=self.ident_bf[:], rhs=self.tri_bf[:], start=False, stop=True),
                              reads=[self.ident_bf, self.tri_bf], writes=[ps], partial=True)
                    pt = pbuf[kp % 4]; kp += 1
                    kb.op("act", lambda: nc.scalar.activation(out=pt[:, c0:512], in_=ps[:, c0:512], func=AF.Exp, bias=b[:, i:i + 1], scale=1.0),
                          reads=[ps, b], writes=[pt])
                    if prev is not None:
                        fox_pv(*prev)
                    prev = (h, i, nch, c0, pt, vt, O, Dn, gt)
            fox_pv(*prev)
            prev = None
            self.wout_block(tb, og, wo, hsrc, hdst, hres, psY)
        kb.pop()
        kb.pop()

    def rms_block(self, src, nchk, gcol, dst, nfeat):
        kb, nc = self.kb, self.nc
        pstat = self.psb[6]
        for c in range(nchk):
            s_ = self.r_sq[c % 2]
            kb.op("act", lambda: nc.scalar.activation(out=s_[:], in_=src[:, c, :], func=AF.Square), reads=[src], writes=[s_])
            kb.op("pe", lambda: nc.tensor.matmul(pstat[:], lhsT=self.consts[:, 3, :], rhs=s_[:], start=(c == 0), stop=(c == nchk - 1)),
                  reads=[s_, self.consts], writes=[pstat], partial=(c > 0), inc=True)
        kb.op("act", lambda: nc.scalar.activation(out=self.r_ln[:], in_=pstat[:], func=AF.Ln, scale=1.0 / nfeat, bias=self.epscol[:]),
              reads=[pstat, self.epscol], writes=[self.r_ln])
        kb.op("act", lambda: nc.scalar.activation(out=self.r_rstd[:], in_=self.r_ln[:], func=AF.Exp, scale=-0.5), reads=[self.r_ln], writes=[self.r_rstd])
        for c in range(nchk):
            d_ap, d_t = dst(c)
            kb.op("dve", lambda: nc.vector.scalar_tensor_tensor(out=d_ap, in0=src[:, c, :], scalar=gcol[:, c:c + 1], in1=self.r_rstd[:], op0=ALU.mult, op1=ALU.mult),
                  reads=[src, gcol, self.r_rstd], writes=[d_t], partial=True)

    def dsa_layer(self, li, hsrc, hdst):
        kb, nc, S = self.kb, self.nc, self.S
        W = self.w[li]
        NTB, NTK = self.NTB, self.NTK
        KIT = 14
        TOPK = min(256, S // 4)
        c_s = self.kT_s
        iq_s = self.qT_s
        kb.push()
        absw = kb.sb("absw", [128, NTK, 16], F32)
        sgn = kb.sb("sgn", [128, NTK, 16], F32)
        ckvnT = kb.sb("ckvnT", [128, 2, S], BF16)
        ckvtok = kb.sb("ckvtok", [128, NTK, 256], BF16)
        kb.push()
        hnT = kb.sb("hnT", [128, NC_, S], BF16)
        self.norm_phase(hsrc, li, hnT=hnT)
        wbuf = [kb.sb("wbuf%d" % i, [128, NC_, 128], BF16) for i in range(2)]
        obuf = [kb.sb("obuf%d" % i, [128, S], BF16) for i in range(2)]
        self.proj_cols(hnT, W["w_in"], QR + KVR + IDXD + IDXH, D, self.gT_s, wbuf=wbuf, obuf=obuf)
        self.proj_cols(hnT, W["w_in"], 0, 896, c_s, wbuf=wbuf, obuf=obuf)
        wiw = kb.sb("wiw", [128, NC_, 16], BF16)
        self.load_w_cols(wiw, W["w_in"], QR + KVR + IDXD, 16, NC_)
        pw = self.psb[5]
        for tk in range(NTK):
            for c in range(NC_):
                kb.op("pe", lambda: nc.tensor.matmul(pw[:, tk * 16:(tk + 1) * 16], lhsT=hnT[:, c, tk * 128:(tk + 1) * 128], rhs=wiw[:, c, :], start=(c == 0), stop=(c == NC_ - 1)),
                      reads=[hnT, wiw], writes=[pw], partial=not (tk == 0 and c == 0), inc=(c == NC_ - 1 and tk == NTK - 1))
        kb.op("act", lambda: nc.scalar.activation(out=absw[:].rearrange("p k h -> p (k h)"), in_=pw[:, 0:NTK * 16], func=AF.Abs, scale=1.0 / 32.0),
              reads=[pw], writes=[absw])
        kb.op("act", lambda: nc.scalar.activation(out=sgn[:].rearrange("p k h -> p (k h)"), in_=pw[:, 0:NTK * 16], func=AF.Sign), reads=[pw], writes=[sgn])
        kb.pop()
        kb.push()
        wq = kb.sb("wq", [128, 4, D], BF16)
        kb.dma("pool", wq[:], W["w_q_up"].rearrange("(c p) n -> p c n", p=128), writes=[wq], sem_of=wq, partial=False)
        wiq = kb.sb("wiq", [128, 4, 1024], BF16)
        kb.dma("pool", wiq[:], W["w_iq"].rearrange("(c p) n -> p c n", p=128), writes=[wiq], sem_of=wiq, partial=False)
        wuk = kb.sb("wuk", [128, H, KVR], BF16)
        kb.dma("pool", wuk[:], W["w_ukT"].rearrange("h d r -> d h r"), writes=[wuk], sem_of=wuk, partial=False)
        qn = kb.sb("qn", [128, 4], F32)
        kvn = kb.sb("kvn", [128, 2], F32)
        kb.dma("sp", qn[:], W["qn"], writes=[qn], sem_of=qn, partial=False)
        kb.dma("sp", kvn[:], W["kvn"], writes=[kvn], sem_of=kvn, partial=False)
        self.r_sq = [kb.sb("r_sq%d" % i, [128, 512], F32) for i in range(2)]
        self.r_ln = kb.sb("r_ln", [128, 512], F32)
        self.r_rstd = kb.sb("r_rstd", [128, 512], F32)
        cin = [kb.sb("cin%d" % i, [128, 6, 512], BF16) for i in range(2)]
        cqn = [kb.sb("cqn%d" % i, [128, 4, 512], BF16) for i in range(2)]
        qh = [kb.sb("qh%d" % i, [128, 512], BF16) for i in range(2)]
        qst = [kb.sb("qst%d" % i, [128, 2, 512], BF16) for i in range(2)]
        ist = [kb.sb("ist%d" % i, [128, 512], BF16) for i in range(2)]
        tst = kb.sb("tst", [128, 1024], BF16)
        qa_view = self.qa_s.t.rearrange("(h r p) t -> p h r t", r=2, p=128)
        c_view = c_s.t.rearrange("(c p) t -> p c t", p=128)
        for tb in range(NTB):
            ts = slice(tb * 512, (tb + 1) * 512)
            ci = cin[tb % 2]; cn = cqn[tb % 2]
            kb.dma("sp", ci[:], c_view[:, 0:6, ts], reads=[c_s], writes=[ci], sem_of=ci, partial=False)
            self.rms_block(ci, 4, qn, lambda c: (cn[:, c, :], cn), QR)
            ckv_src = T(ci.t[:, 4:6, :], "x"); ckv_src.dep = ci.dep
            self.rms_block(ckv_src, 2, kvn, lambda c: (ckvnT[:, c, ts], ckvnT), KVR)
            for q4 in range(4):
                tk = tb * 4 + q4
                for rc in range(2):
                    kb.op("pe", lambda: nc.tensor.transpose(out=self.pst[:, (q4 * 2 + rc) * 128:(q4 * 2 + rc + 1) * 128], in_=ckvnT[:, rc, tk * 128:(tk + 1) * 128], identity=self.ident_bf[:]),
                          reads=[ckvnT, self.ident_bf], writes=[self.pst], partial=not (q4 == 0 and rc == 0), inc=(q4 == 3 and rc == 1))
            kb.op("dve", lambda: nc.vector.tensor_copy(out=ckvtok[:, tb * 4:(tb + 1) * 4, :].rearrange("p k r -> p (k r)"), in_=self.pst[:, :]), reads=[self.pst], writes=[ckvtok], partial=True)
            for h in range(H):
                ps = self.mmps()
                for rc in range(4):
                    kb.op("pe", lambda: nc.tensor.matmul(ps[:], lhsT=wq[:, rc, h * 128:(h + 1) * 128], rhs=cn[:, rc, :], start=(rc == 0), stop=(rc == 3)),
                          reads=[wq, cn], writes=[ps], partial=(rc > 0), inc=(rc == 3))
                qt = qh[h % 2]
                self.evac(qt[:], ps[:], DH ** -0.5, [ps], qt, partial=False)
                st = qst[h % 2]
                for r2 in range(2):
                    ps2 = self.mmps()
                    kb.op("pe", lambda: nc.tensor.matmul(ps2[:], lhsT=wuk[:, h, r2 * 128:(r2 + 1) * 128], rhs=qt[:], start=True, stop=True), reads=[wuk, qt], writes=[ps2])
                    self.evac(st[:, r2, :], ps2[:], 1.0, [ps2], st, partial=(r2 > 0))
                kb.dma("sp", qa_view[:, h, :, ts], st[:], reads=[st], writes=[self.qa_s], sem_of=st)
            for m in range(8):
                ps = self.mmps()
                for rc in range(4):
                    kb.op("pe", lambda: nc.tensor.matmul(ps[:], lhsT=wiq[:, rc, m * 128:(m + 1) * 128], rhs=cn[:, rc, :], start=(rc == 0), stop=(rc == 3)),
                          reads=[wiq, cn], writes=[ps], partial=(rc > 0), inc=(rc == 3))
                it = ist[m % 2]
                self.evac(it[:], ps[:], 1.0, [ps], it, partial=False)
                kb.dma("sp", iq_s[m * 128:(m + 1) * 128, ts], it[:], reads=[it], writes=[iq_s], sem_of=it)
        kb.pop()
        kb.push()
        ikT = kb.sb("ikT", [64, S], BF16)
        kb.dma("sp", ikT[:], c_s[768:832, :], reads=[c_s], writes=[ikT], sem_of=ikT, partial=False)
        iqb = [kb.sb("iqb%d" % i, [64, 16, 128], BF16) for i in range(2)]
        accb = [kb.sb("acc%d" % i, [128, S], F32) for i in range(2)]
        junk = kb.sb("junk", [128, S], BF16)
        mkb = [kb.sb("mk%d" % i, [128, S], BF16) for i in range(2)]
        rb = [kb.sb("rb%d" % i, [128, 512], BF16) for i in range(4)]
        Dgb = [kb.sb("Dg%d" % i, [128, IDXH, 128], BF16) for i in range(2)]
        Mx = kb.sb("Mx", [128, 1], F32)
        stepv = kb.sb("stepv", [128, KIT + 1], F32)
        mid = kb.sb("mid", [128, KIT + 1], F32)
        cnt = kb.sb("cnt", [128, KIT + 1], F32)
        uu = kb.sb("uu", [128, 1], F32)
        thr = kb.sb("thr", [128, 1], F32)
        mst = [kb.sb("mst%d" % i, [128, NTK, 128], BF16) for i in range(2)]
        L01 = kb.sb("L01", [128, 128], BF16)
        kb.op("dve", lambda: nc.vector.tensor_copy(out=L01[:], in_=self.consts[:, 4, :]), reads=[self.consts], writes=[L01])
        iq_view = iq_s.t[0:1024, :].rearrange("(h d) t -> d h t", d=64)
        m_view = self.mT_s.t.rearrange("(c p) t -> p c t", p=128)
        kr = 0
        ka = 0
        paccb = [self.psb[2], self.psb[3]]

        def idx_acc(h, pacc, wd, r_, Dg, acc, ss):
            kb.op("pe", lambda: nc.tensor.matmul(pacc[:, 0:wd], lhsT=Dg[:, h, :], rhs=r_[:, 0:wd], start=(h == 0), stop=(h == IDXH - 1)),
                  reads=[Dg, r_], writes=[pacc], partial=(h > 0), inc=True)
            if h == IDXH - 1:
                self.evac(acc[:, ss], pacc[:, 0:wd], 1.0, [pacc], acc)

        def emit_mask(qb):
            mk = mkb[qb % 2]
            tq = slice(qb * 128, (qb + 1) * 128)
            ms = mst[qb % 2]
            for c0 in range(0, qb + 1, 8):
                ncb = min(8, qb + 1 - c0)
                for c in range(ncb):
                    kb.op("pe", lambda: nc.tensor.transpose(out=self.pst[:, c * 128:(c + 1) * 128], in_=mk[:, (c0 + c) * 128:(c0 + c + 1) * 128], identity=self.ident_bf[:]),
                          reads=[mk, self.ident_bf], writes=[self.pst], partial=(c > 0), inc=(c == ncb - 1))
                kb.op("act", lambda: nc.scalar.activation(out=ms[:, c0:c0 + ncb, :].rearrange("p c t -> p (c t)"), in_=self.pst[:, 0:ncb * 128], func=AF.Copy), reads=[self.pst], writes=[ms], partial=(c0 > 0))
            kb.dma("sp", m_view[:, 0:qb + 1, tq], ms[:, 0:qb + 1, :], reads=[ms], writes=[self.mT_s], sem_of=ms)

        for qb in range(NTK):
            n = (qb + 1) * 128
            tq = slice(qb * 128, (qb + 1) * 128)
            mk = mkb[qb % 2]
            if n > TOPK:
                acc = accb[qb % 2]
                Dg = Dgb[qb % 2]
                iqt = iqb[qb % 2]
                kb.dma("sp", iqt[:], iq_view[:, :, tq], reads=[iq_s], writes=[iqt], sem_of=iqt, partial=False)
                for h in range(IDXH):
                    kb.op("pool", lambda: nc.gpsimd.tensor_scalar(out=Dg[:, h, :], in0=self.ident_bf[:], scalar1=sgn[:, qb, h:h + 1], scalar2=1.0, op0=ALU.mult, op1=ALU.mult),
                          reads=[self.ident_bf, sgn], writes=[Dg], partial=(h > 0))
                pend = []
                for sb_ in range((n + 511) // 512):
                    wd = min(512, n - sb_ * 512)
                    ss = slice(sb_ * 512, sb_ * 512 + wd)
                    pacc = paccb[ka % 2]; ka += 1
                    for h in range(IDXH):
                        ps = self.mmps()
                        kb.op("pe", lambda: nc.tensor.matmul(ps[:, 0:wd], lhsT=iqt[:, h, :], rhs=ikT[:, ss], start=True, stop=True), reads=[iqt, ikT], writes=[ps])
                        r_ = rb[kr % 4]; kr += 1
                        kb.op("act", lambda: nc.scalar.activation(out=r_[:, 0:wd], in_=ps[:, 0:wd], func=AF.Relu, scale=absw[:, qb, h:h + 1]), reads=[ps, absw], writes=[r_])
                        pend.append((h, pacc, wd, r_, Dg, acc, ss))
                        if len(pend) > 1:
                            idx_acc(*pend.pop(0))
                while pend:
                    idx_acc(*pend.pop(0))
                kb.op("dve", lambda: nc.vector.tensor_reduce(out=Mx[:], in_=acc[:, 0:n], axis=AX.X, op=ALU.max, apply_absolute_value=True), reads=[acc], writes=[Mx])
                kb.op("dve", lambda: nc.vector.tensor_tensor(out=acc[:, tq], in0=acc[:, tq], in1=self.consts[:, 5, :], op=ALU.add), reads=[acc, self.consts], writes=[acc])
                kb.op("dve", lambda: nc.vector.tensor_scalar(out=stepv[:], in0=self.pow2[:, 0:KIT + 1], scalar1=Mx[:, 0:1], scalar2=None, op0=ALU.mult), reads=[self.pow2, Mx], writes=[stepv])
                kb.op("dve", lambda: nc.vector.memset(mid[:, 0:1], 0.0), writes=[mid])
                for k in range(KIT):
                    kb.op("dve", lambda: nc.vector.tensor_scalar(out=junk[:, 0:n], in0=acc[:, 0:n], scalar1=mid[:, k:k + 1], scalar2=None, op0=ALU.is_ge, op1=ALU.add, accum_out=cnt[:, k:k + 1]),
                          reads=[acc, mid], writes=[junk, cnt])
                    kb.op("dve", lambda: nc.vector.tensor_scalar(out=uu[:], in0=cnt[:, k:k + 1], scalar1=TOPK - 0.5, scalar2=stepv[:, k:k + 1], op0=ALU.is_ge, op1=ALU.mult),
                          reads=[cnt, stepv], writes=[uu])
                    kb.op("dve", lambda: nc.vector.scalar_tensor_tensor(out=mid[:, k + 1:k + 2], in0=uu[:], scalar=stepv[:, k + 1:k + 2], in1=mid[:, k:k + 1], op0=ALU.subtract, op1=ALU.add),
                          reads=[uu, stepv, mid], writes=[mid])
                kb.op("dve", lambda: nc.vector.tensor_tensor(out=thr[:], in0=mid[:, KIT:KIT + 1], in1=stepv[:, KIT:KIT + 1], op=ALU.subtract), reads=[mid, stepv], writes=[thr])
                kb.op("dve", lambda: nc.vector.tensor_scalar(out=mk[:, 0:n], in0=acc[:, 0:n], scalar1=thr[:, 0:1], scalar2=None, op0=ALU.is_ge), reads=[acc, thr], writes=[mk])
            else:
                if qb > 0:
                    kb.op("dve", lambda: nc.vector.memset(mk[:, 0:qb * 128], 1.0), writes=[mk])
                kb.op("dve", lambda: nc.vector.tensor_copy(out=mk[:, tq], in_=L01[:]), reads=[L01], writes=[mk], partial=(qb > 0))
            if qb >= 1:
                emit_mask(qb - 1)
        emit_mask(NTK - 1)
        kb.pop()
        kb.push()
        wo = kb.sb("wo", [128, H, D], BF16)
        for h4 in range(4):
            src = W["w_out"].rearrange("(c p) n -> p c n", p=128)[:, h4 * 4:(h4 + 1) * 4, :]
            kb.dma("pool", wo[:, h4 * 4:(h4 + 1) * 4, :], src, writes=[wo], sem_of=wo, partial=True)
        wuv = kb.sb("wuv", [128, 2, D], BF16)
        kb.dma("pool", wuv[:], W["w_uv"].rearrange("(c p) n -> p c n", p=128), writes=[wuv], sem_of=wuv, partial=False)
        b31 = kb.sb("b31", [128, 16], F32)
        kb.dma("sp", b31[:], W["rb31"], writes=[b31], sem_of=b31, partial=False)
        Dt = kb.sb("Dt", [128, H, 2, 128], BF16)
        dtmp = kb.sb("dtmp", [128, H, 2, 128], F32)
        kb.dma("sp", dtmp[:], W["rbD"], writes=[dtmp], sem_of=dtmp, partial=False)
        for h in range(H):
            kb.op("dve", lambda: nc.vector.tensor_scalar(out=Dt[:, h, :, :], in0=dtmp[:, h, :, :], scalar1=b31[:, h:h + 1], scalar2=None, op0=ALU.subtract),
                  reads=[dtmp, b31], writes=[Dt], partial=(h > 0))
        mT = kb.sb("mT", [128, NTK, 512], BF16)
        qab = [kb.sb("qab%d" % i, [128, 2, 512], BF16) for i in range(2)]
        gbuf = [kb.sb("gbuf%d" % i, [128, 512], BF16) for i in range(2)]
        pbuf = [kb.sb("pbuf%d" % i, [128, 512], BF16) for i in range(5)]
        ol = kb.sb("ol", [128, 2, 512], BF16)
        rd = kb.sb("rd", [128, 512], F32)
        sg = kb.sb("sg", [128, 512], F32)
        og = kb.sb("og", [128, H, 512], BF16)
        hres = [kb.sb("hres%d" % i, [128, 512], F32) for i in range(2)]
        O0, O1, Dn, psU, psY = self.psb[2], self.psb[3], self.psb[4], self.psb[6], self.psb[6]
        stb = [self.psb[0], self.psb[1], self.psb[5]]
        kp = 0
        pend = []
        LOOK = 2

        def dsa_pv(h, i, nch, c0, pt, gt):
            hs = slice(h * 128, (h + 1) * 128)
            kb.op("pe", lambda: nc.tensor.matmul(O0[:, c0:512], lhsT=ckvtok[:, i, 0:128], rhs=pt[:, c0:512], start=(i == 0), stop=(i == nch - 1)),
                  reads=[ckvtok, pt], writes=[O0], partial=(i > 0), inc=False)
            kb.op("pe", lambda: nc.tensor.matmul(O1[:, c0:512], lhsT=ckvtok[:, i, 128:256], rhs=pt[:, c0:512], start=(i == 0), stop=(i == nch - 1)),
                  reads=[ckvtok, pt], writes=[O1], partial=(i > 0), inc=False)
            kb.op("pe", lambda: nc.tensor.matmul(Dn[:, c0:512], lhsT=self.ones_bf[:], rhs=pt[:, c0:512], start=(i == 0), stop=(i == nch - 1)),
                  reads=[self.ones_bf, pt], writes=[Dn], partial=(i > 0), inc=True)
            if i == nch - 1:
                kb.op("act", lambda: nc.scalar.activation(out=ol[:, 0, :], in_=O0[:], func=AF.Copy), reads=[O0], writes=[ol])
                kb.op("dve", lambda: nc.vector.tensor_copy(out=ol[:, 1, :], in_=O1[:]), reads=[O1], writes=[ol], partial=True)
                kb.op("act", lambda: nc.scalar.activation(out=sg[:], in_=gt[:], func=AF.Exp, scale=-1.0), reads=[gt], writes=[sg])
                kb.op("dve", lambda: nc.vector.scalar_tensor_tensor(out=rd[:], in0=sg[:], scalar=1.0, in1=Dn[:], op0=ALU.add, op1=ALU.mult), reads=[sg, Dn], writes=[rd])
                kb.op("dve", lambda: nc.vector.reciprocal(out=rd[:], in_=rd[:]), reads=[rd], writes=[rd])
                kb.op("pe", lambda: nc.tensor.matmul(psU[:], lhsT=wuv[:, 0, hs], rhs=ol[:, 0, :], start=True, stop=False), reads=[wuv, ol], writes=[psU], inc=False)
                kb.op("pe", lambda: nc.tensor.matmul(psU[:], lhsT=wuv[:, 1, hs], rhs=ol[:, 1, :], start=False, stop=True), reads=[wuv, ol], writes=[psU], partial=True)
                kb.op("dve", lambda: nc.vector.tensor_tensor(out=sg[:], in0=gt[:], in1=rd[:], op=ALU.mult), reads=[gt, rd], writes=[sg])
                kb.op("dve", lambda: nc.vector.tensor_tensor(out=og[:, h, :], in0=psU[:], in1=sg[:], op=ALU.mult), reads=[psU, sg], writes=[og], partial=True)

        for tb in range(NTB):
            ts = slice(tb * 512, (tb + 1) * 512)
            nch = 4 * (tb + 1)
            kb.dma("sp", mT[:, 0:nch, :], m_view[:, 0:nch, ts], reads=[self.mT_s], writes=[mT], sem_of=mT, partial=False)
            for h in range(H):
                hs = slice(h * 128, (h + 1) * 128)
                qa = qab[h % 2]; gt = gbuf[h % 2]
                kb.dma("sp", qa[:], qa_view[:, h, :, ts], reads=[self.qa_s], writes=[qa], sem_of=qa, partial=False)
                kb.dma("sp", gt[:], self.gT_s[hs, ts], reads=[self.gT_s], writes=[gt], sem_of=gt, partial=False)
                for i in range(nch):
                    a = i - 4 * tb
                    c0 = max(a, 0) * 128
                    extra = [(c, 4 * tb + c - i) for c in range(4) if (4 * tb + c - i) in (0, 1) and c * 128 >= c0]
                    ps = stb[kp % 3]
                    kb.op("pe", lambda: nc.tensor.matmul(ps[:, c0:512], lhsT=ckvnT[:, 0, i * 128:(i + 1) * 128], rhs=qa[:, 0, c0:512], start=True, stop=False),
                          reads=[ckvnT, qa], writes=[ps], inc=False)
                    kb.op("pe", lambda: nc.tensor.matmul(ps[:, c0:512], lhsT=ckvnT[:, 1, i * 128:(i + 1) * 128], rhs=qa[:, 1, c0:512], start=False, stop=(len(extra) == 0)),
                          reads=[ckvnT, qa], writes=[ps], partial=True, inc=(len(extra) == 0))
                    for ei, (c, df) in enumerate(extra):
                        kb.op("pe", lambda: nc.tensor.matmul(ps[:, c * 128:(c + 1) * 128], lhsT=self.ident_bf[:], rhs=Dt[:, h, df, :], start=False, stop=(ei == len(extra) - 1)),
                              reads=[self.ident_bf, Dt], writes=[ps], partial=True, inc=(ei == len(extra) - 1))
                    pt = pbuf[kp % 5]; kp += 1
                    kb.op("act", lambda: nc.scalar.activation(out=pt[:, c0:512], in_=ps[:, c0:512], func=AF.Exp, bias=b31[:, h:h + 1], scale=1.0),
                          reads=[ps, b31], writes=[pt])
                    if kp % 2 == 0:
                        kb.op("dve", lambda: nc.vector.tensor_tensor(out=pt[:, c0:512], in0=pt[:, c0:512], in1=mT[:, i, c0:512], op=ALU.mult), reads=[pt, mT], writes=[pt])
                    else:
                        kb.op("pool", lambda: nc.gpsimd.tensor_tensor(out=pt[:, c0:512], in0=pt[:, c0:512], in1=mT[:, i, c0:512], op=ALU.mult), reads=[pt, mT], writes=[pt])
                    pend.append((h, i, nch, c0, pt, gt))
                    if len(pend) > LOOK:
                        dsa_pv(*pend.pop(0))
            while pend:
                dsa_pv(*pend.pop(0))
            self.wout_block(tb, og, wo, hsrc, hdst, hres, psY)
        kb.pop()
        kb.pop()


def make_consts():
    c = np.zeros((128, 6, 128), np.float32)
    pp = np.arange(128)[:, None]; jj = np.arange(128)[None, :]
    c[:, 4, :] = np.where(jj <= pp, 1.0, 0.0)
    c[:, 5, :] = np.where(jj <= pp, 0.0, -1e30)
    c[:, 0, :] = np.eye(128, dtype=np.float32)
    sp = np.arange(128)[:, None]; tp = np.arange(128)[None, :]
    c[:, 1, :] = np.where(sp <= tp, 0.0, NEG)
    c[127, 2, :] = 1.0
    c[:, 3, :] = 1.0
    return c


def cols16(v):
    return np.ascontiguousarray(v.reshape(16, 128).T)


def t5_bucket_np(dist):
    import math
    max_exact = 16
    d = np.maximum(dist, 0)
    df = np.maximum(d, 1).astype(np.float32)
    large = max_exact + (np.log(df / max_exact) / math.log(128 / max_exact) * (32 - max_exact)).astype(np.int32)
    large = np.minimum(large, 31)
    return np.where(d < max_exact, d, large)


def shared_inputs(p, layers, do_final=True):
    im = {"consts": make_consts()}
    g = np.zeros((128, 5, 16), np.float32)
    for i in range(4):
        g[:, i, :] = cols16(p["norm_g"][i])
    g[:, 4, :] = cols16(p["final_g"])
    im["gcols"] = g
    im["pow2"] = np.ascontiguousarray(np.broadcast_to((2.0 ** -np.arange(17, dtype=np.float64)).astype(np.float32), (128, 17)))
    sp = np.arange(128)[:, None]; tp = np.arange(128)[None, :]
    bk = np.stack([t5_bucket_np(tp - sp), t5_bucket_np(128 + tp - sp)], 0)
    for kind, li in layers:
        j = li // 2
        if kind == "b":
            im["w_in%d" % li] = np.ascontiguousarray(p["b_w_in"][j])
            im["f_bias%d" % li] = np.ascontiguousarray(p["b_f_bias"][j].reshape(16, 1))
            im["w_out%d" % li] = np.ascontiguousarray(p["b_w_out"][j])
        else:
            im["w_in%d" % li] = np.ascontiguousarray(p["a_w_in"][j])
            im["qn%d" % li] = np.ascontiguousarray(p["a_q_norm"][j].reshape(4, 128).T)
            im["kvn%d" % li] = np.ascontiguousarray(p["a_kv_norm"][j].reshape(2, 128).T)
            im["w_q_up%d" % li] = np.ascontiguousarray(p["a_w_q_up"][j])
            im["w_ukT%d" % li] = np.ascontiguousarray(np.transpose(p["a_w_uk"][j], (1, 2, 0)))
            im["w_uv%d" % li] = np.ascontiguousarray(p["a_w_uv"][j].reshape(KVR, D))
            im["w_iq%d" % li] = np.ascontiguousarray(p["a_w_iq"][j])
            im["w_out%d" % li] = np.ascontiguousarray(p["a_w_out"][j])
            rb = p["rel_bias"]
            im["rb31_%d" % li] = np.ascontiguousarray(np.broadcast_to(rb[31][None, :], (128, 16)))
            gath = rb[bk]
            im["rbD_%d" % li] = np.ascontiguousarray(np.transpose(gath, (1, 3, 0, 2)))
    return im


_PROG_CACHE = {}


def get_prog(S, layers, do_final):
    key = (S, tuple(layers), do_final)
    if key not in _PROG_CACHE:
        _PROG_CACHE[key] = Prog(S, list(layers), do_final=do_final)
    return _PROG_CACHE[key]


LAYERS = [("a", 0), ("b", 1), ("a", 2), ("b", 3)]
FUSED = True


def run_layers(xT_list, p, layers, do_final):
    S = xT_list[0].shape[1]
    prog = get_prog(S, layers, do_final)
    sh = shared_inputs(p, layers, do_final)
    in_maps = []
    for xT in xT_list:
        m = dict(sh)
        m["xT"] = xT
        in_maps.append(m)
    res = run_bass_kernel_spmd(prog.nc, in_maps, core_ids=list(range(len(xT_list))))
    key = "outT" if do_final else "hT"
    return [r[key] for r in res.results]


def kernel(**inputs):
    p = {k: np.asarray(v) for k, v in inputs.items()}
    x = p["x"]
    B = x.shape[0]
    xT = [np.ascontiguousarray(x[b].T) for b in range(B)]
    if FUSED:
        outT = run_layers(xT, p, LAYERS, True)
    else:
        cur = xT
        for n, lay in enumerate(LAYERS):
            cur = run_layers(cur, p, [lay], n == len(LAYERS) - 1)
        outT = cur
    return np.stack([np.ascontiguousarray(o.T) for o in outT], 0).astype(np.float32)
```
